# Optimizing a Trainium2 kernel written in Bass

```python
import math
import jax, jax.numpy as jnp
from jax import lax
import numpy as np

D_MODEL = 1024
BATCH = 16
SEQ = 2048
DEPTH = 2

GRID_W = 64
N_GROUPS = 4
GROUP_W = D_MODEL // N_GROUPS
MIX_W = N_GROUPS * GROUP_W
SSD_HEAD_DIM = 64
SSD_HEADS = GROUP_W // SSD_HEAD_DIM
SSD_BC_GROUPS = 2
SSD_STATE = 128
SSD_CONV = 3
SSD_CHUNK = 128
SSD_XBC = GROUP_W + 2 * SSD_BC_GROUPS * SSD_STATE
SSD_IN = GROUP_W + SSD_XBC + 2 * SSD_HEADS
NA_HEAD_DIM = 64
NA_HEADS = GROUP_W // NA_HEAD_DIM
NA_KH = 8
NA_KW = 16
NA_QB = 16
NA_KSPAN = 32
NA_IN = 3 * GROUP_W
ML_HEAD_DIM = 64
ML_HEADS = GROUP_W // ML_HEAD_DIM
ML_CHUNK = 128
ML_IN = 4 * GROUP_W + 4 * ML_HEADS
HY_CH = GROUP_W
HY_ORDER = 2
HY_CONV = 3
HY_EMB = 33
HY_FFN = 64
HY_SHORT_DECAY_FRAC = 0.3
HY_LONG_DECAY_FRAC = 1.5
HY_DECAY_TARGET = 1e-2
HY_IN = 3 * HY_CH
OFF_NA = SSD_IN
OFF_ML = OFF_NA + NA_IN
OFF_HY = OFF_ML + ML_IN
P_IN = OFF_HY + HY_IN
FFN_HIDDEN = int(math.ceil(8 * D_MODEL / 3 / 256)) * 256
NORM_EPS = 1e-6

kernel_name = "hybrid_parallel_mixer_encoder"

F32 = jnp.float32


def rms_norm(x, gain=None):
    xf = x.astype(F32)
    y = xf * lax.rsqrt(jnp.mean(xf * xf, axis=-1, keepdims=True) + NORM_EPS)
    if gain is not None:
        y = y * gain.astype(F32)
    return y.astype(x.dtype)


def depthwise_conv(x, w, b):
    K = w.shape[0]
    y = lax.conv_general_dilated(x, w[:, None, :].astype(x.dtype), window_strides=(1,),
                                 padding=[(K // 2, K // 2)],
                                 dimension_numbers=('NWC', 'WIO', 'NWC'),
                                 feature_group_count=x.shape[-1])
    return y + b.astype(x.dtype)


def ssd_chunk_scan(x, dt, A, Bm, Cm):
    b, L, H, P = x.shape
    N = Bm.shape[-1]
    T = SSD_CHUNK
    nc = L // T
    x = x.reshape(b, nc, T, H, P)
    Bm = Bm.reshape(b, nc, T, H, N)
    Cm = Cm.reshape(b, nc, T, H, N)
    dt = dt.reshape(b, nc, T, H)
    a_cum = jnp.cumsum(jnp.moveaxis(dt * A, -1, 1), axis=-1)
    xdt = x * dt[..., None]
    lower = jnp.tril(jnp.ones((T, T), bool))
    decay = jnp.exp(jnp.where(lower, a_cum[..., :, None] - a_cum[..., None, :], -jnp.inf))
    scores = jnp.einsum('bcthn,bcshn->bhcts', Cm, Bm) * decay
    y_diag = jnp.einsum('bhcts,bcshp->bcthp', scores, xdt)
    decay_to_end = jnp.exp(a_cum[..., -1:] - a_cum)
    chunk_states = jnp.einsum('bcshn,bhcs,bcshp->bchpn', Bm, decay_to_end, xdt)
    chunk_decay = jnp.exp(a_cum[..., -1])

    def step(state, inp):
        s_c, d_c = inp
        return state * d_c[..., None, None] + s_c, state

    _, prev = lax.scan(step, jnp.zeros((b, H, P, N), x.dtype),
                       (jnp.moveaxis(chunk_states, 1, 0), jnp.moveaxis(chunk_decay, 2, 0)))
    prev = jnp.moveaxis(prev, 0, 1)
    y_off = jnp.einsum('bcthn,bchpn,bhct->bcthp', Cm, prev, jnp.exp(a_cum))
    return (y_diag + y_off).reshape(b, L, H, P)


def ssd_mixer(u, conv_w, conv_b, dt_bias, a_log, d_skip, norm_g):
    b, L, _ = u.shape
    z = u[..., :GROUP_W]
    xbc = jax.nn.silu(depthwise_conv(u[..., GROUP_W:GROUP_W + SSD_XBC], conv_w, conv_b)).astype(F32)
    dt_raw = u[..., GROUP_W + SSD_XBC:].astype(F32).reshape(b, L, 2, SSD_HEADS)
    xs = xbc[..., :GROUP_W].reshape(b, L, SSD_HEADS, SSD_HEAD_DIM)
    rep = SSD_HEADS // SSD_BC_GROUPS
    nb = SSD_BC_GROUPS * SSD_STATE
    Bm = jnp.repeat(xbc[..., GROUP_W:GROUP_W + nb].reshape(b, L, SSD_BC_GROUPS, SSD_STATE), rep, axis=2)
    Cm = jnp.repeat(xbc[..., GROUP_W + nb:].reshape(b, L, SSD_BC_GROUPS, SSD_STATE), rep, axis=2)
    dt = jax.nn.softplus(dt_raw + dt_bias.astype(F32))
    A = -jnp.exp(a_log.astype(F32))
    flip = lambda t: jnp.flip(t, axis=1)
    y_fwd = ssd_chunk_scan(xs, dt[:, :, 0], A[0], Bm, Cm)
    y_bwd = flip(ssd_chunk_scan(flip(xs), flip(dt[:, :, 1]), A[1], flip(Bm), flip(Cm)))
    y = y_fwd + y_bwd + d_skip.astype(F32)[:, None] * xs
    y = y.reshape(b, L, GROUP_W) * jax.nn.silu(z.astype(F32))
    return rms_norm(y, norm_g).astype(u.dtype)


def neighborhood_attention(u, rpb):
    b, L, _ = u.shape
    rows = L // GRID_W
    kh = min(NA_KH, rows)
    q, k, v = [t.reshape(b, rows, GRID_W, NA_HEADS, NA_HEAD_DIM).transpose(0, 3, 1, 2, 4)
               for t in jnp.split(u, 3, axis=-1)]
    r = jnp.arange(rows)
    row_idx = jnp.clip(r - kh // 2, 0, rows - kh)[:, None] + jnp.arange(kh)[None, :]
    dr_idx = row_idx - r[:, None] + (NA_KH - 1)
    scale = NA_HEAD_DIM ** -0.5

    def col_block(j0):
        ks0 = jnp.clip(j0 - NA_KW // 2, 0, GRID_W - NA_KSPAN)
        qb = lax.dynamic_slice_in_dim(q, j0, NA_QB, axis=3)
        kb = lax.dynamic_slice_in_dim(k, ks0, NA_KSPAN, axis=3)[:, :, row_idx]
        vb = lax.dynamic_slice_in_dim(v, ks0, NA_KSPAN, axis=3)[:, :, row_idx]
        j = j0 + jnp.arange(NA_QB)
        cs = jnp.clip(j - NA_KW // 2, 0, GRID_W - NA_KW)
        kc = ks0 + jnp.arange(NA_KSPAN)
        valid = (kc[None, :] >= cs[:, None]) & (kc[None, :] < cs[:, None] + NA_KW)
        dc_idx = jnp.clip(kc[None, :] - j[:, None] + (NA_KW - 1), 0, 2 * NA_KW - 2)
        bias = rpb[:, dr_idx[:, None, :, None], dc_idx[None, :, None, :]]
        s = jnp.einsum('bhrqd,bhrkud->bhrqku', qb, kb).astype(F32) * scale + bias.astype(F32)
        s = jnp.where(valid[:, None, :], s, -jnp.inf)
        p = jax.nn.softmax(s.reshape(s.shape[:4] + (kh * NA_KSPAN,)), axis=-1)
        p = p.reshape(s.shape).astype(u.dtype)
        return jnp.einsum('bhrqku,bhrkud->bhrqd', p, vb)

    o = lax.map(col_block, jnp.arange(0, GRID_W, NA_QB))
    return o.transpose(1, 3, 0, 4, 2, 5).reshape(b, L, GROUP_W)


def mlstm_scan(q, k, v, log_i, log_f):
    b, H, L, d = q.shape
    T = ML_CHUNK
    nc = L // T
    to_chunks = lambda t: jnp.moveaxis(t.reshape(t.shape[:2] + (nc, T) + t.shape[3:]), 2, 0)
    lower = jnp.tril(jnp.ones((T, T), bool))

    def step(carry, inp):
        C, n, m = carry
        qc, kc, vc, li, lf = inp
        bcum = jnp.cumsum(lf, axis=-1)
        g_inter = bcum + m[..., None]
        g_intra = jnp.where(lower, bcum[..., :, None] - bcum[..., None, :] + li[..., None, :], -jnp.inf)
        m_t = jnp.maximum(g_inter, jnp.max(g_intra, axis=-1))
        w_inter = jnp.exp(g_inter - m_t)
        s = jnp.einsum('bhtd,bhsd->bhts', qc, kc) * jnp.exp(g_intra - m_t[..., None])
        num = w_inter[..., None] * jnp.einsum('bhtd,bhde->bhte', qc, C) + jnp.einsum('bhts,bhse->bhte', s, vc)
        den = w_inter * jnp.einsum('bhtd,bhd->bht', qc, n) + jnp.sum(s, axis=-1)
        h = num / jnp.maximum(jnp.abs(den), jnp.exp(-m_t))[..., None]
        g_state = bcum[..., -1] + m
        g_src = bcum[..., -1:] - bcum + li
        m_new = jnp.maximum(g_state, jnp.max(g_src, axis=-1))
        w_src = jnp.exp(g_src - m_new[..., None])
        carry_decay = jnp.exp(g_state - m_new)
        C_new = carry_decay[..., None, None] * C + jnp.einsum('bhs,bhsd,bhse->bhde', w_src, kc, vc)
        n_new = carry_decay[..., None] * n + jnp.einsum('bhs,bhsd->bhd', w_src, kc)
        return (C_new, n_new, m_new), h

    init = (jnp.zeros((b, H, d, d), F32), jnp.zeros((b, H, d), F32), jnp.zeros((b, H), F32))
    _, h = lax.scan(step, init, (to_chunks(q), to_chunks(k), to_chunks(v), to_chunks(log_i), to_chunks(log_f)))
    return jnp.moveaxis(h, 0, 2).reshape(b, H, L, d)


def mlstm_mixer(u, i_bias, f_bias, norm_g):
    b, L, _ = u.shape
    heads = lambda t: t.astype(F32).reshape(b, L, ML_HEADS, ML_HEAD_DIM).transpose(0, 2, 1, 3)
    q = heads(u[..., :GROUP_W])
    k = heads(u[..., GROUP_W:2 * GROUP_W]) * (ML_HEAD_DIM ** -0.5)
    v = heads(u[..., 2 * GROUP_W:3 * GROUP_W])
    o = u[..., 3 * GROUP_W:4 * GROUP_W].astype(F32)
    gates = u[..., 4 * GROUP_W:].astype(F32).reshape(b, L, 2, 2, ML_HEADS).transpose(2, 3, 0, 4, 1)
    log_i = gates[0] + i_bias.astype(F32)[:, None, :, None]
    log_f = jax.nn.log_sigmoid(gates[1] + f_bias.astype(F32)[:, None, :, None])
    flip = lambda t: jnp.flip(t, axis=2)
    h_fwd = mlstm_scan(q, k, v, log_i[0], log_f[0])
    h_bwd = flip(mlstm_scan(flip(q), flip(k), flip(v), flip(log_i[1]), flip(log_f[1])))
    h = rms_norm(h_fwd + h_bwd, norm_g.reshape(ML_HEADS, 1, ML_HEAD_DIM))
    h = h.transpose(0, 2, 1, 3).reshape(b, L, GROUP_W)
    return (jax.nn.sigmoid(o) * h).astype(u.dtype)


def hyena_filters(L, w1, b1, w2, b2, w3, freq):
    t = jnp.linspace(0.0, 1.0, L, dtype=F32)[:, None]
    bands = (HY_EMB - 1) // 2
    f = jnp.linspace(1e-4, bands - 1, bands, dtype=F32)
    ang = (2.0 * math.pi) * (jnp.arange(L, dtype=F32) / L)[:, None] * f[None, :]
    feats = jnp.concatenate([t, jnp.cos(ang), -jnp.sin(ang)], axis=-1)
    freq = freq.astype(F32)
    h = jnp.sin(freq[0] * (feats @ w1.astype(F32) + b1.astype(F32)))
    h = jnp.sin(freq[1] * (h @ w2.astype(F32) + b2.astype(F32)))
    h = (h @ w3.astype(F32)).reshape(L, HY_ORDER, 2, HY_CH)
    deltas = jnp.abs(jnp.linspace(math.log(HY_DECAY_TARGET) / HY_LONG_DECAY_FRAC,
                                  math.log(HY_DECAY_TARGET) / HY_SHORT_DECAY_FRAC, HY_CH, dtype=F32))
    h = h * jnp.exp(-t * deltas[None, :])[:, None, None, :]
    return h * lax.rsqrt(jnp.sum(h * h, axis=(0, 2), keepdims=True) + NORM_EPS)


def two_sided_fft_conv(z, h_fwd, h_bwd):
    L = z.shape[1]
    k = jnp.concatenate([h_fwd, jnp.zeros_like(h_fwd[:1]), jnp.flip(h_bwd[1:], axis=0)], axis=0)
    zf = jnp.fft.rfft(z, n=2 * L, axis=1)
    kf = jnp.fft.rfft(k, n=2 * L, axis=0)
    return jnp.fft.irfft(zf * kf[None], n=2 * L, axis=1)[:, :L]


def hyena_mixer(u, conv_w, conv_b, w1, b1, w2, b2, w3, freq, skip, norm_g):
    L = u.shape[1]
    uc = depthwise_conv(u, conv_w, conv_b).astype(F32)
    v, x1, x2 = jnp.split(uc, 3, axis=-1)
    h = hyena_filters(L, w1, b1, w2, b2, w3, freq)
    skip = skip.astype(F32)
    z = v
    for o, gate in enumerate((x1, x2)):
        z = gate * (two_sided_fft_conv(z, h[:, o, 0], h[:, o, 1]) + skip[o] * z)
    return rms_norm(z, norm_g).astype(u.dtype)


def setup_inputs(seed: int = 0) -> dict:
    key = jax.random.key(seed)
    ks = iter(jax.random.split(key, 40))
    nrm = lambda shape, s: jax.random.normal(next(ks), shape, F32) * s
    gain = lambda shape: 1.0 + nrm(shape, 0.02)
    Lr = DEPTH
    dt0 = jnp.exp(jax.random.uniform(next(ks), (Lr, 2, SSD_HEADS), F32, math.log(1e-3), math.log(1e-1)))
    return {
        "x": nrm((BATCH, SEQ, D_MODEL), 1.0),
        "c": nrm((BATCH, D_MODEL), 1.0),
        "mod_w": nrm((Lr, D_MODEL, 6 * D_MODEL), D_MODEL ** -0.5),
        "mod_b": nrm((Lr, 6 * D_MODEL), 0.02),
        "w_in": nrm((Lr, D_MODEL, P_IN), D_MODEL ** -0.5),
        "ssd_conv_w": nrm((Lr, SSD_CONV, SSD_XBC), SSD_CONV ** -0.5),
        "ssd_conv_b": nrm((Lr, SSD_XBC), 0.02),
        "ssd_dt_bias": dt0 + jnp.log(-jnp.expm1(-dt0)),
        "ssd_a_log": jnp.log(jax.random.uniform(next(ks), (Lr, 2, SSD_HEADS), F32, 1.0, 16.0)),
        "ssd_d": gain((Lr, SSD_HEADS)),
        "ssd_norm_g": gain((Lr, GROUP_W)),
        "na_rpb": nrm((Lr, NA_HEADS, 2 * NA_KH - 1, 2 * NA_KW - 1), 0.02),
        "na_norm_g": gain((Lr, GROUP_W)),
        "ml_i_bias": nrm((Lr, 2, ML_HEADS), 0.1),
        "ml_f_bias": jnp.linspace(3.0, 6.0, ML_HEADS, dtype=F32)[None, None, :] + nrm((Lr, 2, ML_HEADS), 0.1),
        "ml_norm_g": gain((Lr, GROUP_W)),
        "hy_conv_w": nrm((Lr, HY_CONV, HY_IN), HY_CONV ** -0.5),
        "hy_conv_b": nrm((Lr, HY_IN), 0.02),
        "hy_w1": nrm((Lr, HY_EMB, HY_FFN), HY_EMB ** -0.5),
        "hy_b1": nrm((Lr, HY_FFN), 0.1),
        "hy_w2": nrm((Lr, HY_FFN, HY_FFN), HY_FFN ** -0.5),
        "hy_b2": nrm((Lr, HY_FFN), 0.1),
        "hy_w3": nrm((Lr, HY_FFN, HY_ORDER * 2 * HY_CH), HY_FFN ** -0.5),
        "hy_freq": 1.0 + nrm((Lr, 2, HY_FFN), 0.1),
        "hy_skip": nrm((Lr, HY_ORDER, HY_CH), 0.1),
        "hy_norm_g": gain((Lr, GROUP_W)),
        "w_out": nrm((Lr, MIX_W, D_MODEL), MIX_W ** -0.5),
        "ffn_w_gate": nrm((Lr, D_MODEL, FFN_HIDDEN), D_MODEL ** -0.5),
        "ffn_w_up": nrm((Lr, D_MODEL, FFN_HIDDEN), D_MODEL ** -0.5),
        "ffn_w_down": nrm((Lr, FFN_HIDDEN, D_MODEL), FFN_HIDDEN ** -0.5),
        "final_norm_g": gain((D_MODEL,)),
    }


def reference(x, c, mod_w, mod_b, w_in, ssd_conv_w, ssd_conv_b, ssd_dt_bias, ssd_a_log, ssd_d,
              ssd_norm_g, na_rpb, na_norm_g, ml_i_bias, ml_f_bias, ml_norm_g, hy_conv_w, hy_conv_b,
              hy_w1, hy_b1, hy_w2, hy_b2, hy_w3, hy_freq, hy_skip, hy_norm_g, w_out,
              ffn_w_gate, ffn_w_up, ffn_w_down, final_norm_g):
    cond = jax.nn.silu(c)
    for l in range(DEPTH):
        mod = cond @ mod_w[l] + mod_b[l]
        sh1, sc1, g1, sh2, sc2, g2 = [m[:, None, :] for m in jnp.split(mod, 6, axis=-1)]
        h = rms_norm(x) * (1 + sc1) + sh1
        u = h @ w_in[l]
        y_ssd = ssd_mixer(u[..., :OFF_NA], ssd_conv_w[l], ssd_conv_b[l], ssd_dt_bias[l],
                          ssd_a_log[l], ssd_d[l], ssd_norm_g[l])
        y_na = rms_norm(neighborhood_attention(u[..., OFF_NA:OFF_ML], na_rpb[l]), na_norm_g[l])
        y_ml = mlstm_mixer(u[..., OFF_ML:OFF_HY], ml_i_bias[l], ml_f_bias[l], ml_norm_g[l])
        y_hy = hyena_mixer(u[..., OFF_HY:], hy_conv_w[l], hy_conv_b[l], hy_w1[l], hy_b1[l],
                           hy_w2[l], hy_b2[l], hy_w3[l], hy_freq[l], hy_skip[l], hy_norm_g[l])
        y = jnp.concatenate([y_ssd, y_na, y_ml, y_hy], axis=-1) @ w_out[l]
        x = x + g1 * y
        h = rms_norm(x) * (1 + sc2) + sh2
        x = x + g2 * ((jax.nn.silu(h @ ffn_w_gate[l]) * (h @ ffn_w_up[l])) @ ffn_w_down[l])
    return rms_norm(x, final_norm_g)
```

```python
import os
import numpy as np
from contextlib import ExitStack
import concourse.bass as bass
import concourse.mybir as mybir
from concourse.bass_utils import run_bass_kernel_spmd

F32 = mybir.dt.float32
BF16 = mybir.dt.bfloat16
AF = mybir.ActivationFunctionType
ALU = mybir.AluOpType
AX = mybir.AxisListType

COMPUTE = ("pe", "act", "dve", "pool")
QUEUES = ("pe", "act", "dve", "pool", "sp")


class Prog:
    def __init__(self, nc, stack):
        self.nc = nc
        self.stack = stack
        self.ops = []
        self.W = {}
        self.R = {}
        self.chan_sem = {}
        self.chan_cnt = {}
        self.waited = {q: {} for q in QUEUES}
        self.nphase = 0
        self.epoch = {q: 0 for q in COMPUTE}
        self.unsig = {}
        self.last_unsig = {}
        for q in COMPUTE:
            self._sem(("E", q, 0))

    def _sem(self, chan):
        if chan not in self.chan_sem:
            name = "s_" + "_".join(str(c) for c in (chan if isinstance(chan, tuple) else (chan,)))
            self.chan_sem[chan] = self.stack.enter_context(self.nc.semaphore(name))
            self.chan_cnt[chan] = 0
        return self.chan_sem[chan]

    def op(self, q, fn, reads=(), writes=(), dma=None, sig=True):
        if dma is not None:
            chan = ("dma", dma, q)
            self._sem(chan)
            step = 16
        else:
            if self.chan_cnt[("E", q, self.epoch[q])] >= 30000 and not self.unsig.get(q):
                self.epoch[q] += 1
                self._sem(("E", q, self.epoch[q]))
            chan = ("E", q, self.epoch[q])
            step = 1
        deps = {}

        def need(c, v):
            if c[0] == "E" and c[1] == "pe" and q == "pe" and dma is None:
                return
            if c[0] == "dma":
                v = self.chan_cnt[c]
            elif v > self.chan_cnt[c]:
                ent = self.last_unsig[c]
                ent[4] = 1
                self.chan_cnt[c] += 1
                self.unsig[c[1]] = False
            if deps.get(c, 0) < v:
                deps[c] = v

        for k in reads:
            for c, v in self.W.get(k, {}).items():
                need(c, v)
        for k in writes:
            for c, v in self.W.get(k, {}).items():
                if c[0] == "E" and c[1] == q and dma is None:
                    continue
                need(c, v)
            for c, v in self.R.get(k, {}).items():
                if c[0] == "E" and c[1] == q and dma is None:
                    continue
                need(c, v)
        if sig or dma is not None:
            self.chan_cnt[chan] += step
            tok = self.chan_cnt[chan]
            if dma is None:
                self.unsig[q] = False
        else:
            tok = self.chan_cnt[chan] + step
            self.unsig[q] = True
            step = 0
        for k in reads:
            self.R.setdefault(k, {})[chan] = tok
        for k in writes:
            self.W.setdefault(k, {})[chan] = tok
        ent = [q, fn, deps, chan, step]
        if step == 0:
            self.last_unsig[chan] = ent
        self.ops.append(ent)
        return tok

    def barrier(self):
        snap = dict(self.chan_cnt)
        for q in QUEUES:
            self.ops.append([q, None, snap, None, 0])

    def flush(self, name=None):
        nc = self.nc
        ops, self.ops = self.ops, []
        self.nphase += 1
        per_q = {q: [o for o in ops if o[0] == q] for q in QUEUES}
        prog = self

        def emit(q, eng):
            waited = prog.waited[q]
            for (_, fn, deps, chan, step) in per_q[q]:
                for c, v in deps.items():
                    if v <= 0 or waited.get(c, 0) >= v:
                        continue
                    eng.wait_ge(prog.chan_sem[c], v)
                    waited[c] = v
                if fn is None:
                    continue
                ins = fn(eng)
                if step:
                    ins.then_inc(prog.chan_sem[chan], step)

        with nc.Block() as blk:
            @blk.tensor
            def _(e):
                emit("pe", e)

            @blk.scalar
            def _(e):
                emit("act", e)

            @blk.vector
            def _(e):
                emit("dve", e)

            @blk.gpsimd
            def _(e):
                emit("pool", e)

            @blk.sync
            def _(e):
                emit("sp", e)
        self.barrier()

    def finish(self, q="sp"):
        snap = dict(self.chan_cnt)
        self.ops.append([q, None, snap, None, 0])


L = 2048
D = 1024
NT = 16
FH = 2816
NFC = 22
PI = float(np.pi)
OFF_NA, OFF_ML, OFF_HY = 1032, 1800, 2840
_CONST_CACHE = {}


def _consts():
    if _CONST_CACHE:
        return _CONST_CACHE
    import ml_dtypes
    bf = ml_dtypes.bfloat16
    C = _CONST_CACHE
    C["ident_bf"] = np.eye(128, dtype=np.float32).astype(bf)
    C["ident_f"] = np.eye(128, dtype=np.float32)
    C["ones_f"] = np.ones((128, 128), np.float32)
    k = np.arange(128)[:, None]
    s = np.arange(128)[None, :]
    lm = np.stack([(k > s), (k < s), (k <= s), (k >= s)]).astype(np.float32)
    C["lmask"] = np.ascontiguousarray(lm.transpose(1, 0, 2))
    negf = np.where(k > s, -30000.0, 0.0)
    negb = np.where(k < s, -30000.0, 0.0)
    ng = np.stack([np.tile(negf, (1, 4)), np.tile(negb, (1, 4))]).astype(np.float32)
    C["neg"] = np.ascontiguousarray(ng.transpose(1, 0, 2)).astype(bf)
    j = (np.arange(128) % 64)[:, None]
    kc = np.arange(64)[None, :]
    cs = np.clip(j - 8, 0, 48)
    valid = (kc >= cs) & (kc < cs + 16)
    m = np.where(valid, 0.0, -30000.0).astype(np.float32)
    C["na_mask"] = np.tile(m, (1, 8)).astype(bf)
    t = np.linspace(0.0, 1.0, L, dtype=np.float32)[:, None]
    f = np.linspace(1e-4, 15.0, 16, dtype=np.float32)
    ang = (np.float32(2.0 * np.pi) * (np.arange(L, dtype=np.float32) / np.float32(L))[:, None] * f[None, :]).astype(np.float32)
    feats = np.concatenate([t, np.cos(ang), -np.sin(ang)], axis=-1).astype(np.float32)
    C["featsT"] = np.ascontiguousarray(feats.T)
    deltas = np.abs(np.linspace(np.log(1e-2) / 1.5, np.log(1e-2) / 0.3, 256, dtype=np.float32))
    dec = np.exp(-t * deltas[None, :]).astype(np.float32)
    C["decay"] = np.ascontiguousarray(dec.reshape(16, 128, 256).transpose(1, 0, 2))
    tt = np.arange(L, dtype=np.float64)[None, :]
    ff = np.arange(2048, dtype=np.float64)[:, None]
    Fm = np.empty((4096, L), np.float64)
    Fm[:2048] = np.cos(2 * np.pi * ff * tt / 4096.0)
    Fm[2048:] = -np.sin(2 * np.pi * ff * tt / 4096.0)
    Fm[2048] = np.where(np.arange(L) % 2 == 0, 1.0, -1.0)
    Fb = Fm.astype(np.float32).astype(bf)
    C["FTt"] = np.ascontiguousarray(Fb.reshape(32, 128, 16, 128).transpose(0, 3, 2, 1))
    C["Ft"] = np.ascontiguousarray(Fb.reshape(32, 128, 16, 128).transpose(2, 1, 0, 3))
    return C


def _na_bias_gather(rpb):
    j = (np.arange(128) % 64)[:, None]
    kc = np.arange(64)[None, :]
    dc = np.clip(kc - j + 15, 0, 30)
    out = np.empty((2, 8, 128, 4, 8, 64), np.float32)
    for dl in range(8):
        for kr in range(8):
            g = rpb[:, :, dl + kr, :][:, :, dc]
            out[:, dl, :, :, kr, :] = g.transpose(0, 2, 1, 3)
    return out.reshape(2, 8, 128, 4, 512)


class Slots:
    def __init__(self, name, tiles):
        self.name, self.tiles, self.i = name, tiles, 0

    def next(self):
        n = self.i % len(self.tiles)
        self.i += 1
        return self.tiles[n], (self.name, n)


class KB:
    def __init__(self, dbg=None, nseq=2, nlayer=2, mixers=("ssd", "na", "ml", "hy"), ycat_in=False):
        self.dbg = dbg or {}
        self.nseq, self.nlayer, self.mixers, self.ycat_in = nseq, nlayer, mixers, ycat_in
        self.nc = nc = bass.Bass("TRN2", target_bir_lowering=False)
        self.I = {}
        self.dbg_out = {}

        def din(name, shape, dt=F32):
            self.I[name] = nc.dram_tensor(name, list(shape), dt, kind="ExternalInput").ap()

        din("x", [2, L, D]); din("cT", [128, 8, 2]); din("mod_w", [2, D, 6 * D]); din("mod_bT", [2, 128, 48])
        din("w_in", [2, D, 3608]); din("ssd_conv_w", [2, 3, 768]); din("ssd_conv_b", [2, 768])
        din("ssd_cbT", [2, 128, 6]); din("ssd_dt_bias", [2, 8]); din("ssd_a_log", [2, 8]); din("ssd_d", [2, 4]); din("ssd_norm_g", [2, 256])
        din("na_bias", [2, 8, 128, 4, 512]); din("na_norm_g", [2, 256])
        din("ml_i_bias", [2, 8]); din("ml_f_bias", [2, 8]); din("ml_norm_g", [2, 256])
        din("hy_conv_w", [2, 3, 768]); din("hy_conv_b", [2, 768]); din("hy_w1", [2, 33, 64]); din("hy_b1", [2, 64, 1])
        din("hy_w2", [2, 64, 64]); din("hy_b2", [2, 64, 1]); din("hy_w3", [2, 64, 1024]); din("hy_freq", [2, 2, 64, 1])
        din("hy_skip", [2, 2, 256]); din("hy_norm_g", [2, 256]); din("w_out", [2, D, D])
        din("ffn_w_gate", [2, D, FH]); din("ffn_w_up", [2, D, FH]); din("ffn_w_down", [2, FH, D]); din("final_norm_g", [D])
        din("ident_bf", [128, 128], BF16); din("ident_f", [128, 128]); din("ones_f", [128, 128])
        din("lmask", [128, 4, 128]); din("neg", [128, 2, 512], BF16); din("na_mask", [128, 512], BF16)
        din("featsT", [33, L]); din("decay", [128, 16, 256]); din("FTt", [32, 128, 16, 128], BF16); din("Ft", [16, 128, 32, 128], BF16)
        if ycat_in:
            din("ycatT", [2, 2, 128, 8, L], BF16)
        self.out = nc.dram_tensor("out", [2, L, D], F32, kind="ExternalOutput").ap()
        for k, shp in self.dbg.items():
            self.dbg_out[k] = nc.dram_tensor("dbg_" + k, list(shp), F32, kind="ExternalOutput").ap()
        self.wcv = nc.dram_tensor("wcv", [2, 2, 3, D, 768], BF16, kind="Internal").ap()
        self.kfs = nc.dram_tensor("kfs", [2, 2, 16, 128, 3, 256], F32, kind="Internal").ap()

    def sb(self, st, name, shape, dt=F32):
        self._uid = getattr(self, "_uid", 0) + 1
        return st.enter_context(self.nc.sbuf_tensor(f"{name}_{self._uid}", list(shape), dt))

    def ps(self, st, name, shape, dt=F32):
        self._uid = getattr(self, "_uid", 0) + 1
        return st.enter_context(self.nc.psum_tensor(f"{name}_{self._uid}", list(shape), dt))

    def dump(self, name, src_ap, key, dst=None):
        if name not in self.dbg_out:
            return
        d = self.dbg_out[name] if dst is None else dst
        self.P.op("pool", lambda e: e.dma_start(out=d, in_=src_ap), reads=[key], dma="dbg")

    def build(self):
        nc, I = self.nc, self.I
        with ExitStack() as st:
            self.P = P = Prog(nc, st)
            self.ident_bf = self.sb(st, "ident_bf", [128, 128], BF16)
            self.ident_f = self.sb(st, "ident_f", [128, 128])
            self.ones_f = self.sb(st, "ones_f", [128, 128])
            self.modT = self.sb(st, "modT", [128, 2, 48, 2])
            self.condT = self.sb(st, "condT", [128, 8, 2], BF16)
            for nm in ("ident_bf", "ident_f", "ones_f"):
                t = getattr(self, nm)
                P.op("sp", lambda e, t=t, nm=nm: e.dma_start(out=t[:], in_=I[nm]), writes=[nm], dma="const")
            self.phase0()
            with ExitStack() as sx:
                self.x = self.sb(sx, "xres", [128, NT, D])
                for b in range(self.nseq):
                    for i in range(NT):
                        P.op("sp", lambda e, b=b, i=i: e.dma_start(out=self.x[:, i, :], in_=I["x"][b, i * 128:(i + 1) * 128, :]),
                             writes=[("x", i)], dma="xload")
                    for l in range(self.nlayer):
                        self.layer(b, l)
                    self.final(b)
            P.finish()
            P.flush()
        return nc

    def phase0(self):
        nc, I, P = self.nc, self.I, self.P
        with ExitStack() as st:
            cTf = self.sb(st, "cTf", [128, 8, 2])
            mbT = self.sb(st, "mbT", [128, 2, 48])
            wsl = Slots("mw", [self.sb(st, f"mw{i}", [128, 8, 512], BF16) for i in range(2)])
            mps = self.ps(st, "mps", [128, 96])
            P.op("sp", lambda e: e.dma_start(out=cTf[:], in_=I["cT"]), writes=["cTf"], dma="const")
            for l in range(2):
                P.op("sp", lambda e, l=l: e.dma_start(out=mbT[:, l, :], in_=I["mod_bT"][l]), writes=["mbT"], dma="const")
            P.op("act", lambda e: e.activation(out=self.condT[:], in_=cTf[:], func=AF.Silu), reads=["cTf"], writes=["condT"])
            for l in range(2):
                wv = I["mod_w"][l].rearrange("(j p) f -> p j f", p=128)
                for blk in range(12):
                    w, wk = wsl.next()
                    P.op("pool", lambda e, w=w, blk=blk, wv=wv: e.dma_start(out=w[:], in_=wv[:, :, blk * 512:(blk + 1) * 512]),
                         writes=[wk], dma=wk)
                    for fc in range(4):
                        col = (blk * 4 + fc) * 2
                        for j in range(8):
                            P.op("pe", lambda e, w=w, fc=fc, j=j, col=col: e.matmul(
                                mps[:, col:col + 2], lhsT=w[:, j, fc * 128:(fc + 1) * 128], rhs=self.condT[:, j, :],
                                start=(j == 0), stop=(j == 7)), reads=[wk, "condT"], writes=["mps"], sig=(j == 7))
                mv = mps[:].rearrange("p (c b) -> p c b", b=2)
                P.op("dve", lambda e, l=l, mv=mv: e.tensor_tensor(
                    out=self.modT[:, l, :, :], in0=mv, in1=mbT[:, l, :].unsqueeze(2).broadcast_to([128, 48, 2]), op=ALU.add),
                    reads=["mps", "mbT"], writes=["modT"])
                for c0 in (8, 32):
                    P.op("dve", lambda e, l=l, c0=c0: e.tensor_scalar_add(
                        out=self.modT[:, l, c0:c0 + 8, :], in0=self.modT[:, l, c0:c0 + 8, :], scalar1=1.0),
                        reads=["modT"], writes=["modT"])
            if "modT" in self.dbg_out:
                self.dump("modT", self.modT[:], "modT")
            P.flush()
        if "hy" in self.mixers or "ssd" in self.mixers:
            self.phase0_wcv()
        if "hy" in self.mixers:
            self.phase0_hyena()

    def phase0_wcv(self):
        pass

    def phase0_hyena(self):
        pass

    def norm_to_hT(self, st, b, l, which):
        nc, P = self.nc, self.P
        sh_c, sc_c = (0, 8) if which == 0 else (24, 32)
        ss = self.sb(st, "n_ss", [128, NT])
        rstd = self.sb(st, "n_rstd", [128, NT])
        junk = self.sb(st, "n_junk", [128, D], BF16)
        xn = [self.sb(st, f"n_xn{i}", [128, 4, D], BF16) for i in range(2)]
        tp = [self.ps(st, f"n_tp{i}", [128, 512], BF16) for i in range(2)]
        hT = self.hT
        P.op("dve", lambda e: e.memset(hT[:, :, 0:1], 0.0), writes=["hT"])
        P.op("dve", lambda e: e.memset(hT[:, :, L + 1:L + 2], 0.0), writes=["hT"])
        n = 0
        for tb in range(4):
            xb = xn[tb % 2]
            xk = ("n_xn", tb % 2)
            for ii in range(4):
                i = tb * 4 + ii
                P.op("act", lambda e, i=i: e.activation(out=junk[:], in_=self.x[:, i, :], func=AF.Square,
                                                         accum_out=ss[:, i:i + 1]), reads=[("x", i)], writes=["n_junk", ("n_ss", i)])
                P.op("act", lambda e, i=i: e.activation(out=rstd[:, i:i + 1], in_=ss[:, i:i + 1], func=AF.Sqrt, bias=1e-6, scale=1.0 / D),
                     reads=[("n_ss", i)], writes=[("n_rs", i)])
                P.op("dve", lambda e, i=i: e.reciprocal(out=rstd[:, i:i + 1], in_=rstd[:, i:i + 1]), reads=[("n_rs", i)], writes=[("n_rs", i)])
                P.op("dve", lambda e, i=i, ii=ii, xb=xb: e.tensor_scalar(out=xb[:, ii, :], in0=self.x[:, i, :],
                                                                        scalar1=rstd[:, i:i + 1], scalar2=None, op0=ALU.mult),
                     reads=[("x", i), ("n_rs", i)], writes=[xk])
            for j in range(8):
                t, tk = tp[n % 2], ("n_tp", n % 2)
                n += 1
                for ii in range(4):
                    P.op("pe", lambda e, t=t, ii=ii, j=j, xb=xb: e.transpose(
                        t[:, ii * 128:(ii + 1) * 128], xb[:, ii, j * 128:(j + 1) * 128], self.ident_bf[:]),
                        reads=[xk, "ident_bf"], writes=[tk], sig=(ii == 3))
                dst = hT[:, j, 1 + tb * 512:1 + (tb + 1) * 512]
                if j % 2 == 0:
                    P.op("act", lambda e, t=t, dst=dst, j=j: e.activation(
                        out=dst, in_=t[:], func=AF.Identity, bias=self.modT[:, l, sh_c + j, b:b + 1],
                        scale=self.modT[:, l, sc_c + j, b:b + 1]), reads=[tk, "modT"], writes=[("hT", tb)])
                else:
                    P.op("dve", lambda e, t=t, dst=dst, j=j: e.tensor_scalar(
                        out=dst, in0=t[:], scalar1=self.modT[:, l, sc_c + j, b:b + 1],
                        scalar2=self.modT[:, l, sh_c + j, b:b + 1], op0=ALU.mult, op1=ALU.add),
                        reads=[tk, "modT"], writes=[("hT", tb)])

    HT_ALL = ["hT"] + [("hT", tb) for tb in range(4)]

    def layer(self, b, l):
        nc, I, P = self.nc, self.I, self.P
        with ExitStack() as sl:
            self.G = [self.sb(sl, f"Gk{g}", [128, D]) for g in range(2)]
            with ExitStack() as st:
                self._grow_into(st, b, l)
                P.flush()
            with ExitStack() as sm:
                self.hT = self.sb(sm, "hT", [128, 8, L + 2], BF16)
                with ExitStack() as st:
                    self.norm_to_hT(st, b, l, 0)
                    if l == 0 and b == 0 and "hT0" in self.dbg_out:
                        pass
                    P.flush()
                for mi, m in enumerate(("ssd", "na", "ml", "hy")):
                    with ExitStack() as sy:
                        self.yT = self.sb(sy, "yT", [128, 2, L], BF16)
                        if m in self.mixers:
                            getattr(self, "mix_" + m)(b, l)
                        elif self.ycat_in:
                            P.op("sp", lambda e, mi=mi: e.dma_start(out=self.yT[:], in_=I["ycatT"][b, l, :, 2 * mi:2 * mi + 2, :]),
                                 writes=["yT"], dma="yT")
                        else:
                            P.op("dve", lambda e: e.memset(self.yT[:], 0.0), writes=["yT"])
                        with ExitStack() as st:
                            self.wout_part(st, b, l, mi)
                            P.flush()
            with ExitStack() as sm:
                self.hT = self.sb(sm, "hT", [128, 8, L + 2], BF16)
                with ExitStack() as st:
                    self.norm_to_hT(st, b, l, 1)
                    P.flush()
                self.ffn(b, l)

    def _grow_into(self, st, b, l):
        P = self.P
        dg = [self.sb(st, f"g_dg{i}", [128, 128]) for i in range(2)]
        gps = self.ps(st, "g_ps", [128, D])
        n = 0
        for g in range(2):
            c0 = 16 if g == 0 else 40
            for j in range(8):
                d, dk = dg[n % 2], ("g_dg", n % 2)
                n += 1
                P.op("dve", lambda e, d=d, j=j, c0=c0: e.tensor_scalar(
                    out=d[:], in0=self.ident_f[:], scalar1=self.modT[:, l, c0 + j, b:b + 1], scalar2=None, op0=ALU.mult),
                    reads=["ident_f", "modT"], writes=[dk])
                P.op("pe", lambda e, d=d, j=j: e.matmul(gps[:, j * 128:(j + 1) * 128], lhsT=self.ones_f[:], rhs=d[:],
                                                         start=True, stop=True), reads=["ones_f", dk], writes=["g_ps"], sig=(j == 7))
            P.op("act", lambda e, g=g: e.activation(out=self.G[g][:], in_=gps[:], func=AF.Copy), reads=["g_ps"], writes=[("G", g)])

    def wout_part(self, st, b, l, mi):
        I, P = self.I, self.P
        wo = self.sb(st, "wo", [128, 2, D], BF16)
        tmp = [self.sb(st, f"wo_t{i}", [128, 512]) for i in range(2)]
        ops_ = [self.ps(st, f"wo_ps{i}", [128, 512]) for i in range(2)]
        wv = I["w_out"][l, 256 * mi:256 * (mi + 1), :].rearrange("(j p) f -> p j f", p=128)
        P.op("pool", lambda e: e.dma_start(out=wo[:], in_=wv), writes=["wo"], dma="wo")
        n = 0
        for i in range(NT):
            for dh in range(2):
                pt, pk = ops_[n % 2], ("wo_ps", n % 2)
                tt, tk = tmp[n % 2], ("wo_t", n % 2)
                n += 1
                for j in range(2):
                    P.op("pe", lambda e, pt=pt, i=i, j=j, dh=dh: e.matmul(
                        pt[:], lhsT=self.yT[:, j, i * 128:(i + 1) * 128], rhs=wo[:, j, dh * 512:(dh + 1) * 512],
                        start=(j == 0), stop=(j == 1)), reads=["yT", "wo"], writes=[pk], sig=(j == 1))
                P.op("dve", lambda e, pt=pt, tt=tt, dh=dh: e.tensor_tensor(
                    out=tt[:], in0=pt[:], in1=self.G[0][:, dh * 512:(dh + 1) * 512], op=ALU.mult),
                    reads=[pk, ("G", 0)], writes=[tk])
                xs = self.x[:, i, dh * 512:(dh + 1) * 512]
                P.op("pool", lambda e, tt=tt, xs=xs: e.tensor_tensor(out=xs, in0=xs, in1=tt[:], op=ALU.add),
                     reads=[tk, ("x", i)], writes=[("x", i)])

    def ffn(self, b, l):
        I, P = self.I, self.P
        wg_v = I["ffn_w_gate"][l].rearrange("(j p) f -> p j f", p=128)
        wu_v = I["ffn_w_up"][l].rearrange("(j p) f -> p j f", p=128)
        wd_v = I["ffn_w_down"][l].rearrange("(c p) f -> p c f", p=128)
        for half in range(2):
            with ExitStack() as sa:
                act = self.sb(sa, "f_act", [128, NFC, 1024], BF16)
                with ExitStack() as st:
                    wsl = Slots("f_w", [self.sb(st, f"f_w{i}", [128, 2, 8, 128], BF16) for i in range(3)])
                    sg = [self.sb(st, f"f_sg{i}", [128, 512]) for i in range(2)]
                    pg = [self.ps(st, f"f_pg{i}", [128, 512]) for i in range(2)]
                    pu = [self.ps(st, f"f_pu{i}", [128, 512]) for i in range(2)]
                    n = 0
                    for fc in range(NFC):
                        w, wk = wsl.next()
                        P.op("pool", lambda e, w=w, fc=fc: e.dma_start(out=w[:, 0, :, :], in_=wg_v[:, :, fc * 128:(fc + 1) * 128]),
                             writes=[wk], dma=wk)
                        P.op("pool", lambda e, w=w, fc=fc: e.dma_start(out=w[:, 1, :, :], in_=wu_v[:, :, fc * 128:(fc + 1) * 128]),
                             writes=[wk], dma=wk)
                        for q in range(2):
                            t0 = 1 + half * 1024 + q * 512
                            g_, gk = pg[n % 2], ("f_pg", n % 2)
                            u_, uk = pu[n % 2], ("f_pu", n % 2)
                            s_, sk = sg[n % 2], ("f_sg", n % 2)
                            n += 1
                            for gi, (pt, pk) in enumerate(((g_, gk), (u_, uk))):
                                for j in range(8):
                                    P.op("pe", lambda e, pt=pt, w=w, gi=gi, j=j, t0=t0: e.matmul(
                                        pt[:], lhsT=w[:, gi, j, :], rhs=self.hT[:, j, t0:t0 + 512], start=(j == 0), stop=(j == 7)),
                                        reads=[wk] + self.HT_ALL, writes=[pk], sig=(j == 7))
                            P.op("act", lambda e, g_=g_, s_=s_: e.activation(out=s_[:], in_=g_[:], func=AF.Silu),
                                 reads=[gk], writes=[sk])
                            P.op("dve", lambda e, u_=u_, s_=s_, fc=fc, q=q: e.tensor_tensor(
                                out=act[:, fc, q * 512:(q + 1) * 512], in0=u_[:], in1=s_[:], op=ALU.mult),
                                reads=[uk, sk], writes=["f_act"])
                    P.flush()
                with ExitStack() as st:
                    wsl = Slots("f_wd", [self.sb(st, f"f_wd{i}", [128, 512], BF16) for i in range(3)])
                    acc = [self.ps(st, f"f_acc{i}", [128, 512]) for i in range(8)]
                    tmp = [self.sb(st, f"f_t{i}", [128, 512]) for i in range(2)]
                    n = 0
                    for dh in range(2):
                        for fc in range(NFC):
                            w, wk = wsl.next()
                            P.op("pool", lambda e, w=w, fc=fc, dh=dh: e.dma_start(out=w[:], in_=wd_v[:, fc, dh * 512:(dh + 1) * 512]),
                                 writes=[wk], dma=wk)
                            for tt in range(8):
                                P.op("pe", lambda e, w=w, fc=fc, tt=tt: e.matmul(
                                    acc[tt][:], lhsT=act[:, fc, tt * 128:(tt + 1) * 128], rhs=w[:], start=(fc == 0), stop=(fc == NFC - 1)),
                                    reads=[wk, "f_act"], writes=[("f_acc", tt)], sig=(tt == 7 or fc == NFC - 1))
                        for tt in range(8):
                            i = half * 8 + tt
                            t_, tk = tmp[n % 2], ("f_t", n % 2)
                            n += 1
                            P.op("dve", lambda e, t_=t_, tt=tt, dh=dh: e.tensor_tensor(
                                out=t_[:], in0=acc[tt][:], in1=self.G[1][:, dh * 512:(dh + 1) * 512], op=ALU.mult),
                                reads=[("f_acc", tt), ("G", 1)], writes=[tk])
                            xs = self.x[:, i, dh * 512:(dh + 1) * 512]
                            P.op("pool", lambda e, t_=t_, xs=xs: e.tensor_tensor(out=xs, in0=xs, in1=t_[:], op=ALU.add),
                                 reads=[tk, ("x", i)], writes=[("x", i)])
                    P.flush()

    def final(self, b):
        I, P = self.I, self.P
        with ExitStack() as st:
            gb = self.sb(st, "fn_g", [128, D])
            ss = self.sb(st, "fn_ss", [128, NT])
            rs = self.sb(st, "fn_rs", [128, NT])
            junk = self.sb(st, "fn_junk", [128, D], BF16)
            ob = [self.sb(st, f"fn_o{i}", [128, D]) for i in range(2)]
            P.op("sp", lambda e: e.dma_start(out=gb[:], in_=I["final_norm_g"].partition_broadcast(128)), writes=["fn_g"], dma="fn_g")
            for i in range(NT):
                o, ok = ob[i % 2], ("fn_o", i % 2)
                P.op("act", lambda e, i=i: e.activation(out=junk[:], in_=self.x[:, i, :], func=AF.Square, accum_out=ss[:, i:i + 1]),
                     reads=[("x", i)], writes=["fn_junk", ("fn_ss", i)])
                P.op("act", lambda e, i=i: e.activation(out=rs[:, i:i + 1], in_=ss[:, i:i + 1], func=AF.Sqrt, bias=1e-6, scale=1.0 / D),
                     reads=[("fn_ss", i)], writes=[("fn_rs", i)])
                P.op("dve", lambda e, i=i: e.reciprocal(out=rs[:, i:i + 1], in_=rs[:, i:i + 1]), reads=[("fn_rs", i)], writes=[("fn_rs", i)])
                P.op("dve", lambda e, i=i, o=o: e.scalar_tensor_tensor(out=o[:], in0=self.x[:, i, :], scalar=rs[:, i:i + 1],
                                                                       in1=gb[:], op0=ALU.mult, op1=ALU.mult),
                     reads=[("x", i), ("fn_rs", i), "fn_g"], writes=[ok])
                P.op("sp", lambda e, i=i, o=o: e.dma_start(out=self.out[b, i * 128:(i + 1) * 128, :], in_=o[:]),
                     reads=[ok], dma=("out", i % 2))
            P.flush()


def make_in_maps(inp, ncores=8):
    C = _consts()
    g = lambda k: np.ascontiguousarray(np.asarray(inp[k], dtype=np.float32))
    shared = {
        "mod_w": g("mod_w"), "mod_bT": np.ascontiguousarray(g("mod_b").reshape(2, 48, 128).transpose(0, 2, 1)),
        "w_in": g("w_in"), "ssd_conv_w": g("ssd_conv_w"), "ssd_conv_b": g("ssd_conv_b"),
        "ssd_cbT": np.ascontiguousarray(g("ssd_conv_b").reshape(2, 6, 128).transpose(0, 2, 1)), "ssd_dt_bias": g("ssd_dt_bias").reshape(2, 8), "ssd_a_log": g("ssd_a_log").reshape(2, 8), "ssd_d": g("ssd_d"),
        "ssd_norm_g": g("ssd_norm_g"), "na_bias": _na_bias_gather(g("na_rpb")), "na_norm_g": g("na_norm_g"),
        "ml_i_bias": g("ml_i_bias").reshape(2, 8), "ml_f_bias": g("ml_f_bias").reshape(2, 8), "ml_norm_g": g("ml_norm_g"),
        "hy_conv_w": g("hy_conv_w"), "hy_conv_b": g("hy_conv_b"), "hy_w1": g("hy_w1"), "hy_b1": g("hy_b1").reshape(2, 64, 1),
        "hy_w2": g("hy_w2"), "hy_b2": g("hy_b2").reshape(2, 64, 1), "hy_w3": g("hy_w3"), "hy_freq": g("hy_freq").reshape(2, 2, 64, 1),
        "hy_skip": g("hy_skip"), "hy_norm_g": g("hy_norm_g"), "w_out": g("w_out"),
        "ffn_w_gate": g("ffn_w_gate"), "ffn_w_up": g("ffn_w_up"), "ffn_w_down": g("ffn_w_down"), "final_norm_g": g("final_norm_g"),
    }
    for k in ("ident_bf", "ident_f", "ones_f", "lmask", "neg", "na_mask", "featsT", "decay", "FTt", "Ft"):
        shared[k] = C[k]
    x, c = g("x"), g("c")
    maps = []
    for i in range(ncores):
        m = dict(shared)
        m["x"] = np.ascontiguousarray(x[2 * i:2 * i + 2])
        m["cT"] = np.ascontiguousarray(c[2 * i:2 * i + 2].reshape(2, 8, 128).transpose(2, 1, 0))
        maps.append(m)
    return maps


def _phase0_wcv(self):
    I, P = self.I, self.P
    with ExitStack() as st:
        cw = self.sb(st, "cv_cw", [128, 3, 768])
        wf = Slots("cv_wf", [self.sb(st, f"cv_wf{i}", [128, 768]) for i in range(2)])
        ob = Slots("cv_ob", [self.sb(st, f"cv_ob{i}", [128, 768], BF16) for i in range(3)])
        n = 0
        for l in range(2):
            for grp, (cwn, c0) in enumerate((("ssd_conv_w", 256), ("hy_conv_w", OFF_HY))):
                P.op("sp", lambda e, l=l, cwn=cwn: e.dma_start(
                    out=cw[:].rearrange("p a b -> p (a b)"), in_=I[cwn][l].rearrange("a b -> (a b)").partition_broadcast(128)),
                    writes=["cv_cw"], dma="cv_cw")
                for j in range(8):
                    w, wk = wf.next()
                    P.op("sp", lambda e, w=w, l=l, j=j, c0=c0: e.dma_start(out=w[:], in_=I["w_in"][l, j * 128:(j + 1) * 128, c0:c0 + 768]),
                         writes=[wk], dma=wk)
                    for tap in range(3):
                        o, ok = ob.next()
                        q = "dve" if n % 2 == 0 else "pool"
                        n += 1
                        P.op(q, lambda e, o=o, w=w, tap=tap: e.tensor_tensor(out=o[:], in0=w[:], in1=cw[:, tap, :], op=ALU.mult),
                             reads=[wk, "cv_cw"], writes=[ok])
                        P.op("sp", lambda e, o=o, l=l, grp=grp, tap=tap, j=j: e.dma_start(
                            out=self.wcv[l, grp, tap, j * 128:(j + 1) * 128, :], in_=o[:]), reads=[ok], writes=["wcv"], dma=("wcvst", ok[1]))
        P.flush()


def _proj_tm(self, st, l, specs, evac, tiles=range(NT), tok_off=0, tagp="ptm"):
    I, P = self.I, self.P
    mt = 3 if any(sp[0] is not None for sp in specs) else 1
    wsl = Slots(tagp + "w", [self.sb(st, f"{tagp}w{i}", [128, mt, 8, 256], BF16) for i in range(2)])
    pps = [self.ps(st, f"{tagp}p{i}", [128, 256]) for i in range(2)]
    cnt = 0
    for (src, c0, n, tag, bias_ap) in specs:
        w, wk = wsl.next()
        taps = 1 if src is None else 3
        for tap in range(taps):
            if src is None:
                v = I["w_in"][l].rearrange("(j p) c -> p j c", p=128)[:, :, c0:c0 + n]
                P.op("pool", lambda e, w=w, v=v, n=n: e.dma_start(out=w[:, 0, :, :n], in_=v), writes=[wk], dma=wk)
            else:
                v = self.wcv[l, src, tap].rearrange("(j p) c -> p j c", p=128)[:, :, c0:c0 + n]
                P.op("sp", lambda e, w=w, v=v, n=n, tap=tap: e.dma_start(out=w[:, tap, :, :n], in_=v), reads=["wcv"], writes=[wk], dma=wk)
        for i in tiles:
            pt, pk = pps[cnt % 2], (tagp + "p", cnt % 2)
            cnt += 1
            k, tot = 0, taps * 8
            for tap in range(taps):
                sh = 0 if src is None else tap - 1
                t0 = 1 + tok_off + i * 128 + sh
                for j in range(8):
                    last = (k == tot - 1) and bias_ap is None
                    P.op("pe", lambda e, pt=pt, w=w, tap=tap, j=j, t0=t0, n=n, k=k, last=last: e.matmul(
                        pt[:, :n], lhsT=self.hT[:, j, t0:t0 + 128], rhs=w[:, tap, j, :n], start=(k == 0), stop=last),
                        reads=[wk] + self.HT_ALL, writes=[pk], sig=(k == tot - 1))
                    k += 1
            if bias_ap is not None:
                P.op("pe", lambda e, pt=pt, n=n, bias_ap=bias_ap: e.matmul(pt[:, :n], lhsT=self.ones_f[0:1, :], rhs=bias_ap,
                                                                           start=False, stop=True), reads=["ones_f", "cbrow"], writes=[pk])
            evac(tag, i, pt[:, :n], pk)


def _proj_fm(self, st, l, specs, evac, tagp="pfm"):
    I, P = self.I, self.P
    mt = 3 if any(sp[0] is not None for sp in specs) else 1
    wsl = Slots(tagp + "w", [self.sb(st, f"{tagp}w{i}", [128, mt, 8, 128], BF16) for i in range(2)])
    pps = [self.ps(st, f"{tagp}p{i}", [128, 512]) for i in range(2)]
    cnt = 0
    for (src, c0, n, tag) in specs:
        w, wk = wsl.next()
        taps = 1 if src is None else 3
        for tap in range(taps):
            if src is None:
                v = I["w_in"][l].rearrange("(j p) c -> p j c", p=128)[:, :, c0:c0 + n]
                P.op("pool", lambda e, w=w, v=v, n=n: e.dma_start(out=w[:, 0, :, :n], in_=v), writes=[wk], dma=wk)
            else:
                v = self.wcv[l, src, tap].rearrange("(j p) c -> p j c", p=128)[:, :, c0:c0 + n]
                P.op("sp", lambda e, w=w, v=v, n=n, tap=tap: e.dma_start(out=w[:, tap, :, :n], in_=v), reads=["wcv"], writes=[wk], dma=wk)
        for tb in range(4):
            pt, pk = pps[cnt % 2], (tagp + "p", cnt % 2)
            cnt += 1
            k, tot = 0, taps * 8
            for tap in range(taps):
                sh = 0 if src is None else tap - 1
                t0 = 1 + tb * 512 + sh
                for j in range(8):
                    P.op("pe", lambda e, pt=pt, w=w, tap=tap, j=j, t0=t0, n=n, k=k, tot=tot: e.matmul(
                        pt[:n, :], lhsT=w[:, tap, j, :n], rhs=self.hT[:, j, t0:t0 + 512], start=(k == 0), stop=(k == tot - 1)),
                        reads=[wk] + self.HT_ALL, writes=[pk], sig=(k == tot - 1))
                    k += 1
            evac(tag, tb, pt[:n, :], pk)


def _epilogue(self, st, tagp):
    P = self.P
    junk = self.sb(st, tagp + "ej", [128, 256])
    ss = self.sb(st, tagp + "ess", [128, NT])
    rs = self.sb(st, tagp + "ers", [128, NT])
    yb = [self.sb(st, f"{tagp}eyb{i}", [128, 256], BF16) for i in range(2)]
    tp = [self.ps(st, f"{tagp}etp{i}", [128, 256], BF16) for i in range(2)]

    def fn(i, src, skey, grow, gkey):
        y, yk = yb[i % 2], (tagp + "eyb", i % 2)
        t, tk = tp[i % 2], (tagp + "etp", i % 2)
        P.op("act", lambda e: e.activation(out=junk[:], in_=src, func=AF.Square, accum_out=ss[:, i:i + 1]),
             reads=[skey], writes=[tagp + "ej", (tagp + "ess", i)])
        P.op("act", lambda e: e.activation(out=rs[:, i:i + 1], in_=ss[:, i:i + 1], func=AF.Sqrt, bias=1e-6, scale=1.0 / 256),
             reads=[(tagp + "ess", i)], writes=[(tagp + "ers", i)])
        P.op("dve", lambda e: e.reciprocal(out=rs[:, i:i + 1], in_=rs[:, i:i + 1]), reads=[(tagp + "ers", i)], writes=[(tagp + "ers", i)])
        P.op("dve", lambda e: e.scalar_tensor_tensor(out=y[:], in0=src, scalar=rs[:, i:i + 1], in1=grow, op0=ALU.mult, op1=ALU.mult),
             reads=[skey, (tagp + "ers", i), gkey], writes=[yk])
        self.to_yT(i, y, yk, t, tk)
    return fn


def _to_yT(self, i, y, yk, t, tk):
    P = self.P
    for jj in range(2):
        P.op("pe", lambda e, jj=jj: e.transpose(t[:, jj * 128:(jj + 1) * 128], y[:, jj * 128:(jj + 1) * 128], self.ident_bf[:]),
             reads=[yk, "ident_bf"], writes=[tk], sig=(jj == 1))
    P.op("act", lambda e: e.activation(out=self.yT[:, :, i * 128:(i + 1) * 128], in_=t[:].rearrange("p (a b) -> p a b", a=2), func=AF.Copy),
         reads=[tk], writes=["yT"])


def _sweep(self, st, tagp, nq, pv, sgroups, KT, QT, Ktm, strow, a_ap, build_vd, accum, ufull=False):
    P = self.P
    H = 4
    lm = self.sb(st, tagp + "lm", [128, 4, 128])
    neg = self.sb(st, tagp + "neg", [128, 2, 512], BF16)
    P.op("sp", lambda e: e.dma_start(out=lm[:], in_=self.I["lmask"]), writes=[tagp + "lm"], dma=tagp + "c")
    P.op("sp", lambda e: e.dma_start(out=neg[:], in_=self.I["neg"]), writes=[tagp + "neg"], dma=tagp + "c")
    pvp = ((pv + 7) // 8) * 8
    St = self.sb(st, tagp + "St", [128, 2, H, pvp])[:, :, :, 0:pv]
    Stb = self.sb(st, tagp + "Stb", [128, 2, H, pvp], BF16)[:, :, :, 0:pv]
    P.op("dve", lambda e: e.memset(St[:], 0.0), writes=[tagp + "St0", tagp + "St1"])
    P.op("dve", lambda e: e.memset(Stb[:], 0.0), writes=[tagp + "Stb0", tagp + "Stb1"])
    hv = lambda t: t[:, 0:H * pvp].rearrange("p (h c) -> p h c", h=H)[:, :, 0:pv]
    gps = self.ps(st, tagp + "gps", [128, 512])
    Yps = hv(self.ps(st, tagp + "Yps", [128, 512]))
    Ups = hv(self.ps(st, tagp + "Ups", [128, 512]))
    U2ps = hv(self.ps(st, tagp + "U2ps", [128, 512]))
    Dps_ = [self.ps(st, f"{tagp}Dps{d}", [128, 512]) for d in range(2)]
    Sps_ = [self.ps(st, f"{tagp}Sps{d}", [128, 512]) for d in range(2)]
    egs_ = [self.sb(st, f"{tagp}egs{d}", [128, 16]) for d in range(2)]
    Lm_ = [self.sb(st, f"{tagp}Lm{d}", [128, 4, 128]) for d in range(2)]
    ET_ = [self.sb(st, f"{tagp}ET{d}", [128, 512]) for d in range(2)]
    PT_ = [self.sb(st, f"{tagp}PT{d}", [128, 512], BF16) for d in range(2)]
    Vd_ = [self.sb(st, f"{tagp}Vd{d}", [128, H, pvp], BF16)[:, :, 0:pv] for d in range(2)]
    Vp_ = [self.sb(st, f"{tagp}Vp{d}", [128, H, pvp], BF16)[:, :, 0:pv] for d in range(2)]
    T1_ = [self.sb(st, f"{tagp}T1{d}", [128, H, pvp])[:, :, 0:pv] for d in range(2)]
    ng = len(sgroups)
    rep = H // ng
    for step in range(NT):
        for d in range(2):
            k = lambda nm, d=d: tagp + nm + str(d)
            kg = lambda nm: tagp + nm
            Dps, Sps, egs, Lm, ET, PT, Vd, Vp, T1 = Dps_[d], Sps_[d], egs_[d], Lm_[d], ET_[d], PT_[d], Vd_[d], Vp_[d], T1_[d]
            c = step if d == 0 else NT - 1 - step
            a = a_ap(c, d)
            mL, mR = lm[:, d, :], lm[:, 2 + d, :]
            P.op("pe", lambda e, mR=mR, a=a: e.matmul(gps[:, 0:4], lhsT=mR, rhs=a, start=True, stop=True), reads=[kg("lm"), "avals"], writes=[kg("gps")], sig=False)
            P.op("pe", lambda e, mL=mL, a=a: e.matmul(gps[:, 4:8], lhsT=mL, rhs=a, start=True, stop=True), reads=[kg("lm"), "avals"], writes=[kg("gps")], sig=False)
            P.op("pe", lambda e, a=a: e.matmul(gps[:, 8:12], lhsT=self.ones_f[:], rhs=a, start=True, stop=True), reads=["ones_f", "avals"], writes=[kg("gps")])
            P.op("act", lambda e, egs=egs: e.activation(out=egs[:, 0:12], in_=gps[:, 0:12], func=AF.Exp), reads=[kg("gps")], writes=[k("egs")])
            P.op("dve", lambda e, mL=mL, a=a, Lm=Lm: e.tensor_tensor(out=Lm[:], in0=mL.unsqueeze(1).broadcast_to([128, 4, 128]),
                                                                     in1=a.unsqueeze(2).broadcast_to([128, 4, 128]), op=ALU.mult),
                 reads=[kg("lm"), "avals"], writes=[k("Lm")])
            P.op("pe", lambda e, d=d, Dps=Dps: e.matmul(Dps[:], lhsT=self.ident_bf[:], rhs=neg[:, d, :], start=True, stop=False),
                 reads=["ident_bf", kg("neg")], writes=[k("Dps")], sig=False)
            for h in range(H):
                P.op("pe", lambda e, h=h, mR=mR, Dps=Dps, Lm=Lm: e.matmul(Dps[:, h * 128:(h + 1) * 128], lhsT=Lm[:, h, :], rhs=mR, start=False, stop=(h == H - 1)),
                     reads=[k("Lm"), kg("lm")], writes=[k("Dps")], sig=(h == H - 1))
            P.op("act", lambda e, ET=ET, Dps=Dps: e.activation(out=ET[:], in_=Dps[:], func=AF.Exp), reads=[k("Dps")], writes=[k("ET")])
            for gi, (g, heads) in enumerate(sgroups):
                P.op("pe", lambda e, gi=gi, g=g, c=c, Sps=Sps: e.matmul(Sps[:, gi * 128:(gi + 1) * 128], lhsT=KT(g, c), rhs=QT(g, c), start=True, stop=True),
                     reads=["qkT"], writes=[k("Sps")], sig=(gi == ng - 1))
            if rep == 1:
                P.op("dve", lambda e, PT=PT, ET=ET, Sps=Sps: e.tensor_tensor(out=PT[:], in0=ET[:], in1=Sps[:], op=ALU.mult), reads=[k("ET"), k("Sps")], writes=[k("PT")])
            else:
                P.op("dve", lambda e, PT=PT, ET=ET, Sps=Sps: e.tensor_tensor(
                    out=PT[:].rearrange("p (g r t) -> p g r t", g=ng, r=rep), in0=ET[:].rearrange("p (g r t) -> p g r t", g=ng, r=rep),
                    in1=Sps[:, 0:ng * 128].rearrange("p (g t) -> p g t", g=ng).unsqueeze(2).broadcast_to([128, ng, rep, 128]), op=ALU.mult),
                    reads=[k("ET"), k("Sps")], writes=[k("PT")])
            build_vd(c, d, Vd, k("Vd"))
            P.op("pool", lambda e, Vp=Vp, Vd=Vd, egs=egs: e.tensor_tensor(out=Vp[:], in0=Vd[:], in1=egs[:, 4:8].unsqueeze(2).broadcast_to([128, H, pv]), op=ALU.mult),
                 reads=[k("Vd"), k("egs")], writes=[k("Vp")])
            for h in range(H):
                P.op("pe", lambda e, h=h, PT=PT, Vd=Vd: e.matmul(Yps[:, h, :], lhsT=PT[:, h * 128:(h + 1) * 128], rhs=Vd[:, h, :], start=True, stop=True),
                     reads=[k("PT"), k("Vd")], writes=[kg("Yps")], sig=(h == H - 1))
            for h in range(H):
                r0, r1 = (0, 128) if ufull else strow(h)
                P.op("pe", lambda e, h=h, c=c, d=d, r0=r0, r1=r1: e.matmul(Ups[:, h, :], lhsT=QT(h if ng == H else h // rep, c), rhs=Stb[r0:r1, d, h, :],
                                                                           start=True, stop=True), reads=["qkT", k("Stb")], writes=[kg("Ups")], sig=(h == H - 1))
            P.op("dve", lambda e, T1=T1, egs=egs: e.tensor_tensor(out=T1[:], in0=Ups[:], in1=egs[:, 0:4].unsqueeze(2).broadcast_to([128, H, pv]), op=ALU.mult),
                 reads=[kg("Ups"), k("egs")], writes=[k("T1")])
            P.op("dve", lambda e, T1=T1: e.tensor_tensor(out=T1[:], in0=T1[:], in1=Yps[:], op=ALU.add), reads=[k("T1"), kg("Yps")], writes=[k("T1")])
            accum(c, d, T1, k("T1"))
            for h in range(H):
                P.op("pe", lambda e, h=h, c=c, Vp=Vp: e.matmul(U2ps[:, h, :], lhsT=Ktm(h, c), rhs=Vp[:, h, :], start=True, stop=True),
                     reads=["ktm", k("Vp")], writes=[kg("U2ps")], sig=(h == H - 1))
            rows = sorted(set(strow(h) for h in range(H)))
            for (r0, r1) in rows:
                hs = [h for h in range(H) if strow(h) == (r0, r1)]
                h0, hstep = hs[0], (hs[1] - hs[0] if len(hs) > 1 else 1)
                sl = slice(h0, hs[-1] + 1, hstep)
                P.op("pool", lambda e, r0=r0, r1=r1, sl=sl, d=d, nh=len(hs), egs=egs: e.tensor_tensor(
                    out=St[r0:r1, d, sl, :], in0=St[r0:r1, d, sl, :], in1=egs[r0:r1, 8:12][:, sl].unsqueeze(2).broadcast_to([r1 - r0, nh, pv]), op=ALU.mult),
                    reads=[k("St"), k("egs")], writes=[k("St")])
                P.op("dve", lambda e, r0=r0, r1=r1, sl=sl, d=d: e.tensor_tensor(out=St[r0:r1, d, sl, :], in0=St[r0:r1, d, sl, :], in1=U2ps[r0:r1, sl, :], op=ALU.add),
                     reads=[k("St"), kg("U2ps")], writes=[k("St")])
                P.op("act", lambda e, r0=r0, r1=r1, sl=sl, d=d: e.activation(out=Stb[r0:r1, d, sl, :], in_=St[r0:r1, d, sl, :], func=AF.Copy),
                     reads=[k("St")], writes=[k("Stb")])


def _mix_ssd(self, b, l):
    I, P = self.I, self.P
    with ExitStack() as sm:
        xs = self.sb(sm, "s_xs", [128, NT, 256], BF16)
        Btm = self.sb(sm, "s_Btm", [128, NT, 256], BF16)
        BT = self.sb(sm, "s_BT", [128, 2, L], BF16)
        CT = self.sb(sm, "s_CT", [128, 2, L], BF16)
        dt = self.sb(sm, "s_dt", [128, NT, 8])
        av = self.sb(sm, "s_av", [128, NT, 8])
        cbT = self.sb(sm, "s_cbT", [128, 6])
        cbrow = self.sb(sm, "s_cbrow", [1, 768])
        dtb = self.sb(sm, "s_dtb", [128, 8])
        Arow = self.sb(sm, "s_Arow", [128, 8])
        Drow = self.sb(sm, "s_Drow", [128, 4])
        grow = self.sb(sm, "s_grow", [128, 256])
        Yacc = self.sb(sm, "s_Yacc", [128, NT, 256])
        P.op("sp", lambda e: e.dma_start(out=cbT[:], in_=I["ssd_cbT"][l]), writes=["s_cbT"], dma="s_c")
        P.op("sp", lambda e: e.dma_start(out=cbrow[:], in_=I["ssd_conv_b"][l:l + 1, :]), writes=["cbrow"], dma="s_c")
        P.op("sp", lambda e: e.dma_start(out=dtb[:], in_=I["ssd_dt_bias"][l].partition_broadcast(128)), writes=["s_dtb"], dma="s_c")
        P.op("sp", lambda e: e.dma_start(out=Arow[:], in_=I["ssd_a_log"][l].partition_broadcast(128)), writes=["s_Arow"], dma="s_c")
        P.op("sp", lambda e: e.dma_start(out=Drow[:], in_=I["ssd_d"][l].partition_broadcast(128)), writes=["s_Drow"], dma="s_c")
        P.op("sp", lambda e: e.dma_start(out=grow[:], in_=I["ssd_norm_g"][l].partition_broadcast(128)), writes=["s_grow"], dma="s_c")
        P.op("act", lambda e: e.activation(out=Arow[:], in_=Arow[:], func=AF.Exp), reads=["s_Arow"], writes=["s_Arow"])
        P.op("dve", lambda e: e.tensor_scalar(out=Arow[:], in0=Arow[:], scalar1=-1.0, scalar2=None, op0=ALU.mult), reads=["s_Arow"], writes=["s_Arow"])
        with ExitStack() as st:
            def ev_tm(tag, i, ps, pk):
                if tag == "x":
                    P.op("act", lambda e: e.activation(out=xs[:, i, :], in_=ps, func=AF.Silu), reads=[pk], writes=["s_xs"])
                elif tag == "B":
                    P.op("act", lambda e: e.activation(out=Btm[:, i, :], in_=ps, func=AF.Silu), reads=[pk], writes=["ktm"])
                else:
                    P.op("dve", lambda e: e.tensor_tensor(out=dt[:, i, :], in0=ps, in1=dtb[:], op=ALU.add), reads=[pk, "s_dtb"], writes=["s_dt"])
            self.proj_tm(st, l, [(0, 0, 256, "x", cbrow[0:1, 0:256]), (0, 256, 256, "B", cbrow[0:1, 256:512]),
                                 (None, 1024, 8, "dt", None)], ev_tm)

            def ev_fm(tag, tb, ps, pk):
                dst = (BT if tag < 2 else CT)[:, tag % 2, tb * 512:(tb + 1) * 512]
                P.op("act", lambda e: e.activation(out=dst, in_=ps, func=AF.Silu, bias=cbT[:, 2 + tag:3 + tag]), reads=[pk, "s_cbT"], writes=["qkT"])
            self.proj_fm(st, l, [(0, 256 + 128 * t, 128, t) for t in range(4)], ev_fm)
            P.op("act", lambda e: e.activation(out=av[:], in_=dt[:], func=AF.Exp), reads=["s_dt"], writes=["avals"])
            P.op("act", lambda e: e.activation(out=dt[:], in_=av[:], func=AF.Ln, bias=1.0), reads=["avals"], writes=["s_dt"])
            P.op("dve", lambda e: e.tensor_tensor(out=av[:], in0=dt[:], in1=Arow[:].unsqueeze(1).broadcast_to([128, NT, 8]), op=ALU.mult),
                 reads=["s_dt", "s_Arow"], writes=["avals"])
            P.op("dve", lambda e: e.tensor_tensor(out=Yacc[:].rearrange("p i (h c) -> p i h c", h=4), in0=xs[:].rearrange("p i (h c) -> p i h c", h=4),
                                                  in1=Drow[:].unsqueeze(1).unsqueeze(3).broadcast_to([128, NT, 4, 64]), op=ALU.mult),
                 reads=["s_xs", "s_Drow"], writes=["s_Yacc"])
            P.flush()
        with ExitStack() as st:
            def build_vd(c, d, Vd, vk):
                P.op("dve", lambda e: e.tensor_tensor(out=Vd[:], in0=xs[:, c, :].rearrange("p (h c) -> p h c", h=4),
                                                      in1=dt[:, c, 4 * d:4 * d + 4].unsqueeze(2).broadcast_to([128, 4, 64]), op=ALU.mult),
                     reads=["s_xs", "s_dt"], writes=[vk])

            def accum(c, d, T1, tk):
                P.op("pool", lambda e: e.tensor_tensor(out=Yacc[:, c, :], in0=Yacc[:, c, :], in1=T1[:].rearrange("p h c -> p (h c)"), op=ALU.add),
                     reads=[tk, "s_Yacc"], writes=["s_Yacc"])
            self.sweep(st, "ss_", 128, 64, [(0, [0, 1]), (1, [2, 3])],
                       KT=lambda g, c: BT[:, g, c * 128:(c + 1) * 128], QT=lambda g, c: CT[:, g, c * 128:(c + 1) * 128],
                       Ktm=lambda h, c: Btm[:, c, (h // 2) * 128:(h // 2 + 1) * 128], strow=lambda h: (0, 128),
                       a_ap=lambda c, d: av[:, c, 4 * d:4 * d + 4], build_vd=build_vd, accum=accum)
            P.flush()
        with ExitStack() as st:
            epi = self.epilogue(st, "s_")
            zt = [self.sb(st, f"s_zt{i}", [128, 256]) for i in range(2)]

            def ev_z(tag, i, ps, pk):
                z, zk = zt[i % 2], ("s_zt", i % 2)
                P.op("act", lambda e: e.activation(out=z[:], in_=ps, func=AF.Silu), reads=[pk], writes=[zk])
                P.op("dve", lambda e: e.tensor_tensor(out=z[:], in0=z[:], in1=Yacc[:, i, :], op=ALU.mult), reads=[zk, "s_Yacc"], writes=[zk])
                epi(i, z[:], zk, grow[:], "s_grow")
            self.proj_tm(st, l, [(None, 0, 256, "z", None)], ev_z, tagp="pz")
            P.flush()


KB.phase0_wcv = _phase0_wcv
KB.proj_tm = _proj_tm
KB.proj_fm = _proj_fm
KB.epilogue = _epilogue
KB.to_yT = _to_yT
KB.sweep = _sweep
KB.mix_ssd = _mix_ssd


def _mix_ml(self, b, l):
    I, P = self.I, self.P
    base = OFF_ML
    with ExitStack() as sm:
        qT = self.sb(sm, "m_qT", [128, 2, L], BF16)
        kT = self.sb(sm, "m_kT", [128, 4, L], BF16)
        P.op("pool", lambda e: e.memset(kT[:], 0.0), writes=["qkT"])
        Ktm = self.sb(sm, "m_Ktm", [128, NT, 256], BF16)
        vtm = self.sb(sm, "m_vtm", [128, NT, 256], BF16)
        gt = self.sb(sm, "m_gt", [128, NT, 16])
        ei = self.sb(sm, "m_ei", [128, NT, 8])
        av = self.sb(sm, "m_av", [128, NT, 8])
        ib = self.sb(sm, "m_ib", [128, 8])
        fb = self.sb(sm, "m_fb", [128, 8])
        grow = self.sb(sm, "m_grow", [128, 256])
        Hacc = self.sb(sm, "m_Hacc", [128, NT, 256])
        P.op("sp", lambda e: e.dma_start(out=ib[:], in_=I["ml_i_bias"][l].partition_broadcast(128)), writes=["m_ib"], dma="m_c")
        P.op("sp", lambda e: e.dma_start(out=fb[:], in_=I["ml_f_bias"][l].partition_broadcast(128)), writes=["m_fb"], dma="m_c")
        P.op("sp", lambda e: e.dma_start(out=grow[:], in_=I["ml_norm_g"][l].partition_broadcast(128)), writes=["m_grow"], dma="m_c")
        P.op("pool", lambda e: e.memset(Hacc[:], 0.0), writes=["m_Hacc"])
        with ExitStack() as st:
            def ev_fm(tag, tb, ps, pk):
                sl = slice(tb * 512, (tb + 1) * 512)
                if tag < 2:
                    P.op("act", lambda e: e.activation(out=qT[:, tag, sl], in_=ps, func=AF.Copy), reads=[pk], writes=["qkT"])
                else:
                    pair = tag - 2
                    P.op("act", lambda e: e.activation(out=kT[0:64, 2 * pair, sl], in_=ps[0:64, :], func=AF.Copy), reads=[pk], writes=["qkT"])
                    P.op("dve", lambda e: e.tensor_copy(out=kT[64:128, 2 * pair + 1, sl], in_=ps[64:128, :]), reads=[pk], writes=["qkT"])
            self.proj_fm(st, l, [(None, base + 128 * t, 128, t) for t in range(4)], ev_fm)

            def ev_tm(tag, i, ps, pk):
                if tag == "k":
                    P.op("act", lambda e: e.activation(out=Ktm[:, i, :], in_=ps, func=AF.Copy), reads=[pk], writes=["ktm"])
                elif tag == "v":
                    P.op("dve", lambda e: e.tensor_copy(out=vtm[:, i, :], in_=ps), reads=[pk], writes=["m_vtm"])
                else:
                    P.op("dve", lambda e: e.tensor_copy(out=gt[:, i, :], in_=ps), reads=[pk], writes=["m_gt"])
            self.proj_tm(st, l, [(None, base + 256, 256, "k", None), (None, base + 512, 256, "v", None),
                                 (None, base + 1024, 16, "g", None)], ev_tm)
            P.op("dve", lambda e: e.tensor_tensor(out=ei[:], in0=gt[:, :, 0:8], in1=ib[:].unsqueeze(1).broadcast_to([128, NT, 8]), op=ALU.add),
                 reads=["m_gt", "m_ib"], writes=["m_ei"])
            P.op("act", lambda e: e.activation(out=ei[:], in_=ei[:], func=AF.Exp), reads=["m_ei"], writes=["m_ei"])
            P.op("dve", lambda e: e.tensor_scalar(out=ei[:], in0=ei[:], scalar1=0.125, scalar2=None, op0=ALU.mult), reads=["m_ei"], writes=["m_ei"])
            P.op("dve", lambda e: e.tensor_tensor(out=av[:], in0=gt[:, :, 8:16], in1=fb[:].unsqueeze(1).broadcast_to([128, NT, 8]), op=ALU.add),
                 reads=["m_gt", "m_fb"], writes=["avals"])
            P.op("act", lambda e: e.activation(out=av[:], in_=av[:], func=AF.Exp, scale=-1.0), reads=["avals"], writes=["avals"])
            P.op("act", lambda e: e.activation(out=av[:], in_=av[:], func=AF.Ln, bias=1.0), reads=["avals"], writes=["avals"])
            P.op("dve", lambda e: e.tensor_scalar(out=av[:], in0=av[:], scalar1=-1.0, scalar2=None, op0=ALU.mult), reads=["avals"], writes=["avals"])
            P.flush()
        with ExitStack() as st:
            dd_ = [self.sb(st, f"m_dd{i}", [128, 4, 1]) for i in range(2)]
            hh_ = [self.sb(st, f"m_hh{i}", [128, 4, 64]) for i in range(2)]

            def build_vd(c, d, Vd, vk):
                P.op("dve", lambda e: e.tensor_tensor(out=Vd[:, :, 0:64], in0=vtm[:, c, :].rearrange("p (h c) -> p h c", h=4),
                                                      in1=ei[:, c, 4 * d:4 * d + 4].unsqueeze(2).broadcast_to([128, 4, 64]), op=ALU.mult),
                     reads=["m_vtm", "m_ei"], writes=[vk])
                P.op("act", lambda e: e.activation(out=Vd[:, :, 64:65], in_=ei[:, c, 4 * d:4 * d + 4].unsqueeze(2), func=AF.Copy),
                     reads=["m_ei"], writes=[vk])

            def accum(c, d, T1, tk):
                dd, hh, dk, hk = dd_[d], hh_[d], f"m_dd{d}", f"m_hh{d}"
                den = T1[:, :, 64:65]
                P.op("dve", lambda e: e.scalar_tensor_tensor(out=dd[:], in0=den, scalar=-1.0, in1=den, op0=ALU.mult, op1=ALU.max),
                     reads=[tk], writes=[dk])
                P.op("dve", lambda e: e.tensor_scalar_max(out=dd[:], in0=dd[:], scalar1=1.0), reads=[dk], writes=[dk])
                P.op("dve", lambda e: e.reciprocal(out=dd[:], in_=dd[:]), reads=[dk], writes=[dk])
                P.op("dve", lambda e: e.tensor_tensor(out=hh[:], in0=T1[:, :, 0:64], in1=dd[:].broadcast_to([128, 4, 64]), op=ALU.mult),
                     reads=[tk, dk], writes=[hk])
                P.op("pool", lambda e: e.tensor_tensor(out=Hacc[:, c, :], in0=Hacc[:, c, :], in1=hh[:].rearrange("p h c -> p (h c)"), op=ALU.add),
                     reads=[hk, "m_Hacc"], writes=["m_Hacc"])
            pr = lambda h: ((h % 2) * 64, (h % 2) * 64 + 64)
            self.sweep(st, "ms_", 64, 65, [(h, [h]) for h in range(4)],
                       KT=lambda h, c: kT[:, h, c * 128:(c + 1) * 128],
                       QT=lambda h, c: qT[:, h // 2, c * 128:(c + 1) * 128],
                       Ktm=lambda h, c: Ktm[:, c, (h // 2) * 128:(h // 2 + 1) * 128], strow=pr,
                       a_ap=lambda c, d: av[:, c, 4 * d:4 * d + 4], build_vd=build_vd, accum=accum, ufull=True)
            P.flush()
        with ExitStack() as st:
            junk = self.sb(st, "m_junk", [128, 256])
            ss4 = self.sb(st, "m_ss4", [128, NT, 4])
            sg = [self.sb(st, f"m_sg{i}", [128, 256]) for i in range(2)]
            yb = [self.sb(st, f"m_yb{i}", [128, 256], BF16) for i in range(2)]
            tp = [self.ps(st, f"m_tp{i}", [128, 256], BF16) for i in range(2)]

            def ev_o(tag, i, ps, pk):
                s, sk = sg[i % 2], ("m_sg", i % 2)
                y, yk = yb[i % 2], ("m_yb", i % 2)
                P.op("act", lambda e: e.activation(out=s[:], in_=ps, func=AF.Sigmoid), reads=[pk], writes=[sk])
                P.op("act", lambda e: e.activation(out=junk[:], in_=Hacc[:, i, :], func=AF.Square), reads=["m_Hacc"], writes=["m_junk"])
                P.op("dve", lambda e: e.reduce_sum(out=ss4[:, i, :], in_=junk[:].rearrange("p (h c) -> p h c", h=4), axis=AX.X),
                     reads=["m_junk"], writes=[("m_ss4", i)])
                P.op("act", lambda e: e.activation(out=ss4[:, i, :], in_=ss4[:, i, :], func=AF.Sqrt, bias=1e-6, scale=1.0 / 64),
                     reads=[("m_ss4", i)], writes=[("m_ss4", i)])
                P.op("dve", lambda e: e.reciprocal(out=ss4[:, i, :], in_=ss4[:, i, :]), reads=[("m_ss4", i)], writes=[("m_ss4", i)])
                P.op("dve", lambda e: e.tensor_tensor(out=s[:], in0=s[:], in1=grow[:], op=ALU.mult), reads=[sk, "m_grow"], writes=[sk])
                P.op("dve", lambda e: e.tensor_tensor(out=s[:].rearrange("p (h c) -> p h c", h=4), in0=s[:].rearrange("p (h c) -> p h c", h=4),
                                                      in1=ss4[:, i, :].unsqueeze(2).broadcast_to([128, 4, 64]), op=ALU.mult),
                     reads=[sk, ("m_ss4", i)], writes=[sk])
                P.op("dve", lambda e: e.tensor_tensor(out=y[:], in0=s[:], in1=Hacc[:, i, :], op=ALU.mult), reads=[sk, "m_Hacc"], writes=[yk])
                self.to_yT(i, y, yk, tp[i % 2], ("m_tp", i % 2))
            self.proj_tm(st, l, [(None, base + 768, 256, "o", None)], ev_o, tagp="po")
            P.flush()


def _mix_na(self, b, l):
    I, P = self.I, self.P
    base = OFF_NA
    with ExitStack() as sm:
        qT = self.sb(sm, "a_qT", [128, 2, L], BF16)
        kT = self.sb(sm, "a_kT", [128, 2, L], BF16)
        Ve = self.sb(sm, "a_Ve", [128, NT, 256], BF16)
        Vo = self.sb(sm, "a_Vo", [128, NT - 1, 256], BF16)
        yna = self.sb(sm, "a_y", [128, NT, 256])
        mask = self.sb(sm, "a_mask", [128, 512], BF16)
        grow = self.sb(sm, "a_grow", [128, 256])
        P.op("sp", lambda e: e.dma_start(out=mask[:], in_=I["na_mask"]), writes=["a_mask"], dma="a_c")
        P.op("sp", lambda e: e.dma_start(out=grow[:], in_=I["na_norm_g"][l].partition_broadcast(128)), writes=["a_grow"], dma="a_c")
        with ExitStack() as st:
            def ev_fm(tag, tb, ps, pk):
                dst = (qT if tag < 2 else kT)[:, tag % 2, tb * 512:(tb + 1) * 512]
                P.op("act", lambda e: e.activation(out=dst, in_=ps, func=AF.Copy, scale=(0.125 if tag < 2 else 1.0)), reads=[pk], writes=["a_qk"])
            self.proj_fm(st, l, [(None, base + 128 * t, 128, t) for t in range(4)], ev_fm)
            self.proj_tm(st, l, [(None, base + 512, 256, "v", None)],
                         lambda tag, i, ps, pk: P.op("act", lambda e: e.activation(out=Ve[:, i, :], in_=ps, func=AF.Copy), reads=[pk], writes=["a_V"]),
                         tagp="pve")
            P.flush()
        with ExitStack() as st:
            self.proj_tm(st, l, [(None, base + 512, 256, "v", None)],
                         lambda tag, i, ps, pk: P.op("act", lambda e: e.activation(out=Vo[:, i, :], in_=ps, func=AF.Copy), reads=[pk], writes=["a_V"]),
                         tiles=range(NT - 1), tok_off=64, tagp="pvo")
            P.flush()
        with ExitStack() as st:
            bsl = Slots("a_b", [self.sb(st, f"a_b{i}", [128, 4, 512], BF16) for i in range(2)])
            NB = 3
            Sps = [self.ps(st, f"a_S{i}", [128, 512]) for i in range(NB)]
            PTall = self.ps(st, "a_PTall", [128, 2048], BF16)
            PTp = [PTall[:, i * 512:(i + 1) * 512] for i in range(NB)]
            Oall = self.ps(st, "a_Oall", [128, 512])
            Ops = [Oall[:, i * 64:(i + 1) * 64] for i in range(NB)]
            Pb = [self.sb(st, f"a_P{i}", [128, 512], BF16) for i in range(NB)]
            PTs = [self.sb(st, f"a_PT{i}", [128, 512], BF16) for i in range(NB)]
            nmx = self.sb(st, "a_nmx", [128, NB])
            rsum = self.sb(st, "a_rs", [128, NB])
            n = 0
            last_dl, bt, bk = None, None, None
            for r in range(32):
                r0 = min(max(r - 4, 0), 24)
                dl = r0 - r + 7
                if dl != last_dl:
                    bt, bk = bsl.next()
                    P.op("pool", lambda e, bt=bt, dl=dl: e.dma_start(out=bt[:], in_=I["na_bias"][l, dl]), writes=[bk], dma=bk)
                    last_dl = dl
                ti, prow = r // 2, (r % 2) * 64
                Vt, v0 = (Ve, r0 // 2) if r0 % 2 == 0 else (Vo, (r0 - 1) // 2)
                for h in range(4):
                    p0 = (h % 2) * 64
                    u = n % NB
                    n += 1
                    S, Sk = Sps[u], ("a_S", u)
                    P.op("pe", lambda e, S=S, p0=p0, h=h, ti=ti, r0=r0: e.matmul(
                        S[:], lhsT=qT[p0:p0 + 64, h // 2, ti * 128:(ti + 1) * 128], rhs=kT[p0:p0 + 64, h // 2, 64 * r0:64 * r0 + 512],
                        start=True, stop=False), reads=["a_qk"], writes=[Sk], sig=False)
                    P.op("pe", lambda e, S=S, bt=bt, h=h: e.matmul(S[:], lhsT=self.ident_bf[:], rhs=bt[:, h, :], start=False, stop=False),
                         reads=["ident_bf", bk], writes=[Sk], sig=False)
                    P.op("pe", lambda e, S=S: e.matmul(S[:], lhsT=self.ident_bf[:], rhs=mask[:], start=False, stop=True),
                         reads=["ident_bf", "a_mask"], writes=[Sk])
                    P.op("dve", lambda e, S=S, u=u: e.reduce_max(out=nmx[:, u:u + 1], in_=S[:], axis=AX.X, negate=True), reads=[Sk], writes=[("a_nmx", u)])
                    P.op("act", lambda e, S=S, u=u: e.activation(out=Pb[u][:], in_=S[:], func=AF.Exp, bias=nmx[:, u:u + 1], accum_out=rsum[:, u:u + 1]),
                         reads=[Sk, ("a_nmx", u)], writes=[("a_P", u), ("a_rs", u)])
                    for kb in range(4):
                        P.op("pe", lambda e, u=u, kb=kb: e.transpose(PTp[u][:, kb * 128:(kb + 1) * 128], Pb[u][:, kb * 128:(kb + 1) * 128], self.ident_bf[:]),
                             reads=[("a_P", u), "ident_bf"], writes=[("a_PTp", u)], sig=(kb == 3))
                    P.op("dve", lambda e, u=u: e.tensor_copy(out=PTs[u][:], in_=PTp[u]), reads=[("a_PTp", u)], writes=[("a_PT", u)])
                    for kb in range(4):
                        P.op("pe", lambda e, u=u, kb=kb, Vt=Vt, v0=v0, h=h: e.matmul(
                            Ops[u], lhsT=PTs[u][:, kb * 128:(kb + 1) * 128], rhs=Vt[:, v0 + kb, h * 64:(h + 1) * 64], start=(kb == 0), stop=(kb == 3)),
                            reads=[("a_PT", u), "a_V"], writes=[("a_O", u)], sig=(kb == 3))
                    P.op("dve", lambda e, u=u: e.reciprocal(out=rsum[:, u:u + 1], in_=rsum[:, u:u + 1]), reads=[("a_rs", u)], writes=[("a_rs", u)])
                    P.op("act", lambda e, u=u, prow=prow, ti=ti, h=h: e.activation(
                        out=yna[prow:prow + 64, ti, h * 64:(h + 1) * 64], in_=Ops[u][prow:prow + 64, :], func=AF.Copy, scale=rsum[prow:prow + 64, u:u + 1]),
                        reads=[("a_O", u), ("a_rs", u)], writes=["a_y"])
            P.flush()
        with ExitStack() as st:
            epi = self.epilogue(st, "a_")
            for i in range(NT):
                epi(i, yna[:, i, :], "a_y", grow[:], "a_grow")
            P.flush()


KB.mix_ml = _mix_ml
KB.mix_na = _mix_na


def _phase0_hyena(self):
    I, P = self.I, self.P
    M = 12582912.0
    for l in range(2):
        with ExitStack() as sm:
            hn = self.sb(sm, "h0_hn", [128, NT, 1024], BF16)
            with ExitStack() as st:
                featsT = self.sb(st, "h0_f", [33, L])
                w1 = self.sb(st, "h0_w1", [33, 64]); w2 = self.sb(st, "h0_w2", [64, 64]); w3 = self.sb(st, "h0_w3", [64, 1024])
                bb = self.sb(st, "h0_bb", [64, 2]); fq = self.sb(st, "h0_fq", [64, 2])
                dec = self.sb(st, "h0_dec", [128, NT, 256])
                hT_ = [self.sb(st, f"h0_hT{i}", [64, L]) for i in range(2)]
                arg = self.sb(st, "h0_arg", [64, 512]); t1 = self.sb(st, "h0_t1", [64, 512])
                hw = self.sb(st, "h0_hw", [128, NT, 1024])
                sq = self.sb(st, "h0_sq", [128, 1024])
                rn = self.sb(st, "h0_rn", [128, 2, 256])
                mp = self.ps(st, "h0_mp", [64, 512])
                hp = self.ps(st, "h0_hp", [128, 1024])
                ssq = self.ps(st, "h0_ssq", [128, 1024])
                ld = lambda t, src, key: P.op("sp", lambda e: e.dma_start(out=t, in_=src), writes=[key], dma="h0c")
                ld(featsT[:], I["featsT"], "h0_f"); ld(w1[:], I["hy_w1"][l], "h0_w"); ld(w2[:], I["hy_w2"][l], "h0_w"); ld(w3[:], I["hy_w3"][l], "h0_w")
                ld(bb[:, 0:1], I["hy_b1"][l], "h0_w"); ld(bb[:, 1:2], I["hy_b2"][l], "h0_w")
                ld(fq[:, 0:1], I["hy_freq"][l, 0], "h0_w"); ld(fq[:, 1:2], I["hy_freq"][l, 1], "h0_w"); ld(dec[:], I["decay"], "h0_dec")
                for layer in range(2):
                    for tb in range(4):
                        sl = slice(tb * 512, (tb + 1) * 512)
                        if layer == 0:
                            P.op("pe", lambda e, sl=sl: e.matmul(mp[:], lhsT=w1[:], rhs=featsT[:, sl], start=True, stop=True), reads=["h0_w", "h0_f"], writes=["h0_mp"])
                        else:
                            P.op("pe", lambda e, sl=sl: e.matmul(mp[:], lhsT=w2[:], rhs=hT_[0][:, sl], start=True, stop=True), reads=["h0_w", "h0_hT0"], writes=["h0_mp"])
                        P.op("dve", lambda e, layer=layer: e.tensor_scalar(out=arg[:], in0=mp[:], scalar1=bb[:, layer:layer + 1], scalar2=fq[:, layer:layer + 1],
                                                                            op0=ALU.add, op1=ALU.mult), reads=["h0_mp", "h0_w"], writes=["h0_arg"])
                        P.op("dve", lambda e: e.tensor_scalar(out=t1[:], in0=arg[:], scalar1=1.0 / (2 * PI), scalar2=M, op0=ALU.mult, op1=ALU.add),
                             reads=["h0_arg"], writes=["h0_t1"])
                        P.op("dve", lambda e: e.tensor_scalar(out=t1[:], in0=t1[:], scalar1=-M, scalar2=-2 * PI, op0=ALU.add, op1=ALU.mult),
                             reads=["h0_t1"], writes=["h0_t1"])
                        P.op("dve", lambda e: e.tensor_tensor(out=arg[:], in0=arg[:], in1=t1[:], op=ALU.add), reads=["h0_arg", "h0_t1"], writes=["h0_arg"])
                        P.op("act", lambda e, layer=layer, sl=sl: e.activation(out=hT_[layer][:, sl], in_=arg[:], func=AF.Sin), reads=["h0_arg"], writes=[f"h0_hT{layer}"])
                for tt in range(NT):
                    for half in range(2):
                        P.op("pe", lambda e, tt=tt, half=half: e.matmul(hp[:, half * 512:(half + 1) * 512], lhsT=hT_[1][:, tt * 128:(tt + 1) * 128],
                                                                        rhs=w3[:, half * 512:(half + 1) * 512], start=True, stop=True),
                             reads=["h0_hT1", "h0_w"], writes=["h0_hp"], sig=(half == 1))
                    P.op("dve", lambda e, tt=tt: e.tensor_tensor(out=hw[:, tt, :].rearrange("p (a c) -> p a c", a=4), in0=hp[:].rearrange("p (a c) -> p a c", a=4),
                                                                  in1=dec[:, tt, :].unsqueeze(1).broadcast_to([128, 4, 256]), op=ALU.mult),
                         reads=["h0_hp", "h0_dec"], writes=["h0_hw"])
                    P.op("act", lambda e, tt=tt: e.activation(out=sq[:], in_=hw[:, tt, :], func=AF.Square), reads=["h0_hw"], writes=["h0_sq"])
                    for half in range(2):
                        P.op("pe", lambda e, tt=tt, half=half: e.matmul(ssq[:, half * 512:(half + 1) * 512], lhsT=self.ones_f[:], rhs=sq[:, half * 512:(half + 1) * 512],
                                                                        start=(tt == 0), stop=(tt == NT - 1)), reads=["ones_f", "h0_sq"], writes=["h0_ssq"], sig=(half == 1))
                P.op("act", lambda e: e.activation(out=sq[:], in_=ssq[:], func=AF.Copy), reads=["h0_ssq"], writes=["h0_sq"])
                sv = sq[:].rearrange("p (o d c) -> p o d c", o=2, d=2)
                P.op("dve", lambda e: e.tensor_tensor(out=rn[:], in0=sv[:, :, 0, :], in1=sv[:, :, 1, :], op=ALU.add), reads=["h0_sq"], writes=["h0_rn"])
                P.op("act", lambda e: e.activation(out=rn[:], in_=rn[:], func=AF.Sqrt, bias=1e-6), reads=["h0_rn"], writes=["h0_rn"])
                P.op("dve", lambda e: e.reciprocal(out=rn[:], in_=rn[:]), reads=["h0_rn"], writes=["h0_rn"])
                for tt in range(NT):
                    P.op("dve" if tt % 2 else "pool", lambda e, tt=tt: e.tensor_tensor(
                        out=hn[:, tt, :].rearrange("p (o d c) -> p o d c", o=2, d=2), in0=hw[:, tt, :].rearrange("p (o d c) -> p o d c", o=2, d=2),
                        in1=rn[:].unsqueeze(2).broadcast_to([128, 2, 2, 256]), op=ALU.mult), reads=["h0_hw", "h0_rn"], writes=["h0_hn"])
                P.op("dve", lambda e: e.memset(hn[0:1, 0, :].rearrange("p (o d c) -> p o d c", o=2, d=2)[:, :, 1, :], 0.0), reads=["h0_hn"], writes=["h0_hn"])
                if l == 0:
                    self.dump("hn", hn[:], "h0_hn")
                    self.dump("hw", hw[:], "h0_hw")
                    self.dump("h2T", hT_[1][:], "h0_hT1")
                P.flush()
            with ExitStack() as st:
                fsl = Slots("h0_F", [self.sb(st, f"h0_F{i}", [128, NT, 128], BF16) for i in range(3)])
                KP = [self.ps(st, f"h0_KP{i}", [128, 1024]) for i in range(2)]
                ksb = [self.sb(st, f"h0_ks{i}", [128, 1024]) for i in range(2)]
                tab = self.sb(st, "h0_tab", [128, 2, 3, 256])
                sc = 2.0 / 4096.0
                for j in range(NT):
                    for part in range(2):
                        r = j + 16 * part
                        f, fk = fsl.next()
                        P.op("sp", lambda e, f=f, r=r: e.dma_start(out=f[:], in_=I["FTt"][r]), writes=[fk], dma=fk)
                        for kk in range(NT):
                            for half in range(2):
                                P.op("pe", lambda e, f=f, kk=kk, half=half, part=part: e.matmul(
                                    KP[part][:, half * 512:(half + 1) * 512], lhsT=f[:, kk, :], rhs=hn[:, kk, half * 512:(half + 1) * 512],
                                    start=(kk == 0), stop=(kk == NT - 1)), reads=[fk, "h0_hn"], writes=[("h0_KP", part)], sig=(kk == NT - 1 and half == 1))
                        P.op("act", lambda e, part=part: e.activation(out=ksb[part][:], in_=KP[part][:], func=AF.Copy, scale=sc),
                             reads=[("h0_KP", part)], writes=[("h0_ks", part)])
                    kre = ksb[0][:].rearrange("p (o d c) -> p o d c", o=2, d=2)
                    kim = ksb[1][:].rearrange("p (o d c) -> p o d c", o=2, d=2)
                    P.op("dve", lambda e, kre=kre: e.tensor_tensor(out=tab[:, :, 0, :], in0=kre[:, :, 0, :], in1=kre[:, :, 1, :], op=ALU.add),
                         reads=[("h0_ks", 0)], writes=["h0_tab"])
                    P.op("dve", lambda e, kim=kim: e.tensor_tensor(out=tab[:, :, 1, :], in0=kim[:, :, 0, :], in1=kim[:, :, 1, :], op=ALU.subtract),
                         reads=[("h0_ks", 1)], writes=["h0_tab"])
                    P.op("dve", lambda e: e.tensor_copy(out=tab[:, :, 2, :], in_=tab[:, :, 0, :]), reads=["h0_tab"], writes=["h0_tab"])
                    if j == 0:
                        P.op("dve", lambda e, kim=kim: e.tensor_tensor(out=tab[0:1, :, 2, :], in0=kim[0:1, :, 0, :], in1=kim[0:1, :, 1, :], op=ALU.add),
                             reads=[("h0_ks", 1), "h0_tab"], writes=["h0_tab"])
                        P.op("dve", lambda e: e.tensor_scalar(out=tab[0:1, :, 2, :], in0=tab[0:1, :, 2, :], scalar1=0.5, scalar2=None, op0=ALU.mult),
                             reads=["h0_tab"], writes=["h0_tab"])
                        P.op("dve", lambda e: e.tensor_scalar(out=tab[0:1, :, 0, :], in0=tab[0:1, :, 0, :], scalar1=0.5, scalar2=None, op0=ALU.mult),
                             reads=["h0_tab"], writes=["h0_tab"])
                        P.op("dve", lambda e: e.memset(tab[0:1, :, 1, :], 0.0), reads=["h0_tab"], writes=["h0_tab"])
                    for o in range(2):
                        P.op("sp", lambda e, o=o, j=j: e.dma_start(out=self.kfs[l, o, j], in_=tab[:, o, :, :]), reads=["h0_tab"], writes=["kfs"], dma="kfst")
                        if l == 0 and "kfs" in self.dbg_out:
                            P.op("sp", lambda e, o=o, j=j: e.dma_start(out=self.dbg_out["kfs"][o, j], in_=tab[:, o, :, :]), reads=["h0_tab"], dma="dbg2")
                P.flush()


def _mix_hy(self, b, l):
    I, P = self.I, self.P
    with ExitStack() as sm:
        z = self.sb(sm, "y_z", [128, NT, 256], BF16)
        g1 = self.sb(sm, "y_g1", [128, NT, 256], BF16)
        g2 = self.sb(sm, "y_g2", [128, NT, 256], BF16)
        cbrow = self.sb(sm, "y_cbrow", [1, 768])
        skb = self.sb(sm, "y_skb", [128, 2, 256])
        grow = self.sb(sm, "y_grow", [128, 256])
        P.op("sp", lambda e: e.dma_start(out=cbrow[:], in_=I["hy_conv_b"][l:l + 1, :]), writes=["cbrow"], dma="y_c")
        P.op("sp", lambda e: e.dma_start(out=skb[:].rearrange("p a b -> p (a b)"), in_=I["hy_skip"][l].rearrange("a b -> (a b)").partition_broadcast(128)),
             writes=["y_skb"], dma="y_c")
        P.op("sp", lambda e: e.dma_start(out=grow[:], in_=I["hy_norm_g"][l].partition_broadcast(128)), writes=["y_grow"], dma="y_c")
        with ExitStack() as st:
            dsts = {"v": z, "x1": g1, "x2": g2}

            def ev(tag, i, ps, pk):
                P.op("act", lambda e: e.activation(out=dsts[tag][:, i, :], in_=ps, func=AF.Copy), reads=[pk], writes=["y_" + tag])
            self.proj_tm(st, l, [(1, 0, 256, "v", cbrow[0:1, 0:256]), (1, 256, 256, "x1", cbrow[0:1, 256:512]),
                                 (1, 512, 256, "x2", cbrow[0:1, 512:768])], ev)
            P.flush()
        with ExitStack() as st:
            Y = self.sb(st, "y_Y", [128, 32, 256], BF16)
            fsl = Slots("y_F", [self.sb(st, f"y_F{i}", [128, NT, 128], BF16) for i in range(4)])
            ksl = Slots("y_k", [self.sb(st, f"y_k{i}", [128, 3, 256]) for i in range(2)])
            Zp = [self.ps(st, f"y_Zp{i}", [128, 256]) for i in range(2)]
            Cp = [self.ps(st, f"y_Cp{i}", [128, 256]) for i in range(2)]
            zs = [self.sb(st, f"y_zs{i}", [128, 256]) for i in range(2)]
            tt_ = [self.sb(st, f"y_t{i}", [128, 256]) for i in range(4)]
            zf = [self.sb(st, f"y_zf{i}", [128, 256]) for i in range(2)]
            epi = self.epilogue(st, "y_")
            if b == 0 and l == 0:
                self.dump("z0", z[:], "y_v")
                self.dump("g1", g1[:], "y_x1")
            for o in range(2):
                if o == 1 and b == 0 and l == 0:
                    self.dump("z1", z[:], "y_v")
                gate = g1 if o == 0 else g2
                gk = "y_x1" if o == 0 else "y_x2"
                for j in range(NT):
                    kt, kk_ = ksl.next()
                    P.op("sp", lambda e, kt=kt, o=o, j=j: e.dma_start(out=kt[:], in_=self.kfs[l, o, j]), reads=["kfs"], writes=[kk_], dma=kk_)
                    for part in range(2):
                        f, fk = fsl.next()
                        P.op("sp", lambda e, f=f, j=j, part=part: e.dma_start(out=f[:], in_=I["FTt"][j + 16 * part]), writes=[fk], dma=fk)
                        for kk in range(NT):
                            P.op("pe", lambda e, f=f, kk=kk, part=part: e.matmul(Zp[part][:], lhsT=f[:, kk, :], rhs=z[:, kk, :], start=(kk == 0), stop=(kk == NT - 1)),
                                 reads=[fk, "y_v"], writes=[("y_Zp", part)], sig=(kk == NT - 1))
                        P.op("act", lambda e, part=part: e.activation(out=zs[part][:], in_=Zp[part][:], func=AF.Copy), reads=[("y_Zp", part)], writes=[("y_zs", part)])
                    for n_, (src, tb_) in enumerate(((0, 0), (1, 1), (0, 1), (1, 2))):
                        P.op("dve", lambda e, n_=n_, src=src, tb_=tb_, kt=kt: e.tensor_tensor(out=tt_[n_][:], in0=zs[src][:], in1=kt[:, tb_, :], op=ALU.mult),
                             reads=[("y_zs", src), kk_], writes=[("y_t", n_)])
                    P.op("pool", lambda e, j=j: e.tensor_tensor(out=Y[:, j, :], in0=tt_[0][:], in1=tt_[1][:], op=ALU.subtract),
                         reads=[("y_t", 0), ("y_t", 1)], writes=["y_Y"])
                    P.op("pool", lambda e, j=j: e.tensor_tensor(out=Y[:, 16 + j, :], in0=tt_[2][:], in1=tt_[3][:], op=ALU.add),
                         reads=[("y_t", 2), ("y_t", 3)], writes=["y_Y"])
                if o == 0 and b == 0 and l == 0:
                    self.dump("Y0", Y[:], "y_Y")
                for i in range(NT):
                    C, Ck = Cp[i % 2], ("y_Cp", i % 2)
                    for hf in range(2):
                        f, fk = fsl.next()
                        P.op("sp", lambda e, f=f, i=i, hf=hf: e.dma_start(out=f[:], in_=I["Ft"][i, :, hf * 16:(hf + 1) * 16, :]), writes=[fk], dma=fk)
                        for rr in range(16):
                            r = hf * 16 + rr
                            P.op("pe", lambda e, f=f, rr=rr, r=r, C=C: e.matmul(C[:], lhsT=f[:, rr, :], rhs=Y[:, r, :], start=(r == 0), stop=(r == 31)),
                                 reads=[fk, "y_Y"], writes=[Ck], sig=(rr == 15))
                    t_, tk = tt_[i % 2], ("y_t", i % 2)
                    P.op("dve", lambda e, t_=t_, i=i, o=o: e.tensor_tensor(out=t_[:], in0=z[:, i, :], in1=skb[:, o, :], op=ALU.mult),
                         reads=["y_v", "y_skb"], writes=[tk])
                    P.op("dve", lambda e, t_=t_, C=C: e.tensor_tensor(out=t_[:], in0=t_[:], in1=C[:], op=ALU.add), reads=[tk, Ck], writes=[tk])
                    if o == 0:
                        P.op("pool", lambda e, t_=t_, i=i, gate=gate: e.tensor_tensor(out=z[:, i, :], in0=t_[:], in1=gate[:, i, :], op=ALU.mult),
                             reads=[tk, gk], writes=["y_v"])
                    else:
                        zz, zk = zf[i % 2], ("y_zf", i % 2)
                        P.op("pool", lambda e, t_=t_, i=i, zz=zz, gate=gate: e.tensor_tensor(out=zz[:], in0=t_[:], in1=gate[:, i, :], op=ALU.mult),
                             reads=[tk, gk], writes=[zk])
                        epi(i, zz[:], zk, grow[:], "y_grow")
            P.flush()


KB.phase0_hyena = _phase0_hyena
KB.mix_hy = _mix_hy


def kernel(**inputs):
    kb = KB()
    nc = kb.build()
    maps = make_in_maps(inputs, ncores=8)
    res = run_bass_kernel_spmd(nc, maps, core_ids=list(range(8)))
    return np.concatenate([np.asarray(r["out"], dtype=np.float32) for r in res.results], axis=0)
```

```python
import os
import numpy as np
from contextlib import ExitStack
import concourse.bass as bass
import concourse.mybir as mybir
from concourse.bass_utils import run_bass_kernel_spmd

F32 = mybir.dt.float32
BF16 = mybir.dt.bfloat16
AF = mybir.ActivationFunctionType
ALU = mybir.AluOpType
AX = mybir.AxisListType

COMPUTE = ("pe", "act", "dve", "pool")
QUEUES = ("pe", "act", "dve", "pool", "sp")


class Prog:
    def __init__(self, nc, stack):
        self.nc = nc
        self.stack = stack
        self.ops = []
        self.W = {}
        self.R = {}
        self.chan_sem = {}
        self.chan_cnt = {}
        self.waited = {q: {} for q in QUEUES}
        self.nphase = 0
        self.epoch = {q: 0 for q in COMPUTE}
        self.unsig = {}
        self.last_unsig = {}
        for q in COMPUTE:
            self._sem(("E", q, 0))

    def _sem(self, chan):
        if chan not in self.chan_sem:
            name = "s_" + "_".join(str(c) for c in (chan if isinstance(chan, tuple) else (chan,)))
            self.chan_sem[chan] = self.stack.enter_context(self.nc.semaphore(name))
            self.chan_cnt[chan] = 0
        return self.chan_sem[chan]

    def op(self, q, fn, reads=(), writes=(), dma=None, sig=True):
        if dma is not None:
            chan = ("dma", dma, q)
            self._sem(chan)
            step = 16
        else:
            if self.chan_cnt[("E", q, self.epoch[q])] >= 30000 and not self.unsig.get(q):
                self.epoch[q] += 1
                self._sem(("E", q, self.epoch[q]))
            chan = ("E", q, self.epoch[q])
            step = 1
        deps = {}

        def need(c, v):
            if c[0] == "E" and c[1] == "pe" and q == "pe" and dma is None:
                return
            if c[0] == "dma":
                v = self.chan_cnt[c]
            elif v > self.chan_cnt[c]:
                ent = self.last_unsig[c]
                ent[4] = 1
                self.chan_cnt[c] += 1
                self.unsig[c[1]] = False
            if deps.get(c, 0) < v:
                deps[c] = v

        for k in reads:
            for c, v in self.W.get(k, {}).items():
                need(c, v)
        for k in writes:
            for c, v in self.W.get(k, {}).items():
                if c[0] == "E" and c[1] == q and dma is None:
                    continue
                need(c, v)
            for c, v in self.R.get(k, {}).items():
                if c[0] == "E" and c[1] == q and dma is None:
                    continue
                need(c, v)
        if sig or dma is not None:
            self.chan_cnt[chan] += step
            tok = self.chan_cnt[chan]
            if dma is None:
                self.unsig[q] = False
        else:
            tok = self.chan_cnt[chan] + step
            self.unsig[q] = True
            step = 0
        for k in reads:
            self.R.setdefault(k, {})[chan] = tok
        for k in writes:
            self.W.setdefault(k, {})[chan] = tok
        ent = [q, fn, deps, chan, step]
        if step == 0:
            self.last_unsig[chan] = ent
        self.ops.append(ent)
        return tok

    def barrier(self):
        snap = dict(self.chan_cnt)
        for q in QUEUES:
            self.ops.append([q, None, snap, None, 0])

    def flush(self, name=None):
        nc = self.nc
        ops, self.ops = self.ops, []
        self.nphase += 1
        per_q = {q: [o for o in ops if o[0] == q] for q in QUEUES}
        prog = self

        def emit(q, eng):
            waited = prog.waited[q]
            for (_, fn, deps, chan, step) in per_q[q]:
                for c, v in deps.items():
                    if v <= 0 or waited.get(c, 0) >= v:
                        continue
                    eng.wait_ge(prog.chan_sem[c], v)
                    waited[c] = v
                if fn is None:
                    continue
                ins = fn(eng)
                if step:
                    ins.then_inc(prog.chan_sem[chan], step)

        with nc.Block() as blk:
            @blk.tensor
            def _(e):
                emit("pe", e)

            @blk.scalar
            def _(e):
                emit("act", e)

            @blk.vector
            def _(e):
                emit("dve", e)

            @blk.gpsimd
            def _(e):
                emit("pool", e)

            @blk.sync
            def _(e):
                emit("sp", e)
        self.barrier()

    def finish(self, q="sp"):
        snap = dict(self.chan_cnt)
        self.ops.append([q, None, snap, None, 0])


L = 2048
D = 1024
NT = 16
FH = 2816
NFC = 22
PI = float(np.pi)
OFF_NA, OFF_ML, OFF_HY = 1032, 1800, 2840
_CONST_CACHE = {}


def _consts():
    if _CONST_CACHE:
        return _CONST_CACHE
    import ml_dtypes
    bf = ml_dtypes.bfloat16
    C = _CONST_CACHE
    C["ident_bf"] = np.eye(128, dtype=np.float32).astype(bf)
    C["ident_f"] = np.eye(128, dtype=np.float32)
    C["ones_f"] = np.ones((128, 128), np.float32)
    k = np.arange(128)[:, None]
    s = np.arange(128)[None, :]
    lm = np.stack([(k > s), (k < s), (k <= s), (k >= s)]).astype(np.float32)
    C["lmask"] = np.ascontiguousarray(lm.transpose(1, 0, 2))
    negf = np.where(k > s, -30000.0, 0.0)
    negb = np.where(k < s, -30000.0, 0.0)
    ng = np.stack([np.tile(negf, (1, 4)), np.tile(negb, (1, 4))]).astype(np.float32)
    C["neg"] = np.ascontiguousarray(ng.transpose(1, 0, 2)).astype(bf)
    j = (np.arange(128) % 64)[:, None]
    kc = np.arange(64)[None, :]
    cs = np.clip(j - 8, 0, 48)
    valid = (kc >= cs) & (kc < cs + 16)
    m = np.where(valid, 0.0, -30000.0).astype(np.float32)
    C["na_mask"] = np.tile(m, (1, 8)).astype(bf)
    t = np.linspace(0.0, 1.0, L, dtype=np.float32)[:, None]
    f = np.linspace(1e-4, 15.0, 16, dtype=np.float32)
    ang = (np.float32(2.0 * np.pi) * (np.arange(L, dtype=np.float32) / np.float32(L))[:, None] * f[None, :]).astype(np.float32)
    feats = np.concatenate([t, np.cos(ang), -np.sin(ang)], axis=-1).astype(np.float32)
    C["featsT"] = np.ascontiguousarray(feats.T)
    deltas = np.abs(np.linspace(np.log(1e-2) / 1.5, np.log(1e-2) / 0.3, 256, dtype=np.float32))
    dec = np.exp(-t * deltas[None, :]).astype(np.float32)
    C["decay"] = np.ascontiguousarray(dec.reshape(16, 128, 256).transpose(1, 0, 2))
    tt = np.arange(L, dtype=np.float64)[None, :]
    ff = np.arange(2048, dtype=np.float64)[:, None]
    Fm = np.empty((4096, L), np.float64)
    Fm[:2048] = np.cos(2 * np.pi * ff * tt / 4096.0)
    Fm[2048:] = -np.sin(2 * np.pi * ff * tt / 4096.0)
    Fm[2048] = np.where(np.arange(L) % 2 == 0, 1.0, -1.0)
    Fb = Fm.astype(np.float32).astype(bf)
    C["FTt"] = np.ascontiguousarray(Fb.reshape(32, 128, 16, 128).transpose(0, 3, 2, 1))
    C["Ft"] = np.ascontiguousarray(Fb.reshape(32, 128, 16, 128).transpose(2, 1, 0, 3))
    return C


def _na_bias_gather(rpb):
    j = (np.arange(128) % 64)[:, None]
    kc = np.arange(64)[None, :]
    dc = np.clip(kc - j + 15, 0, 30)
    out = np.empty((2, 8, 128, 4, 8, 64), np.float32)
    for dl in range(8):
        for kr in range(8):
            g = rpb[:, :, dl + kr, :][:, :, dc]
            out[:, dl, :, :, kr, :] = g.transpose(0, 2, 1, 3)
    return out.reshape(2, 8, 128, 4, 512)


class Slots:
    def __init__(self, name, tiles):
        self.name, self.tiles, self.i = name, tiles, 0

    def next(self):
        n = self.i % len(self.tiles)
        self.i += 1
        return self.tiles[n], (self.name, n)


class KB:
    def __init__(self, dbg=None, nseq=2, nlayer=2, mixers=("ssd", "na", "ml", "hy"), ycat_in=False):
        self.dbg = dbg or {}
        self.nseq, self.nlayer, self.mixers, self.ycat_in = nseq, nlayer, mixers, ycat_in
        self.nc = nc = bass.Bass("TRN2", target_bir_lowering=False)
        self.I = {}
        self.dbg_out = {}

        def din(name, shape, dt=F32):
            self.I[name] = nc.dram_tensor(name, list(shape), dt, kind="ExternalInput").ap()

        din("x", [2, L, D]); din("cT", [128, 8, 2]); din("mod_w", [2, D, 6 * D]); din("mod_bT", [2, 128, 48])
        din("w_in", [2, D, 3608]); din("ssd_conv_w", [2, 3, 768]); din("ssd_conv_b", [2, 768])
        din("ssd_cbT", [2, 128, 6]); din("ssd_dt_bias", [2, 8]); din("ssd_a_log", [2, 8]); din("ssd_d", [2, 4]); din("ssd_norm_g", [2, 256])
        din("na_bias", [2, 8, 128, 4, 512]); din("na_norm_g", [2, 256])
        din("ml_i_bias", [2, 8]); din("ml_f_bias", [2, 8]); din("ml_norm_g", [2, 256])
        din("hy_conv_w", [2, 3, 768]); din("hy_conv_b", [2, 768]); din("hy_w1", [2, 33, 64]); din("hy_b1", [2, 64, 1])
        din("hy_w2", [2, 64, 64]); din("hy_b2", [2, 64, 1]); din("hy_w3", [2, 64, 1024]); din("hy_freq", [2, 2, 64, 1])
        din("hy_skip", [2, 2, 256]); din("hy_norm_g", [2, 256]); din("w_out", [2, D, D])
        din("ffn_w_gate", [2, D, FH]); din("ffn_w_up", [2, D, FH]); din("ffn_w_down", [2, FH, D]); din("final_norm_g", [D])
        din("ident_bf", [128, 128], BF16); din("ident_f", [128, 128]); din("ones_f", [128, 128])
        din("lmask", [128, 4, 128]); din("neg", [128, 2, 512], BF16); din("na_mask", [128, 512], BF16)
        din("featsT", [33, L]); din("decay", [128, 16, 256]); din("FTt", [32, 128, 16, 128], BF16); din("Ft", [16, 128, 32, 128], BF16)
        if ycat_in:
            din("ycatT", [2, 2, 128, 8, L], BF16)
        self.out = nc.dram_tensor("out", [2, L, D], F32, kind="ExternalOutput").ap()
        for k, shp in self.dbg.items():
            self.dbg_out[k] = nc.dram_tensor("dbg_" + k, list(shp), F32, kind="ExternalOutput").ap()
        self.wcv = nc.dram_tensor("wcv", [2, 2, 3, D, 768], BF16, kind="Internal").ap()
        self.kfs = nc.dram_tensor("kfs", [2, 2, 16, 128, 3, 256], F32, kind="Internal").ap()

    def sb(self, st, name, shape, dt=F32):
        self._uid = getattr(self, "_uid", 0) + 1
        return st.enter_context(self.nc.sbuf_tensor(f"{name}_{self._uid}", list(shape), dt))

    def ps(self, st, name, shape, dt=F32):
        self._uid = getattr(self, "_uid", 0) + 1
        return st.enter_context(self.nc.psum_tensor(f"{name}_{self._uid}", list(shape), dt))

    def dump(self, name, src_ap, key, dst=None):
        if name not in self.dbg_out:
            return
        d = self.dbg_out[name] if dst is None else dst
        self.P.op("pool", lambda e: e.dma_start(out=d, in_=src_ap), reads=[key], dma="dbg")

    def build(self):
        nc, I = self.nc, self.I
        with ExitStack() as st:
            self.P = P = Prog(nc, st)
            self.ident_bf = self.sb(st, "ident_bf", [128, 128], BF16)
            self.ident_f = self.sb(st, "ident_f", [128, 128])
            self.ones_f = self.sb(st, "ones_f", [128, 128])
            self.modT = self.sb(st, "modT", [128, 2, 48, 2])
            self.condT = self.sb(st, "condT", [128, 8, 2], BF16)
            for nm in ("ident_bf", "ident_f", "ones_f"):
                t = getattr(self, nm)
                P.op("sp", lambda e, t=t, nm=nm: e.dma_start(out=t[:], in_=I[nm]), writes=[nm], dma="const")
            self.phase0()
            with ExitStack() as sx:
                self.x = self.sb(sx, "xres", [128, NT, D])
                for b in range(self.nseq):
                    for i in range(NT):
                        P.op("sp", lambda e, b=b, i=i: e.dma_start(out=self.x[:, i, :], in_=I["x"][b, i * 128:(i + 1) * 128, :]),
                             writes=[("x", i)], dma="xload")
                    for l in range(self.nlayer):
                        self.layer(b, l)
                    self.final(b)
            P.finish()
            P.flush()
        return nc

    def phase0(self):
        nc, I, P = self.nc, self.I, self.P
        with ExitStack() as st:
            cTf = self.sb(st, "cTf", [128, 8, 2])
            mbT = self.sb(st, "mbT", [128, 2, 48])
            wsl = Slots("mw", [self.sb(st, f"mw{i}", [128, 8, 512], BF16) for i in range(2)])
            mps = self.ps(st, "mps", [128, 96])
            P.op("sp", lambda e: e.dma_start(out=cTf[:], in_=I["cT"]), writes=["cTf"], dma="const")
            for l in range(2):
                P.op("sp", lambda e, l=l: e.dma_start(out=mbT[:, l, :], in_=I["mod_bT"][l]), writes=["mbT"], dma="const")
            P.op("act", lambda e: e.activation(out=self.condT[:], in_=cTf[:], func=AF.Silu), reads=["cTf"], writes=["condT"])
            for l in range(2):
                wv = I["mod_w"][l].rearrange("(j p) f -> p j f", p=128)
                for blk in range(12):
                    w, wk = wsl.next()
                    P.op("pool", lambda e, w=w, blk=blk, wv=wv: e.dma_start(out=w[:], in_=wv[:, :, blk * 512:(blk + 1) * 512]),
                         writes=[wk], dma=wk)
                    for fc in range(4):
                        col = (blk * 4 + fc) * 2
                        for j in range(8):
                            P.op("pe", lambda e, w=w, fc=fc, j=j, col=col: e.matmul(
                                mps[:, col:col + 2], lhsT=w[:, j, fc * 128:(fc + 1) * 128], rhs=self.condT[:, j, :],
                                start=(j == 0), stop=(j == 7)), reads=[wk, "condT"], writes=["mps"], sig=(j == 7))
                mv = mps[:].rearrange("p (c b) -> p c b", b=2)
                P.op("dve", lambda e, l=l, mv=mv: e.tensor_tensor(
                    out=self.modT[:, l, :, :], in0=mv, in1=mbT[:, l, :].unsqueeze(2).broadcast_to([128, 48, 2]), op=ALU.add),
                    reads=["mps", "mbT"], writes=["modT"])
                for c0 in (8, 32):
                    P.op("dve", lambda e, l=l, c0=c0: e.tensor_scalar_add(
                        out=self.modT[:, l, c0:c0 + 8, :], in0=self.modT[:, l, c0:c0 + 8, :], scalar1=1.0),
                        reads=["modT"], writes=["modT"])
            if "modT" in self.dbg_out:
                self.dump("modT", self.modT[:], "modT")
            P.flush()
        if "hy" in self.mixers or "ssd" in self.mixers:
            self.phase0_wcv()
        if "hy" in self.mixers:
            self.phase0_hyena()

    def phase0_wcv(self):
        pass

    def phase0_hyena(self):
        pass

    def norm_to_hT(self, st, b, l, which):
        nc, P = self.nc, self.P
        sh_c, sc_c = (0, 8) if which == 0 else (24, 32)
        ss = self.sb(st, "n_ss", [128, NT])
        rstd = self.sb(st, "n_rstd", [128, NT])
        junk = self.sb(st, "n_junk", [128, D], BF16)
        xn = [self.sb(st, f"n_xn{i}", [128, 4, D], BF16) for i in range(2)]
        tp = [self.ps(st, f"n_tp{i}", [128, 512], BF16) for i in range(2)]
        hT = self.hT
        P.op("dve", lambda e: e.memset(hT[:, :, 0:1], 0.0), writes=["hT"])
        P.op("dve", lambda e: e.memset(hT[:, :, L + 1:L + 2], 0.0), writes=["hT"])
        cnt = [0]

        def chain(tb):
            xb, xk = xn[tb % 2], ("n_xn", tb % 2)
            for ii in range(4):
                i = tb * 4 + ii
                P.op("act", lambda e, i=i: e.activation(out=junk[:], in_=self.x[:, i, :], func=AF.Square,
                                                         accum_out=ss[:, i:i + 1]), reads=[("x", i)], writes=["n_junk", ("n_ss", i)])
                P.op("act", lambda e, i=i: e.activation(out=rstd[:, i:i + 1], in_=ss[:, i:i + 1], func=AF.Sqrt, bias=1e-6, scale=1.0 / D),
                     reads=[("n_ss", i)], writes=[("n_rs", i)])
                P.op("dve", lambda e, i=i: e.reciprocal(out=rstd[:, i:i + 1], in_=rstd[:, i:i + 1]), reads=[("n_rs", i)], writes=[("n_rs", i)])
                P.op("dve", lambda e, i=i, ii=ii, xb=xb: e.tensor_scalar(out=xb[:, ii, :], in0=self.x[:, i, :],
                                                                        scalar1=rstd[:, i:i + 1], scalar2=None, op0=ALU.mult),
                     reads=[("x", i), ("n_rs", i)], writes=[xk])

        def trans(tb):
            xb, xk = xn[tb % 2], ("n_xn", tb % 2)
            for j in range(8):
                n = cnt[0]
                cnt[0] += 1
                t, tk = tp[n % 2], ("n_tp", n % 2)
                for ii in range(4):
                    P.op("pe", lambda e, t=t, ii=ii, j=j, xb=xb: e.transpose(
                        t[:, ii * 128:(ii + 1) * 128], xb[:, ii, j * 128:(j + 1) * 128], self.ident_bf[:]),
                        reads=[xk, "ident_bf"], writes=[tk], sig=(ii == 3))
                dst = hT[:, j, 1 + tb * 512:1 + (tb + 1) * 512]
                if j % 2 == 0:
                    P.op("act", lambda e, t=t, dst=dst, j=j: e.activation(
                        out=dst, in_=t[:], func=AF.Identity, bias=self.modT[:, l, sh_c + j, b:b + 1],
                        scale=self.modT[:, l, sc_c + j, b:b + 1]), reads=[tk, "modT"], writes=[("hT", tb)])
                else:
                    P.op("dve", lambda e, t=t, dst=dst, j=j: e.tensor_scalar(
                        out=dst, in0=t[:], scalar1=self.modT[:, l, sc_c + j, b:b + 1],
                        scalar2=self.modT[:, l, sh_c + j, b:b + 1], op0=ALU.mult, op1=ALU.add),
                        reads=[tk, "modT"], writes=[("hT", tb)])

        chain(0)
        for tb in range(4):
            if tb + 1 < 4:
                chain(tb + 1)
            trans(tb)

    HT_ALL = ["hT"] + [("hT", tb) for tb in range(4)]

    def layer(self, b, l):
        nc, I, P = self.nc, self.I, self.P
        with ExitStack() as sl:
            self.G = [self.sb(sl, f"Gk{g}", [128, D]) for g in range(2)]
            with ExitStack() as st:
                self._grow_into(st, b, l)
                P.flush()
            with ExitStack() as sm:
                self.hT = self.sb(sm, "hT", [128, 8, L + 2], BF16)
                with ExitStack() as st:
                    self.norm_to_hT(st, b, l, 0)
                    if l == 0 and b == 0 and "hT0" in self.dbg_out:
                        pass
                    P.flush()
                for mi, m in enumerate(("ssd", "na", "ml", "hy")):
                    with ExitStack() as sy:
                        self.yT = self.sb(sy, "yT", [128, 2, L], BF16)
                        if m in self.mixers:
                            getattr(self, "mix_" + m)(b, l)
                        elif self.ycat_in:
                            P.op("sp", lambda e, mi=mi: e.dma_start(out=self.yT[:], in_=I["ycatT"][b, l, :, 2 * mi:2 * mi + 2, :]),
                                 writes=["yT"], dma="yT")
                        else:
                            P.op("dve", lambda e: e.memset(self.yT[:], 0.0), writes=["yT"])
                        with ExitStack() as st:
                            self.wout_part(st, b, l, mi)
                            P.flush()
            with ExitStack() as sm:
                self.hT = self.sb(sm, "hT", [128, 8, L + 2], BF16)
                with ExitStack() as st:
                    self.norm_to_hT(st, b, l, 1)
                    P.flush()
                self.ffn(b, l)

    def _grow_into(self, st, b, l):
        P = self.P
        dg = [self.sb(st, f"g_dg{i}", [128, 128]) for i in range(2)]
        gps = self.ps(st, "g_ps", [128, D])
        n = 0
        for g in range(2):
            c0 = 16 if g == 0 else 40
            for j in range(8):
                d, dk = dg[n % 2], ("g_dg", n % 2)
                n += 1
                P.op("dve", lambda e, d=d, j=j, c0=c0: e.tensor_scalar(
                    out=d[:], in0=self.ident_f[:], scalar1=self.modT[:, l, c0 + j, b:b + 1], scalar2=None, op0=ALU.mult),
                    reads=["ident_f", "modT"], writes=[dk])
                P.op("pe", lambda e, d=d, j=j: e.matmul(gps[:, j * 128:(j + 1) * 128], lhsT=self.ones_f[:], rhs=d[:],
                                                         start=True, stop=True), reads=["ones_f", dk], writes=["g_ps"], sig=(j == 7))
            P.op("act", lambda e, g=g: e.activation(out=self.G[g][:], in_=gps[:], func=AF.Copy), reads=["g_ps"], writes=[("G", g)])

    def wout_part(self, st, b, l, mi):
        I, P = self.I, self.P
        wo = self.sb(st, "wo", [128, 2, D], BF16)
        tmp = [self.sb(st, f"wo_t{i}", [128, 512]) for i in range(2)]
        ops_ = [self.ps(st, f"wo_ps{i}", [128, 512]) for i in range(2)]
        wv = I["w_out"][l, 256 * mi:256 * (mi + 1), :].rearrange("(j p) f -> p j f", p=128)
        P.op("pool", lambda e: e.dma_start(out=wo[:], in_=wv), writes=["wo"], dma="wo")
        n = 0
        for i in range(NT):
            for dh in range(2):
                pt, pk = ops_[n % 2], ("wo_ps", n % 2)
                tt, tk = tmp[n % 2], ("wo_t", n % 2)
                n += 1
                for j in range(2):
                    P.op("pe", lambda e, pt=pt, i=i, j=j, dh=dh: e.matmul(
                        pt[:], lhsT=self.yT[:, j, i * 128:(i + 1) * 128], rhs=wo[:, j, dh * 512:(dh + 1) * 512],
                        start=(j == 0), stop=(j == 1)), reads=["yT", "wo"], writes=[pk], sig=(j == 1))
                P.op("dve", lambda e, pt=pt, tt=tt, dh=dh: e.tensor_tensor(
                    out=tt[:], in0=pt[:], in1=self.G[0][:, dh * 512:(dh + 1) * 512], op=ALU.mult),
                    reads=[pk, ("G", 0)], writes=[tk])
                xs = self.x[:, i, dh * 512:(dh + 1) * 512]
                P.op("pool", lambda e, tt=tt, xs=xs: e.tensor_tensor(out=xs, in0=xs, in1=tt[:], op=ALU.add),
                     reads=[tk, ("x", i)], writes=[("x", i)])

    def ffn(self, b, l):
        I, P = self.I, self.P
        wg_v = I["ffn_w_gate"][l].rearrange("(j p) f -> p j f", p=128)
        wu_v = I["ffn_w_up"][l].rearrange("(j p) f -> p j f", p=128)
        wd_v = I["ffn_w_down"][l].rearrange("(c p) f -> p c f", p=128)
        for half in range(2):
            with ExitStack() as sa:
                act = self.sb(sa, "f_act", [128, NFC, 1024], BF16)
                with ExitStack() as st:
                    wsl = Slots("f_w", [self.sb(st, f"f_w{i}", [128, 2, 8, 128], BF16) for i in range(3)])
                    sg = [self.sb(st, f"f_sg{i}", [128, 512]) for i in range(2)]
                    pg = [self.ps(st, f"f_pg{i}", [128, 512]) for i in range(2)]
                    pu = [self.ps(st, f"f_pu{i}", [128, 512]) for i in range(2)]
                    n = 0
                    for fc in range(NFC):
                        w, wk = wsl.next()
                        P.op("pool", lambda e, w=w, fc=fc: e.dma_start(out=w[:, 0, :, :], in_=wg_v[:, :, fc * 128:(fc + 1) * 128]),
                             writes=[wk], dma=wk)
                        P.op("pool", lambda e, w=w, fc=fc: e.dma_start(out=w[:, 1, :, :], in_=wu_v[:, :, fc * 128:(fc + 1) * 128]),
                             writes=[wk], dma=wk)
                        for q in range(2):
                            t0 = 1 + half * 1024 + q * 512
                            g_, gk = pg[n % 2], ("f_pg", n % 2)
                            u_, uk = pu[n % 2], ("f_pu", n % 2)
                            s_, sk = sg[n % 2], ("f_sg", n % 2)
                            n += 1
                            for gi, (pt, pk) in enumerate(((g_, gk), (u_, uk))):
                                for j in range(8):
                                    P.op("pe", lambda e, pt=pt, w=w, gi=gi, j=j, t0=t0: e.matmul(
                                        pt[:], lhsT=w[:, gi, j, :], rhs=self.hT[:, j, t0:t0 + 512], start=(j == 0), stop=(j == 7)),
                                        reads=[wk] + self.HT_ALL, writes=[pk], sig=(j == 7))
                            P.op("act", lambda e, g_=g_, s_=s_: e.activation(out=s_[:], in_=g_[:], func=AF.Silu),
                                 reads=[gk], writes=[sk])
                            P.op("dve", lambda e, u_=u_, s_=s_, fc=fc, q=q: e.tensor_tensor(
                                out=act[:, fc, q * 512:(q + 1) * 512], in0=u_[:], in1=s_[:], op=ALU.mult),
                                reads=[uk, sk], writes=["f_act"])
                    P.flush()
                with ExitStack() as st:
                    wsl = Slots("f_wd", [self.sb(st, f"f_wd{i}", [128, 512], BF16) for i in range(3)])
                    acc = [self.ps(st, f"f_acc{i}", [128, 512]) for i in range(8)]
                    tmp = [self.sb(st, f"f_t{i}", [128, 512]) for i in range(2)]
                    n = 0
                    for dh in range(2):
                        for fc in range(NFC):
                            w, wk = wsl.next()
                            P.op("pool", lambda e, w=w, fc=fc, dh=dh: e.dma_start(out=w[:], in_=wd_v[:, fc, dh * 512:(dh + 1) * 512]),
                                 writes=[wk], dma=wk)
                            for tt in range(8):
                                P.op("pe", lambda e, w=w, fc=fc, tt=tt: e.matmul(
                                    acc[tt][:], lhsT=act[:, fc, tt * 128:(tt + 1) * 128], rhs=w[:], start=(fc == 0), stop=(fc == NFC - 1)),
                                    reads=[wk, "f_act"], writes=[("f_acc", tt)], sig=(tt == 7 or fc == NFC - 1))
                        for tt in range(8):
                            i = half * 8 + tt
                            t_, tk = tmp[n % 2], ("f_t", n % 2)
                            n += 1
                            P.op("dve", lambda e, t_=t_, tt=tt, dh=dh: e.tensor_tensor(
                                out=t_[:], in0=acc[tt][:], in1=self.G[1][:, dh * 512:(dh + 1) * 512], op=ALU.mult),
                                reads=[("f_acc", tt), ("G", 1)], writes=[tk])
                            xs = self.x[:, i, dh * 512:(dh + 1) * 512]
                            P.op("pool", lambda e, t_=t_, xs=xs: e.tensor_tensor(out=xs, in0=xs, in1=t_[:], op=ALU.add),
                                 reads=[tk, ("x", i)], writes=[("x", i)])
                    P.flush()

    def final(self, b):
        I, P = self.I, self.P
        with ExitStack() as st:
            gb = self.sb(st, "fn_g", [128, D])
            ss = self.sb(st, "fn_ss", [128, NT])
            rs = self.sb(st, "fn_rs", [128, NT])
            junk = self.sb(st, "fn_junk", [128, D], BF16)
            ob = [self.sb(st, f"fn_o{i}", [128, D]) for i in range(2)]
            P.op("sp", lambda e: e.dma_start(out=gb[:], in_=I["final_norm_g"].partition_broadcast(128)), writes=["fn_g"], dma="fn_g")
            for i in range(NT):
                o, ok = ob[i % 2], ("fn_o", i % 2)
                P.op("act", lambda e, i=i: e.activation(out=junk[:], in_=self.x[:, i, :], func=AF.Square, accum_out=ss[:, i:i + 1]),
                     reads=[("x", i)], writes=["fn_junk", ("fn_ss", i)])
                P.op("act", lambda e, i=i: e.activation(out=rs[:, i:i + 1], in_=ss[:, i:i + 1], func=AF.Sqrt, bias=1e-6, scale=1.0 / D),
                     reads=[("fn_ss", i)], writes=[("fn_rs", i)])
                P.op("dve", lambda e, i=i: e.reciprocal(out=rs[:, i:i + 1], in_=rs[:, i:i + 1]), reads=[("fn_rs", i)], writes=[("fn_rs", i)])
                P.op("dve", lambda e, i=i, o=o: e.scalar_tensor_tensor(out=o[:], in0=self.x[:, i, :], scalar=rs[:, i:i + 1],
                                                                       in1=gb[:], op0=ALU.mult, op1=ALU.mult),
                     reads=[("x", i), ("fn_rs", i), "fn_g"], writes=[ok])
                P.op("sp", lambda e, i=i, o=o: e.dma_start(out=self.out[b, i * 128:(i + 1) * 128, :], in_=o[:]),
                     reads=[ok], dma=("out", i % 2))
            P.flush()


def make_in_maps(inp, ncores=8):
    C = _consts()
    g = lambda k: np.ascontiguousarray(np.asarray(inp[k], dtype=np.float32))
    shared = {
        "mod_w": g("mod_w"), "mod_bT": np.ascontiguousarray(g("mod_b").reshape(2, 48, 128).transpose(0, 2, 1)),
        "w_in": g("w_in"), "ssd_conv_w": g("ssd_conv_w"), "ssd_conv_b": g("ssd_conv_b"),
        "ssd_cbT": np.ascontiguousarray(g("ssd_conv_b").reshape(2, 6, 128).transpose(0, 2, 1)), "ssd_dt_bias": g("ssd_dt_bias").reshape(2, 8), "ssd_a_log": g("ssd_a_log").reshape(2, 8), "ssd_d": g("ssd_d"),
        "ssd_norm_g": g("ssd_norm_g"), "na_bias": _na_bias_gather(g("na_rpb")), "na_norm_g": g("na_norm_g"),
        "ml_i_bias": g("ml_i_bias").reshape(2, 8), "ml_f_bias": g("ml_f_bias").reshape(2, 8), "ml_norm_g": g("ml_norm_g"),
        "hy_conv_w": g("hy_conv_w"), "hy_conv_b": g("hy_conv_b"), "hy_w1": g("hy_w1"), "hy_b1": g("hy_b1").reshape(2, 64, 1),
        "hy_w2": g("hy_w2"), "hy_b2": g("hy_b2").reshape(2, 64, 1), "hy_w3": g("hy_w3"), "hy_freq": g("hy_freq").reshape(2, 2, 64, 1),
        "hy_skip": g("hy_skip"), "hy_norm_g": g("hy_norm_g"), "w_out": g("w_out"),
        "ffn_w_gate": g("ffn_w_gate"), "ffn_w_up": g("ffn_w_up"), "ffn_w_down": g("ffn_w_down"), "final_norm_g": g("final_norm_g"),
    }
    for k in ("ident_bf", "ident_f", "ones_f", "lmask", "neg", "na_mask", "featsT", "decay", "FTt", "Ft"):
        shared[k] = C[k]
    x, c = g("x"), g("c")
    maps = []
    for i in range(ncores):
        m = dict(shared)
        m["x"] = np.ascontiguousarray(x[2 * i:2 * i + 2])
        m["cT"] = np.ascontiguousarray(c[2 * i:2 * i + 2].reshape(2, 8, 128).transpose(2, 1, 0))
        maps.append(m)
    return maps


def _phase0_wcv(self):
    I, P = self.I, self.P
    with ExitStack() as st:
        cw = self.sb(st, "cv_cw", [128, 3, 768])
        wf = Slots("cv_wf", [self.sb(st, f"cv_wf{i}", [128, 768]) for i in range(2)])
        ob = Slots("cv_ob", [self.sb(st, f"cv_ob{i}", [128, 768], BF16) for i in range(3)])
        n = 0
        for l in range(2):
            for grp, (cwn, c0) in enumerate((("ssd_conv_w", 256), ("hy_conv_w", OFF_HY))):
                P.op("sp", lambda e, l=l, cwn=cwn: e.dma_start(
                    out=cw[:].rearrange("p a b -> p (a b)"), in_=I[cwn][l].rearrange("a b -> (a b)").partition_broadcast(128)),
                    writes=["cv_cw"], dma="cv_cw")
                for j in range(8):
                    w, wk = wf.next()
                    P.op("sp", lambda e, w=w, l=l, j=j, c0=c0: e.dma_start(out=w[:], in_=I["w_in"][l, j * 128:(j + 1) * 128, c0:c0 + 768]),
                         writes=[wk], dma=wk)
                    for tap in range(3):
                        o, ok = ob.next()
                        q = "dve" if n % 2 == 0 else "pool"
                        n += 1
                        P.op(q, lambda e, o=o, w=w, tap=tap: e.tensor_tensor(out=o[:], in0=w[:], in1=cw[:, tap, :], op=ALU.mult),
                             reads=[wk, "cv_cw"], writes=[ok])
                        P.op("sp", lambda e, o=o, l=l, grp=grp, tap=tap, j=j: e.dma_start(
                            out=self.wcv[l, grp, tap, j * 128:(j + 1) * 128, :], in_=o[:]), reads=[ok], writes=["wcv"], dma=("wcvst", ok[1]))
        P.flush()


def _proj_tm(self, st, l, specs, evac, tiles=range(NT), tok_off=0, tagp="ptm"):
    I, P = self.I, self.P
    mt = 3 if any(sp[0] is not None for sp in specs) else 1
    wsl = Slots(tagp + "w", [self.sb(st, f"{tagp}w{i}", [128, mt, 8, 256], BF16) for i in range(2)])
    pps = [self.ps(st, f"{tagp}p{i}", [128, 256]) for i in range(2)]
    cnt = 0
    for (src, c0, n, tag, bias_ap) in specs:
        w, wk = wsl.next()
        taps = 1 if src is None else 3
        for tap in range(taps):
            if src is None:
                v = I["w_in"][l].rearrange("(j p) c -> p j c", p=128)[:, :, c0:c0 + n]
                P.op("pool", lambda e, w=w, v=v, n=n: e.dma_start(out=w[:, 0, :, :n], in_=v), writes=[wk], dma=wk)
            else:
                v = self.wcv[l, src, tap].rearrange("(j p) c -> p j c", p=128)[:, :, c0:c0 + n]
                P.op("sp", lambda e, w=w, v=v, n=n, tap=tap: e.dma_start(out=w[:, tap, :, :n], in_=v), reads=["wcv"], writes=[wk], dma=wk)
        for i in tiles:
            pt, pk = pps[cnt % 2], (tagp + "p", cnt % 2)
            cnt += 1
            k, tot = 0, taps * 8
            for tap in range(taps):
                sh = 0 if src is None else tap - 1
                t0 = 1 + tok_off + i * 128 + sh
                for j in range(8):
                    last = (k == tot - 1) and bias_ap is None
                    P.op("pe", lambda e, pt=pt, w=w, tap=tap, j=j, t0=t0, n=n, k=k, last=last: e.matmul(
                        pt[:, :n], lhsT=self.hT[:, j, t0:t0 + 128], rhs=w[:, tap, j, :n], start=(k == 0), stop=last),
                        reads=[wk] + self.HT_ALL, writes=[pk], sig=(k == tot - 1))
                    k += 1
            if bias_ap is not None:
                P.op("pe", lambda e, pt=pt, n=n, bias_ap=bias_ap: e.matmul(pt[:, :n], lhsT=self.ones_f[0:1, :], rhs=bias_ap,
                                                                           start=False, stop=True), reads=["ones_f", "cbrow"], writes=[pk])
            evac(tag, i, pt[:, :n], pk)


def _proj_fm(self, st, l, specs, evac, tagp="pfm"):
    I, P = self.I, self.P
    mt = 3 if any(sp[0] is not None for sp in specs) else 1
    wsl = Slots(tagp + "w", [self.sb(st, f"{tagp}w{i}", [128, mt, 8, 128], BF16) for i in range(2)])
    pps = [self.ps(st, f"{tagp}p{i}", [128, 512]) for i in range(2)]
    cnt = 0
    for (src, c0, n, tag) in specs:
        w, wk = wsl.next()
        taps = 1 if src is None else 3
        for tap in range(taps):
            if src is None:
                v = I["w_in"][l].rearrange("(j p) c -> p j c", p=128)[:, :, c0:c0 + n]
                P.op("pool", lambda e, w=w, v=v, n=n: e.dma_start(out=w[:, 0, :, :n], in_=v), writes=[wk], dma=wk)
            else:
                v = self.wcv[l, src, tap].rearrange("(j p) c -> p j c", p=128)[:, :, c0:c0 + n]
                P.op("sp", lambda e, w=w, v=v, n=n, tap=tap: e.dma_start(out=w[:, tap, :, :n], in_=v), reads=["wcv"], writes=[wk], dma=wk)
        for tb in range(4):
            pt, pk = pps[cnt % 2], (tagp + "p", cnt % 2)
            cnt += 1
            k, tot = 0, taps * 8
            for tap in range(taps):
                sh = 0 if src is None else tap - 1
                t0 = 1 + tb * 512 + sh
                for j in range(8):
                    P.op("pe", lambda e, pt=pt, w=w, tap=tap, j=j, t0=t0, n=n, k=k, tot=tot: e.matmul(
                        pt[:n, :], lhsT=w[:, tap, j, :n], rhs=self.hT[:, j, t0:t0 + 512], start=(k == 0), stop=(k == tot - 1)),
                        reads=[wk] + self.HT_ALL, writes=[pk], sig=(k == tot - 1))
                    k += 1
            evac(tag, tb, pt[:n, :], pk)


def _epilogue(self, st, tagp):
    P = self.P
    junk = self.sb(st, tagp + "ej", [128, 256])
    ss = self.sb(st, tagp + "ess", [128, NT])
    rs = self.sb(st, tagp + "ers", [128, NT])
    yb = [self.sb(st, f"{tagp}eyb{i}", [128, 256], BF16) for i in range(3)]
    tp = [self.ps(st, f"{tagp}etp{i}", [128, 256], BF16) for i in range(2)]
    pending = []

    def fn(i, src, skey, grow, gkey):
        y, yk = yb[i % 3], (tagp + "eyb", i % 3)
        t, tk = tp[i % 2], (tagp + "etp", i % 2)
        P.op("act", lambda e: e.activation(out=junk[:], in_=src, func=AF.Square, accum_out=ss[:, i:i + 1]),
             reads=[skey], writes=[tagp + "ej", (tagp + "ess", i)])
        P.op("act", lambda e: e.activation(out=rs[:, i:i + 1], in_=ss[:, i:i + 1], func=AF.Sqrt, bias=1e-6, scale=1.0 / 256),
             reads=[(tagp + "ess", i)], writes=[(tagp + "ers", i)])
        P.op("dve", lambda e: e.reciprocal(out=rs[:, i:i + 1], in_=rs[:, i:i + 1]), reads=[(tagp + "ers", i)], writes=[(tagp + "ers", i)])
        P.op("dve", lambda e: e.scalar_tensor_tensor(out=y[:], in0=src, scalar=rs[:, i:i + 1], in1=grow, op0=ALU.mult, op1=ALU.mult),
             reads=[skey, (tagp + "ers", i), gkey], writes=[yk])
        fn.finish()
        pending.append((i, y, yk, t, tk))

    def finish():
        while pending:
            self.to_yT(*pending.pop(0))
    fn.finish = finish
    return fn


def _to_yT(self, i, y, yk, t, tk):
    P = self.P
    for jj in range(2):
        P.op("pe", lambda e, jj=jj: e.transpose(t[:, jj * 128:(jj + 1) * 128], y[:, jj * 128:(jj + 1) * 128], self.ident_bf[:]),
             reads=[yk, "ident_bf"], writes=[tk], sig=(jj == 1))
    P.op("act", lambda e: e.activation(out=self.yT[:, :, i * 128:(i + 1) * 128], in_=t[:].rearrange("p (a b) -> p a b", a=2), func=AF.Copy),
         reads=[tk], writes=["yT"])


def _sweep(self, st, tagp, nq, pv, sgroups, KT, QT, Ktm, strow, a_ap, build_vd, accum, ufull=False):
    P = self.P
    H = 4
    lm = self.sb(st, tagp + "lm", [128, 4, 128])
    neg = self.sb(st, tagp + "neg", [128, 2, 512], BF16)
    P.op("sp", lambda e: e.dma_start(out=lm[:], in_=self.I["lmask"]), writes=[tagp + "lm"], dma=tagp + "c")
    P.op("sp", lambda e: e.dma_start(out=neg[:], in_=self.I["neg"]), writes=[tagp + "neg"], dma=tagp + "c")
    pvp = ((pv + 7) // 8) * 8
    St = self.sb(st, tagp + "St", [128, 2, H, pvp])[:, :, :, 0:pv]
    Stb = self.sb(st, tagp + "Stb", [128, 2, H, pvp], BF16)[:, :, :, 0:pv]
    P.op("dve", lambda e: e.memset(St[:], 0.0), writes=[tagp + "St0", tagp + "St1"])
    P.op("dve", lambda e: e.memset(Stb[:], 0.0), writes=[tagp + "Stb0", tagp + "Stb1"])
    hv = lambda t: t[:, 0:H * pvp].rearrange("p (h c) -> p h c", h=H)[:, :, 0:pv]
    gps = self.ps(st, tagp + "gps", [128, 512])
    Yps = hv(self.ps(st, tagp + "Yps", [128, 512]))
    Ups = hv(self.ps(st, tagp + "Ups", [128, 512]))
    U2ps = hv(self.ps(st, tagp + "U2ps", [128, 512]))
    Dps_ = [self.ps(st, f"{tagp}Dps{d}", [128, 512]) for d in range(2)]
    Sps_ = [self.ps(st, f"{tagp}Sps{d}", [128, 512]) for d in range(2)]
    egs_ = [self.sb(st, f"{tagp}egs{d}", [128, 16]) for d in range(2)]
    Lm_ = [self.sb(st, f"{tagp}Lm{d}", [128, 4, 128]) for d in range(2)]
    ET_ = [self.sb(st, f"{tagp}ET{d}", [128, 512]) for d in range(2)]
    PT_ = [self.sb(st, f"{tagp}PT{d}", [128, 512], BF16) for d in range(2)]
    Vd_ = [self.sb(st, f"{tagp}Vd{d}", [128, H, pvp], BF16)[:, :, 0:pv] for d in range(2)]
    Vp_ = [self.sb(st, f"{tagp}Vp{d}", [128, H, pvp], BF16)[:, :, 0:pv] for d in range(2)]
    T1_ = [self.sb(st, f"{tagp}T1{d}", [128, H, pvp])[:, :, 0:pv] for d in range(2)]
    ng = len(sgroups)
    rep = H // ng
    def stageA(step, d):
        k = lambda nm, d=d: tagp + nm + str(d)
        kg = lambda nm: tagp + nm
        Dps, Sps, egs, Lm, ET, PT, Vd, Vp, T1 = Dps_[d], Sps_[d], egs_[d], Lm_[d], ET_[d], PT_[d], Vd_[d], Vp_[d], T1_[d]
        c = step if d == 0 else NT - 1 - step
        a = a_ap(c, d)
        mL, mR = lm[:, d, :], lm[:, 2 + d, :]
        P.op("pe", lambda e, mR=mR, a=a: e.matmul(gps[:, 0:4], lhsT=mR, rhs=a, start=True, stop=True), reads=[kg("lm"), "avals"], writes=[kg("gps")], sig=False)
        P.op("pe", lambda e, mL=mL, a=a: e.matmul(gps[:, 4:8], lhsT=mL, rhs=a, start=True, stop=True), reads=[kg("lm"), "avals"], writes=[kg("gps")], sig=False)
        P.op("pe", lambda e, a=a: e.matmul(gps[:, 8:12], lhsT=self.ones_f[:], rhs=a, start=True, stop=True), reads=["ones_f", "avals"], writes=[kg("gps")])
        P.op("act", lambda e, egs=egs: e.activation(out=egs[:, 0:12], in_=gps[:, 0:12], func=AF.Exp), reads=[kg("gps")], writes=[k("egs")])
        P.op("dve", lambda e, mL=mL, a=a, Lm=Lm: e.tensor_tensor(out=Lm[:], in0=mL.unsqueeze(1).broadcast_to([128, 4, 128]),
                                                                 in1=a.unsqueeze(2).broadcast_to([128, 4, 128]), op=ALU.mult),
             reads=[kg("lm"), "avals"], writes=[k("Lm")])
        P.op("pe", lambda e, d=d, Dps=Dps: e.matmul(Dps[:], lhsT=self.ident_bf[:], rhs=neg[:, d, :], start=True, stop=False),
             reads=["ident_bf", kg("neg")], writes=[k("Dps")], sig=False)
        for h in range(H):
            P.op("pe", lambda e, h=h, mR=mR, Dps=Dps, Lm=Lm: e.matmul(Dps[:, h * 128:(h + 1) * 128], lhsT=Lm[:, h, :], rhs=mR, start=False, stop=(h == H - 1)),
                 reads=[k("Lm"), kg("lm")], writes=[k("Dps")], sig=(h == H - 1))
        P.op("act", lambda e, ET=ET, Dps=Dps: e.activation(out=ET[:], in_=Dps[:], func=AF.Exp), reads=[k("Dps")], writes=[k("ET")])
        for gi, (g, heads) in enumerate(sgroups):
            P.op("pe", lambda e, gi=gi, g=g, c=c, Sps=Sps: e.matmul(Sps[:, gi * 128:(gi + 1) * 128], lhsT=KT(g, c), rhs=QT(g, c), start=True, stop=True),
                 reads=["qkT"], writes=[k("Sps")], sig=(gi == ng - 1))
        if rep == 1:
            P.op("dve", lambda e, PT=PT, ET=ET, Sps=Sps: e.tensor_tensor(out=PT[:], in0=ET[:], in1=Sps[:], op=ALU.mult), reads=[k("ET"), k("Sps")], writes=[k("PT")])
        else:
            P.op("dve", lambda e, PT=PT, ET=ET, Sps=Sps: e.tensor_tensor(
                out=PT[:].rearrange("p (g r t) -> p g r t", g=ng, r=rep), in0=ET[:].rearrange("p (g r t) -> p g r t", g=ng, r=rep),
                in1=Sps[:, 0:ng * 128].rearrange("p (g t) -> p g t", g=ng).unsqueeze(2).broadcast_to([128, ng, rep, 128]), op=ALU.mult),
                reads=[k("ET"), k("Sps")], writes=[k("PT")])
        build_vd(c, d, Vd, k("Vd"))
        P.op("pool", lambda e, Vp=Vp, Vd=Vd, egs=egs: e.tensor_tensor(out=Vp[:], in0=Vd[:], in1=egs[:, 4:8].unsqueeze(2).broadcast_to([128, H, pv]), op=ALU.mult),
             reads=[k("Vd"), k("egs")], writes=[k("Vp")])

    def stageB(step, d):
        k = lambda nm, d=d: tagp + nm + str(d)
        kg = lambda nm: tagp + nm
        Dps, Sps, egs, Lm, ET, PT, Vd, Vp, T1 = Dps_[d], Sps_[d], egs_[d], Lm_[d], ET_[d], PT_[d], Vd_[d], Vp_[d], T1_[d]
        c = step if d == 0 else NT - 1 - step
        a = a_ap(c, d)
        mL, mR = lm[:, d, :], lm[:, 2 + d, :]
        for h in range(H):
            P.op("pe", lambda e, h=h, PT=PT, Vd=Vd: e.matmul(Yps[:, h, :], lhsT=PT[:, h * 128:(h + 1) * 128], rhs=Vd[:, h, :], start=True, stop=True),
                 reads=[k("PT"), k("Vd")], writes=[kg("Yps")], sig=(h == H - 1))
        for h in range(H):
            r0, r1 = (0, 128) if ufull else strow(h)
            P.op("pe", lambda e, h=h, c=c, d=d, r0=r0, r1=r1: e.matmul(Ups[:, h, :], lhsT=QT(h if ng == H else h // rep, c), rhs=Stb[r0:r1, d, h, :],
                                                                       start=True, stop=True), reads=["qkT", k("Stb")], writes=[kg("Ups")], sig=(h == H - 1))
        P.op("dve", lambda e, T1=T1, egs=egs: e.tensor_tensor(out=T1[:], in0=Ups[:], in1=egs[:, 0:4].unsqueeze(2).broadcast_to([128, H, pv]), op=ALU.mult),
             reads=[kg("Ups"), k("egs")], writes=[k("T1")])
        P.op("dve", lambda e, T1=T1: e.tensor_tensor(out=T1[:], in0=T1[:], in1=Yps[:], op=ALU.add), reads=[k("T1"), kg("Yps")], writes=[k("T1")])
        accum(c, d, T1, k("T1"))
        for h in range(H):
            P.op("pe", lambda e, h=h, c=c, Vp=Vp: e.matmul(U2ps[:, h, :], lhsT=Ktm(h, c), rhs=Vp[:, h, :], start=True, stop=True),
                 reads=["ktm", k("Vp")], writes=[kg("U2ps")], sig=(h == H - 1))
        rows = sorted(set(strow(h) for h in range(H)))
        for (r0, r1) in rows:
            hs = [h for h in range(H) if strow(h) == (r0, r1)]
            h0, hstep = hs[0], (hs[1] - hs[0] if len(hs) > 1 else 1)
            sl = slice(h0, hs[-1] + 1, hstep)
            P.op("pool", lambda e, r0=r0, r1=r1, sl=sl, d=d, nh=len(hs), egs=egs: e.tensor_tensor(
                out=St[r0:r1, d, sl, :], in0=St[r0:r1, d, sl, :], in1=egs[r0:r1, 8:12][:, sl].unsqueeze(2).broadcast_to([r1 - r0, nh, pv]), op=ALU.mult),
                reads=[k("St"), k("egs")], writes=[k("St")])
            P.op("dve", lambda e, r0=r0, r1=r1, sl=sl, d=d: e.tensor_tensor(out=St[r0:r1, d, sl, :], in0=St[r0:r1, d, sl, :], in1=U2ps[r0:r1, sl, :], op=ALU.add),
                 reads=[k("St"), kg("U2ps")], writes=[k("St")])
            P.op("act", lambda e, r0=r0, r1=r1, sl=sl, d=d: e.activation(out=Stb[r0:r1, d, sl, :], in_=St[r0:r1, d, sl, :], func=AF.Copy),
                 reads=[k("St")], writes=[k("Stb")])


    items = [(step, d) for step in range(NT) for d in range(2)]
    stageA(*items[0])
    for n, it in enumerate(items):
        if n + 1 < len(items):
            stageA(*items[n + 1])
        stageB(*it)


def _mix_ssd(self, b, l):
    I, P = self.I, self.P
    with ExitStack() as sm:
        xs = self.sb(sm, "s_xs", [128, NT, 256], BF16)
        Btm = self.sb(sm, "s_Btm", [128, NT, 256], BF16)
        BT = self.sb(sm, "s_BT", [128, 2, L], BF16)
        CT = self.sb(sm, "s_CT", [128, 2, L], BF16)
        dt = self.sb(sm, "s_dt", [128, NT, 8])
        av = self.sb(sm, "s_av", [128, NT, 8])
        cbT = self.sb(sm, "s_cbT", [128, 6])
        cbrow = self.sb(sm, "s_cbrow", [1, 768])
        dtb = self.sb(sm, "s_dtb", [128, 8])
        Arow = self.sb(sm, "s_Arow", [128, 8])
        Drow = self.sb(sm, "s_Drow", [128, 4])
        grow = self.sb(sm, "s_grow", [128, 256])
        Yacc = self.sb(sm, "s_Yacc", [128, NT, 256])
        P.op("sp", lambda e: e.dma_start(out=cbT[:], in_=I["ssd_cbT"][l]), writes=["s_cbT"], dma="s_c")
        P.op("sp", lambda e: e.dma_start(out=cbrow[:], in_=I["ssd_conv_b"][l:l + 1, :]), writes=["cbrow"], dma="s_c")
        P.op("sp", lambda e: e.dma_start(out=dtb[:], in_=I["ssd_dt_bias"][l].partition_broadcast(128)), writes=["s_dtb"], dma="s_c")
        P.op("sp", lambda e: e.dma_start(out=Arow[:], in_=I["ssd_a_log"][l].partition_broadcast(128)), writes=["s_Arow"], dma="s_c")
        P.op("sp", lambda e: e.dma_start(out=Drow[:], in_=I["ssd_d"][l].partition_broadcast(128)), writes=["s_Drow"], dma="s_c")
        P.op("sp", lambda e: e.dma_start(out=grow[:], in_=I["ssd_norm_g"][l].partition_broadcast(128)), writes=["s_grow"], dma="s_c")
        P.op("act", lambda e: e.activation(out=Arow[:], in_=Arow[:], func=AF.Exp), reads=["s_Arow"], writes=["s_Arow"])
        P.op("dve", lambda e: e.tensor_scalar(out=Arow[:], in0=Arow[:], scalar1=-1.0, scalar2=None, op0=ALU.mult), reads=["s_Arow"], writes=["s_Arow"])
        with ExitStack() as st:
            def ev_tm(tag, i, ps, pk):
                if tag == "x":
                    P.op("act", lambda e: e.activation(out=xs[:, i, :], in_=ps, func=AF.Silu), reads=[pk], writes=["s_xs"])
                elif tag == "B":
                    P.op("act", lambda e: e.activation(out=Btm[:, i, :], in_=ps, func=AF.Silu), reads=[pk], writes=["ktm"])
                else:
                    P.op("dve", lambda e: e.tensor_tensor(out=dt[:, i, :], in0=ps, in1=dtb[:], op=ALU.add), reads=[pk, "s_dtb"], writes=["s_dt"])
            self.proj_tm(st, l, [(0, 0, 256, "x", cbrow[0:1, 0:256]), (0, 256, 256, "B", cbrow[0:1, 256:512]),
                                 (None, 1024, 8, "dt", None)], ev_tm)

            def ev_fm(tag, tb, ps, pk):
                dst = (BT if tag < 2 else CT)[:, tag % 2, tb * 512:(tb + 1) * 512]
                P.op("act", lambda e: e.activation(out=dst, in_=ps, func=AF.Silu, bias=cbT[:, 2 + tag:3 + tag]), reads=[pk, "s_cbT"], writes=["qkT"])
            self.proj_fm(st, l, [(0, 256 + 128 * t, 128, t) for t in range(4)], ev_fm)
            P.op("act", lambda e: e.activation(out=av[:], in_=dt[:], func=AF.Exp), reads=["s_dt"], writes=["avals"])
            P.op("act", lambda e: e.activation(out=dt[:], in_=av[:], func=AF.Ln, bias=1.0), reads=["avals"], writes=["s_dt"])
            P.op("dve", lambda e: e.tensor_tensor(out=av[:], in0=dt[:], in1=Arow[:].unsqueeze(1).broadcast_to([128, NT, 8]), op=ALU.mult),
                 reads=["s_dt", "s_Arow"], writes=["avals"])
            P.op("dve", lambda e: e.tensor_tensor(out=Yacc[:].rearrange("p i (h c) -> p i h c", h=4), in0=xs[:].rearrange("p i (h c) -> p i h c", h=4),
                                                  in1=Drow[:].unsqueeze(1).unsqueeze(3).broadcast_to([128, NT, 4, 64]), op=ALU.mult),
                 reads=["s_xs", "s_Drow"], writes=["s_Yacc"])
            P.flush()
        with ExitStack() as st:
            def build_vd(c, d, Vd, vk):
                P.op("dve", lambda e: e.tensor_tensor(out=Vd[:], in0=xs[:, c, :].rearrange("p (h c) -> p h c", h=4),
                                                      in1=dt[:, c, 4 * d:4 * d + 4].unsqueeze(2).broadcast_to([128, 4, 64]), op=ALU.mult),
                     reads=["s_xs", "s_dt"], writes=[vk])

            def accum(c, d, T1, tk):
                P.op("pool", lambda e: e.tensor_tensor(out=Yacc[:, c, :], in0=Yacc[:, c, :], in1=T1[:].rearrange("p h c -> p (h c)"), op=ALU.add),
                     reads=[tk, "s_Yacc"], writes=["s_Yacc"])
            self.sweep(st, "ss_", 128, 64, [(0, [0, 1]), (1, [2, 3])],
                       KT=lambda g, c: BT[:, g, c * 128:(c + 1) * 128], QT=lambda g, c: CT[:, g, c * 128:(c + 1) * 128],
                       Ktm=lambda h, c: Btm[:, c, (h // 2) * 128:(h // 2 + 1) * 128], strow=lambda h: (0, 128),
                       a_ap=lambda c, d: av[:, c, 4 * d:4 * d + 4], build_vd=build_vd, accum=accum)
            P.flush()
        with ExitStack() as st:
            epi = self.epilogue(st, "s_")
            zt = [self.sb(st, f"s_zt{i}", [128, 256]) for i in range(3)]

            def ev_z(tag, i, ps, pk):
                z, zk = zt[i % 3], ("s_zt", i % 3)
                P.op("act", lambda e: e.activation(out=z[:], in_=ps, func=AF.Silu), reads=[pk], writes=[zk])
                P.op("dve", lambda e: e.tensor_tensor(out=z[:], in0=z[:], in1=Yacc[:, i, :], op=ALU.mult), reads=[zk, "s_Yacc"], writes=[zk])
                epi(i, z[:], zk, grow[:], "s_grow")
            self.proj_tm(st, l, [(None, 0, 256, "z", None)], ev_z, tagp="pz")
            epi.finish()
            P.flush()


KB.phase0_wcv = _phase0_wcv
KB.proj_tm = _proj_tm
KB.proj_fm = _proj_fm
KB.epilogue = _epilogue
KB.to_yT = _to_yT
KB.sweep = _sweep
KB.mix_ssd = _mix_ssd


def _mix_ml(self, b, l):
    I, P = self.I, self.P
    base = OFF_ML
    with ExitStack() as sm:
        qT = self.sb(sm, "m_qT", [128, 2, L], BF16)
        kT = self.sb(sm, "m_kT", [128, 4, L], BF16)
        P.op("pool", lambda e: e.memset(kT[:], 0.0), writes=["qkT"])
        Ktm = self.sb(sm, "m_Ktm", [128, NT, 256], BF16)
        vtm = self.sb(sm, "m_vtm", [128, NT, 256], BF16)
        gt = self.sb(sm, "m_gt", [128, NT, 16])
        ei = self.sb(sm, "m_ei", [128, NT, 8])
        av = self.sb(sm, "m_av", [128, NT, 8])
        ib = self.sb(sm, "m_ib", [128, 8])
        fb = self.sb(sm, "m_fb", [128, 8])
        grow = self.sb(sm, "m_grow", [128, 256])
        Hacc = self.sb(sm, "m_Hacc", [128, NT, 256])
        P.op("sp", lambda e: e.dma_start(out=ib[:], in_=I["ml_i_bias"][l].partition_broadcast(128)), writes=["m_ib"], dma="m_c")
        P.op("sp", lambda e: e.dma_start(out=fb[:], in_=I["ml_f_bias"][l].partition_broadcast(128)), writes=["m_fb"], dma="m_c")
        P.op("sp", lambda e: e.dma_start(out=grow[:], in_=I["ml_norm_g"][l].partition_broadcast(128)), writes=["m_grow"], dma="m_c")
        P.op("pool", lambda e: e.memset(Hacc[:], 0.0), writes=["m_Hacc"])
        with ExitStack() as st:
            def ev_fm(tag, tb, ps, pk):
                sl = slice(tb * 512, (tb + 1) * 512)
                if tag < 2:
                    P.op("act", lambda e: e.activation(out=qT[:, tag, sl], in_=ps, func=AF.Copy), reads=[pk], writes=["qkT"])
                else:
                    pair = tag - 2
                    P.op("act", lambda e: e.activation(out=kT[0:64, 2 * pair, sl], in_=ps[0:64, :], func=AF.Copy), reads=[pk], writes=["qkT"])
                    P.op("dve", lambda e: e.tensor_copy(out=kT[64:128, 2 * pair + 1, sl], in_=ps[64:128, :]), reads=[pk], writes=["qkT"])
            self.proj_fm(st, l, [(None, base + 128 * t, 128, t) for t in range(4)], ev_fm)

            def ev_tm(tag, i, ps, pk):
                if tag == "k":
                    P.op("act", lambda e: e.activation(out=Ktm[:, i, :], in_=ps, func=AF.Copy), reads=[pk], writes=["ktm"])
                elif tag == "v":
                    P.op("dve", lambda e: e.tensor_copy(out=vtm[:, i, :], in_=ps), reads=[pk], writes=["m_vtm"])
                else:
                    P.op("dve", lambda e: e.tensor_copy(out=gt[:, i, :], in_=ps), reads=[pk], writes=["m_gt"])
            self.proj_tm(st, l, [(None, base + 256, 256, "k", None), (None, base + 512, 256, "v", None),
                                 (None, base + 1024, 16, "g", None)], ev_tm)
            P.op("dve", lambda e: e.tensor_tensor(out=ei[:], in0=gt[:, :, 0:8], in1=ib[:].unsqueeze(1).broadcast_to([128, NT, 8]), op=ALU.add),
                 reads=["m_gt", "m_ib"], writes=["m_ei"])
            P.op("act", lambda e: e.activation(out=ei[:], in_=ei[:], func=AF.Exp), reads=["m_ei"], writes=["m_ei"])
            P.op("dve", lambda e: e.tensor_scalar(out=ei[:], in0=ei[:], scalar1=0.125, scalar2=None, op0=ALU.mult), reads=["m_ei"], writes=["m_ei"])
            P.op("dve", lambda e: e.tensor_tensor(out=av[:], in0=gt[:, :, 8:16], in1=fb[:].unsqueeze(1).broadcast_to([128, NT, 8]), op=ALU.add),
                 reads=["m_gt", "m_fb"], writes=["avals"])
            P.op("act", lambda e: e.activation(out=av[:], in_=av[:], func=AF.Exp, scale=-1.0), reads=["avals"], writes=["avals"])
            P.op("act", lambda e: e.activation(out=av[:], in_=av[:], func=AF.Ln, bias=1.0), reads=["avals"], writes=["avals"])
            P.op("dve", lambda e: e.tensor_scalar(out=av[:], in0=av[:], scalar1=-1.0, scalar2=None, op0=ALU.mult), reads=["avals"], writes=["avals"])
            P.flush()
        with ExitStack() as st:
            dd_ = [self.sb(st, f"m_dd{i}", [128, 4, 1]) for i in range(2)]
            hh_ = [self.sb(st, f"m_hh{i}", [128, 4, 64]) for i in range(2)]

            def build_vd(c, d, Vd, vk):
                P.op("dve", lambda e: e.tensor_tensor(out=Vd[:, :, 0:64], in0=vtm[:, c, :].rearrange("p (h c) -> p h c", h=4),
                                                      in1=ei[:, c, 4 * d:4 * d + 4].unsqueeze(2).broadcast_to([128, 4, 64]), op=ALU.mult),
                     reads=["m_vtm", "m_ei"], writes=[vk])
                P.op("act", lambda e: e.activation(out=Vd[:, :, 64:65], in_=ei[:, c, 4 * d:4 * d + 4].unsqueeze(2), func=AF.Copy),
                     reads=["m_ei"], writes=[vk])

            def accum(c, d, T1, tk):
                dd, hh, dk, hk = dd_[d], hh_[d], f"m_dd{d}", f"m_hh{d}"
                den = T1[:, :, 64:65]
                P.op("dve", lambda e: e.scalar_tensor_tensor(out=dd[:], in0=den, scalar=-1.0, in1=den, op0=ALU.mult, op1=ALU.max),
                     reads=[tk], writes=[dk])
                P.op("dve", lambda e: e.tensor_scalar_max(out=dd[:], in0=dd[:], scalar1=1.0), reads=[dk], writes=[dk])
                P.op("dve", lambda e: e.reciprocal(out=dd[:], in_=dd[:]), reads=[dk], writes=[dk])
                P.op("dve", lambda e: e.tensor_tensor(out=hh[:], in0=T1[:, :, 0:64], in1=dd[:].broadcast_to([128, 4, 64]), op=ALU.mult),
                     reads=[tk, dk], writes=[hk])
                P.op("pool", lambda e: e.tensor_tensor(out=Hacc[:, c, :], in0=Hacc[:, c, :], in1=hh[:].rearrange("p h c -> p (h c)"), op=ALU.add),
                     reads=[hk, "m_Hacc"], writes=["m_Hacc"])
            pr = lambda h: ((h % 2) * 64, (h % 2) * 64 + 64)
            self.sweep(st, "ms_", 64, 65, [(h, [h]) for h in range(4)],
                       KT=lambda h, c: kT[:, h, c * 128:(c + 1) * 128],
                       QT=lambda h, c: qT[:, h // 2, c * 128:(c + 1) * 128],
                       Ktm=lambda h, c: Ktm[:, c, (h // 2) * 128:(h // 2 + 1) * 128], strow=pr,
                       a_ap=lambda c, d: av[:, c, 4 * d:4 * d + 4], build_vd=build_vd, accum=accum, ufull=True)
            P.flush()
        with ExitStack() as st:
            junk = self.sb(st, "m_junk", [128, 256])
            ss4 = self.sb(st, "m_ss4", [128, NT, 4])
            sg = [self.sb(st, f"m_sg{i}", [128, 256]) for i in range(2)]
            yb = [self.sb(st, f"m_yb{i}", [128, 256], BF16) for i in range(3)]
            mpend = []
            tp = [self.ps(st, f"m_tp{i}", [128, 256], BF16) for i in range(2)]

            def ev_o(tag, i, ps, pk):
                s, sk = sg[i % 2], ("m_sg", i % 2)
                y, yk = yb[i % 3], ("m_yb", i % 3)
                P.op("act", lambda e: e.activation(out=s[:], in_=ps, func=AF.Sigmoid), reads=[pk], writes=[sk])
                P.op("act", lambda e: e.activation(out=junk[:], in_=Hacc[:, i, :], func=AF.Square), reads=["m_Hacc"], writes=["m_junk"])
                P.op("dve", lambda e: e.reduce_sum(out=ss4[:, i, :], in_=junk[:].rearrange("p (h c) -> p h c", h=4), axis=AX.X),
                     reads=["m_junk"], writes=[("m_ss4", i)])
                P.op("act", lambda e: e.activation(out=ss4[:, i, :], in_=ss4[:, i, :], func=AF.Sqrt, bias=1e-6, scale=1.0 / 64),
                     reads=[("m_ss4", i)], writes=[("m_ss4", i)])
                P.op("dve", lambda e: e.reciprocal(out=ss4[:, i, :], in_=ss4[:, i, :]), reads=[("m_ss4", i)], writes=[("m_ss4", i)])
                P.op("dve", lambda e: e.tensor_tensor(out=s[:], in0=s[:], in1=grow[:], op=ALU.mult), reads=[sk, "m_grow"], writes=[sk])
                P.op("dve", lambda e: e.tensor_tensor(out=s[:].rearrange("p (h c) -> p h c", h=4), in0=s[:].rearrange("p (h c) -> p h c", h=4),
                                                      in1=ss4[:, i, :].unsqueeze(2).broadcast_to([128, 4, 64]), op=ALU.mult),
                     reads=[sk, ("m_ss4", i)], writes=[sk])
                P.op("dve", lambda e: e.tensor_tensor(out=y[:], in0=s[:], in1=Hacc[:, i, :], op=ALU.mult), reads=[sk, "m_Hacc"], writes=[yk])
                while mpend:
                    self.to_yT(*mpend.pop(0))
                mpend.append((i, y, yk, tp[i % 2], ("m_tp", i % 2)))
            self.proj_tm(st, l, [(None, base + 768, 256, "o", None)], ev_o, tagp="po")
            while mpend:
                self.to_yT(*mpend.pop(0))
            P.flush()


def _mix_na(self, b, l):
    I, P = self.I, self.P
    base = OFF_NA
    with ExitStack() as sm:
        qT = self.sb(sm, "a_qT", [128, 2, L], BF16)
        kT = self.sb(sm, "a_kT", [128, 2, L], BF16)
        Ve = self.sb(sm, "a_Ve", [128, NT, 256], BF16)
        Vo = self.sb(sm, "a_Vo", [128, NT - 1, 256], BF16)
        yna = self.sb(sm, "a_y", [128, NT, 256])
        mask = self.sb(sm, "a_mask", [128, 512], BF16)
        grow = self.sb(sm, "a_grow", [128, 256])
        P.op("sp", lambda e: e.dma_start(out=mask[:], in_=I["na_mask"]), writes=["a_mask"], dma="a_c")
        P.op("sp", lambda e: e.dma_start(out=grow[:], in_=I["na_norm_g"][l].partition_broadcast(128)), writes=["a_grow"], dma="a_c")
        with ExitStack() as st:
            def ev_fm(tag, tb, ps, pk):
                dst = (qT if tag < 2 else kT)[:, tag % 2, tb * 512:(tb + 1) * 512]
                P.op("act", lambda e: e.activation(out=dst, in_=ps, func=AF.Copy, scale=(0.125 if tag < 2 else 1.0)), reads=[pk], writes=["a_qk"])
            self.proj_fm(st, l, [(None, base + 128 * t, 128, t) for t in range(4)], ev_fm)
            self.proj_tm(st, l, [(None, base + 512, 256, "v", None)],
                         lambda tag, i, ps, pk: P.op("act", lambda e: e.activation(out=Ve[:, i, :], in_=ps, func=AF.Copy), reads=[pk], writes=["a_V"]),
                         tagp="pve")
            P.flush()
        with ExitStack() as st:
            self.proj_tm(st, l, [(None, base + 512, 256, "v", None)],
                         lambda tag, i, ps, pk: P.op("act", lambda e: e.activation(out=Vo[:, i, :], in_=ps, func=AF.Copy), reads=[pk], writes=["a_V"]),
                         tiles=range(NT - 1), tok_off=64, tagp="pvo")
            P.flush()
        with ExitStack() as st:
            bsl = Slots("a_b", [self.sb(st, f"a_b{i}", [128, 4, 512], BF16) for i in range(2)])
            NB = 3
            Sps = [self.ps(st, f"a_S{i}", [128, 512]) for i in range(NB)]
            PTall = self.ps(st, "a_PTall", [128, 2048], BF16)
            PTp = [PTall[:, i * 512:(i + 1) * 512] for i in range(NB)]
            Oall = self.ps(st, "a_Oall", [128, 512])
            Ops = [Oall[:, i * 64:(i + 1) * 64] for i in range(NB)]
            Pb = [self.sb(st, f"a_P{i}", [128, 512], BF16) for i in range(NB)]
            PTs = [self.sb(st, f"a_PT{i}", [128, 512], BF16) for i in range(NB)]
            nmx = self.sb(st, "a_nmx", [128, NB])
            rsum = self.sb(st, "a_rs", [128, NB])
            n = 0
            last_dl, bt, bk = None, None, None
            for r in range(32):
                r0 = min(max(r - 4, 0), 24)
                dl = r0 - r + 7
                if dl != last_dl:
                    bt, bk = bsl.next()
                    P.op("pool", lambda e, bt=bt, dl=dl: e.dma_start(out=bt[:], in_=I["na_bias"][l, dl]), writes=[bk], dma=bk)
                    last_dl = dl
                ti, prow = r // 2, (r % 2) * 64
                Vt, v0 = (Ve, r0 // 2) if r0 % 2 == 0 else (Vo, (r0 - 1) // 2)
                for h in range(4):
                    p0 = (h % 2) * 64
                    u = n % NB
                    n += 1
                    S, Sk = Sps[u], ("a_S", u)
                    P.op("pe", lambda e, S=S, p0=p0, h=h, ti=ti, r0=r0: e.matmul(
                        S[:], lhsT=qT[p0:p0 + 64, h // 2, ti * 128:(ti + 1) * 128], rhs=kT[p0:p0 + 64, h // 2, 64 * r0:64 * r0 + 512],
                        start=True, stop=False), reads=["a_qk"], writes=[Sk], sig=False)
                    P.op("pe", lambda e, S=S, bt=bt, h=h: e.matmul(S[:], lhsT=self.ident_bf[:], rhs=bt[:, h, :], start=False, stop=False),
                         reads=["ident_bf", bk], writes=[Sk], sig=False)
                    P.op("pe", lambda e, S=S: e.matmul(S[:], lhsT=self.ident_bf[:], rhs=mask[:], start=False, stop=True),
                         reads=["ident_bf", "a_mask"], writes=[Sk])
                    P.op("dve", lambda e, S=S, u=u: e.reduce_max(out=nmx[:, u:u + 1], in_=S[:], axis=AX.X, negate=True), reads=[Sk], writes=[("a_nmx", u)])
                    P.op("act", lambda e, S=S, u=u: e.activation(out=Pb[u][:], in_=S[:], func=AF.Exp, bias=nmx[:, u:u + 1], accum_out=rsum[:, u:u + 1]),
                         reads=[Sk, ("a_nmx", u)], writes=[("a_P", u), ("a_rs", u)])
                    for kb in range(4):
                        P.op("pe", lambda e, u=u, kb=kb: e.transpose(PTp[u][:, kb * 128:(kb + 1) * 128], Pb[u][:, kb * 128:(kb + 1) * 128], self.ident_bf[:]),
                             reads=[("a_P", u), "ident_bf"], writes=[("a_PTp", u)], sig=(kb == 3))
                    P.op("dve", lambda e, u=u: e.tensor_copy(out=PTs[u][:], in_=PTp[u]), reads=[("a_PTp", u)], writes=[("a_PT", u)])
                    for kb in range(4):
                        P.op("pe", lambda e, u=u, kb=kb, Vt=Vt, v0=v0, h=h: e.matmul(
                            Ops[u], lhsT=PTs[u][:, kb * 128:(kb + 1) * 128], rhs=Vt[:, v0 + kb, h * 64:(h + 1) * 64], start=(kb == 0), stop=(kb == 3)),
                            reads=[("a_PT", u), "a_V"], writes=[("a_O", u)], sig=(kb == 3))
                    P.op("dve", lambda e, u=u: e.reciprocal(out=rsum[:, u:u + 1], in_=rsum[:, u:u + 1]), reads=[("a_rs", u)], writes=[("a_rs", u)])
                    P.op("act", lambda e, u=u, prow=prow, ti=ti, h=h: e.activation(
                        out=yna[prow:prow + 64, ti, h * 64:(h + 1) * 64], in_=Ops[u][prow:prow + 64, :], func=AF.Copy, scale=rsum[prow:prow + 64, u:u + 1]),
                        reads=[("a_O", u), ("a_rs", u)], writes=["a_y"])
            P.flush()
        with ExitStack() as st:
            epi = self.epilogue(st, "a_")
            for i in range(NT):
                epi(i, yna[:, i, :], "a_y", grow[:], "a_grow")
            epi.finish()
            P.flush()


KB.mix_ml = _mix_ml
KB.mix_na = _mix_na


def _phase0_hyena(self):
    I, P = self.I, self.P
    M = 12582912.0
    for l in range(2):
        with ExitStack() as sm:
            hn = self.sb(sm, "h0_hn", [128, NT, 1024], BF16)
            with ExitStack() as st:
                featsT = self.sb(st, "h0_f", [33, L])
                w1 = self.sb(st, "h0_w1", [33, 64]); w2 = self.sb(st, "h0_w2", [64, 64]); w3 = self.sb(st, "h0_w3", [64, 1024])
                bb = self.sb(st, "h0_bb", [64, 2]); fq = self.sb(st, "h0_fq", [64, 2])
                dec = self.sb(st, "h0_dec", [128, NT, 256])
                hT_ = [self.sb(st, f"h0_hT{i}", [64, L]) for i in range(2)]
                arg = self.sb(st, "h0_arg", [64, 512]); t1 = self.sb(st, "h0_t1", [64, 512])
                hw = self.sb(st, "h0_hw", [128, NT, 1024])
                sq = self.sb(st, "h0_sq", [128, 1024])
                rn = self.sb(st, "h0_rn", [128, 2, 256])
                mp = self.ps(st, "h0_mp", [64, 512])
                hp = self.ps(st, "h0_hp", [128, 1024])
                ssq = self.ps(st, "h0_ssq", [128, 1024])
                ld = lambda t, src, key: P.op("sp", lambda e: e.dma_start(out=t, in_=src), writes=[key], dma="h0c")
                ld(featsT[:], I["featsT"], "h0_f"); ld(w1[:], I["hy_w1"][l], "h0_w"); ld(w2[:], I["hy_w2"][l], "h0_w"); ld(w3[:], I["hy_w3"][l], "h0_w")
                ld(bb[:, 0:1], I["hy_b1"][l], "h0_w"); ld(bb[:, 1:2], I["hy_b2"][l], "h0_w")
                ld(fq[:, 0:1], I["hy_freq"][l, 0], "h0_w"); ld(fq[:, 1:2], I["hy_freq"][l, 1], "h0_w"); ld(dec[:], I["decay"], "h0_dec")
                for layer in range(2):
                    for tb in range(4):
                        sl = slice(tb * 512, (tb + 1) * 512)
                        if layer == 0:
                            P.op("pe", lambda e, sl=sl: e.matmul(mp[:], lhsT=w1[:], rhs=featsT[:, sl], start=True, stop=True), reads=["h0_w", "h0_f"], writes=["h0_mp"])
                        else:
                            P.op("pe", lambda e, sl=sl: e.matmul(mp[:], lhsT=w2[:], rhs=hT_[0][:, sl], start=True, stop=True), reads=["h0_w", "h0_hT0"], writes=["h0_mp"])
                        P.op("dve", lambda e, layer=layer: e.tensor_scalar(out=arg[:], in0=mp[:], scalar1=bb[:, layer:layer + 1], scalar2=fq[:, layer:layer + 1],
                                                                            op0=ALU.add, op1=ALU.mult), reads=["h0_mp", "h0_w"], writes=["h0_arg"])
                        P.op("dve", lambda e: e.tensor_scalar(out=t1[:], in0=arg[:], scalar1=1.0 / (2 * PI), scalar2=M, op0=ALU.mult, op1=ALU.add),
                             reads=["h0_arg"], writes=["h0_t1"])
                        P.op("dve", lambda e: e.tensor_scalar(out=t1[:], in0=t1[:], scalar1=-M, scalar2=-2 * PI, op0=ALU.add, op1=ALU.mult),
                             reads=["h0_t1"], writes=["h0_t1"])
                        P.op("dve", lambda e: e.tensor_tensor(out=arg[:], in0=arg[:], in1=t1[:], op=ALU.add), reads=["h0_arg", "h0_t1"], writes=["h0_arg"])
                        P.op("act", lambda e, layer=layer, sl=sl: e.activation(out=hT_[layer][:, sl], in_=arg[:], func=AF.Sin), reads=["h0_arg"], writes=[f"h0_hT{layer}"])
                for tt in range(NT):
                    for half in range(2):
                        P.op("pe", lambda e, tt=tt, half=half: e.matmul(hp[:, half * 512:(half + 1) * 512], lhsT=hT_[1][:, tt * 128:(tt + 1) * 128],
                                                                        rhs=w3[:, half * 512:(half + 1) * 512], start=True, stop=True),
                             reads=["h0_hT1", "h0_w"], writes=["h0_hp"], sig=(half == 1))
                    P.op("dve", lambda e, tt=tt: e.tensor_tensor(out=hw[:, tt, :].rearrange("p (a c) -> p a c", a=4), in0=hp[:].rearrange("p (a c) -> p a c", a=4),
                                                                  in1=dec[:, tt, :].unsqueeze(1).broadcast_to([128, 4, 256]), op=ALU.mult),
                         reads=["h0_hp", "h0_dec"], writes=["h0_hw"])
                    P.op("act", lambda e, tt=tt: e.activation(out=sq[:], in_=hw[:, tt, :], func=AF.Square), reads=["h0_hw"], writes=["h0_sq"])
                    for half in range(2):
                        P.op("pe", lambda e, tt=tt, half=half: e.matmul(ssq[:, half * 512:(half + 1) * 512], lhsT=self.ones_f[:], rhs=sq[:, half * 512:(half + 1) * 512],
                                                                        start=(tt == 0), stop=(tt == NT - 1)), reads=["ones_f", "h0_sq"], writes=["h0_ssq"], sig=(half == 1))
                P.op("act", lambda e: e.activation(out=sq[:], in_=ssq[:], func=AF.Copy), reads=["h0_ssq"], writes=["h0_sq"])
                sv = sq[:].rearrange("p (o d c) -> p o d c", o=2, d=2)
                P.op("dve", lambda e: e.tensor_tensor(out=rn[:], in0=sv[:, :, 0, :], in1=sv[:, :, 1, :], op=ALU.add), reads=["h0_sq"], writes=["h0_rn"])
                P.op("act", lambda e: e.activation(out=rn[:], in_=rn[:], func=AF.Sqrt, bias=1e-6), reads=["h0_rn"], writes=["h0_rn"])
                P.op("dve", lambda e: e.reciprocal(out=rn[:], in_=rn[:]), reads=["h0_rn"], writes=["h0_rn"])
                for tt in range(NT):
                    P.op("dve" if tt % 2 else "pool", lambda e, tt=tt: e.tensor_tensor(
                        out=hn[:, tt, :].rearrange("p (o d c) -> p o d c", o=2, d=2), in0=hw[:, tt, :].rearrange("p (o d c) -> p o d c", o=2, d=2),
                        in1=rn[:].unsqueeze(2).broadcast_to([128, 2, 2, 256]), op=ALU.mult), reads=["h0_hw", "h0_rn"], writes=["h0_hn"])
                P.op("dve", lambda e: e.memset(hn[0:1, 0, :].rearrange("p (o d c) -> p o d c", o=2, d=2)[:, :, 1, :], 0.0), reads=["h0_hn"], writes=["h0_hn"])
                if l == 0:
                    self.dump("hn", hn[:], "h0_hn")
                    self.dump("hw", hw[:], "h0_hw")
                    self.dump("h2T", hT_[1][:], "h0_hT1")
                P.flush()
            with ExitStack() as st:
                fsl = Slots("h0_F", [self.sb(st, f"h0_F{i}", [128, NT, 128], BF16) for i in range(3)])
                KP = [self.ps(st, f"h0_KP{i}", [128, 1024]) for i in range(2)]
                ksb = [self.sb(st, f"h0_ks{i}", [128, 1024]) for i in range(2)]
                tab = self.sb(st, "h0_tab", [128, 2, 3, 256])
                sc = 2.0 / 4096.0
                for j in range(NT):
                    for part in range(2):
                        r = j + 16 * part
                        f, fk = fsl.next()
                        P.op("sp", lambda e, f=f, r=r: e.dma_start(out=f[:], in_=I["FTt"][r]), writes=[fk], dma=fk)
                        for kk in range(NT):
                            for half in range(2):
                                P.op("pe", lambda e, f=f, kk=kk, half=half, part=part: e.matmul(
                                    KP[part][:, half * 512:(half + 1) * 512], lhsT=f[:, kk, :], rhs=hn[:, kk, half * 512:(half + 1) * 512],
                                    start=(kk == 0), stop=(kk == NT - 1)), reads=[fk, "h0_hn"], writes=[("h0_KP", part)], sig=(kk == NT - 1 and half == 1))
                        P.op("act", lambda e, part=part: e.activation(out=ksb[part][:], in_=KP[part][:], func=AF.Copy, scale=sc),
                             reads=[("h0_KP", part)], writes=[("h0_ks", part)])
                    kre = ksb[0][:].rearrange("p (o d c) -> p o d c", o=2, d=2)
                    kim = ksb[1][:].rearrange("p (o d c) -> p o d c", o=2, d=2)
                    P.op("dve", lambda e, kre=kre: e.tensor_tensor(out=tab[:, :, 0, :], in0=kre[:, :, 0, :], in1=kre[:, :, 1, :], op=ALU.add),
                         reads=[("h0_ks", 0)], writes=["h0_tab"])
                    P.op("dve", lambda e, kim=kim: e.tensor_tensor(out=tab[:, :, 1, :], in0=kim[:, :, 0, :], in1=kim[:, :, 1, :], op=ALU.subtract),
                         reads=[("h0_ks", 1)], writes=["h0_tab"])
                    P.op("dve", lambda e: e.tensor_copy(out=tab[:, :, 2, :], in_=tab[:, :, 0, :]), reads=["h0_tab"], writes=["h0_tab"])
                    if j == 0:
                        P.op("dve", lambda e, kim=kim: e.tensor_tensor(out=tab[0:1, :, 2, :], in0=kim[0:1, :, 0, :], in1=kim[0:1, :, 1, :], op=ALU.add),
                             reads=[("h0_ks", 1), "h0_tab"], writes=["h0_tab"])
                        P.op("dve", lambda e: e.tensor_scalar(out=tab[0:1, :, 2, :], in0=tab[0:1, :, 2, :], scalar1=0.5, scalar2=None, op0=ALU.mult),
                             reads=["h0_tab"], writes=["h0_tab"])
                        P.op("dve", lambda e: e.tensor_scalar(out=tab[0:1, :, 0, :], in0=tab[0:1, :, 0, :], scalar1=0.5, scalar2=None, op0=ALU.mult),
                             reads=["h0_tab"], writes=["h0_tab"])
                        P.op("dve", lambda e: e.memset(tab[0:1, :, 1, :], 0.0), reads=["h0_tab"], writes=["h0_tab"])
                    for o in range(2):
                        P.op("sp", lambda e, o=o, j=j: e.dma_start(out=self.kfs[l, o, j], in_=tab[:, o, :, :]), reads=["h0_tab"], writes=["kfs"], dma="kfst")
                        if l == 0 and "kfs" in self.dbg_out:
                            P.op("sp", lambda e, o=o, j=j: e.dma_start(out=self.dbg_out["kfs"][o, j], in_=tab[:, o, :, :]), reads=["h0_tab"], dma="dbg2")
                P.flush()


def _mix_hy(self, b, l):
    I, P = self.I, self.P
    with ExitStack() as sm:
        z = self.sb(sm, "y_z", [128, NT, 256], BF16)
        g1 = self.sb(sm, "y_g1", [128, NT, 256], BF16)
        g2 = self.sb(sm, "y_g2", [128, NT, 256], BF16)
        cbrow = self.sb(sm, "y_cbrow", [1, 768])
        skb = self.sb(sm, "y_skb", [128, 2, 256])
        grow = self.sb(sm, "y_grow", [128, 256])
        P.op("sp", lambda e: e.dma_start(out=cbrow[:], in_=I["hy_conv_b"][l:l + 1, :]), writes=["cbrow"], dma="y_c")
        P.op("sp", lambda e: e.dma_start(out=skb[:].rearrange("p a b -> p (a b)"), in_=I["hy_skip"][l].rearrange("a b -> (a b)").partition_broadcast(128)),
             writes=["y_skb"], dma="y_c")
        P.op("sp", lambda e: e.dma_start(out=grow[:], in_=I["hy_norm_g"][l].partition_broadcast(128)), writes=["y_grow"], dma="y_c")
        with ExitStack() as st:
            dsts = {"v": z, "x1": g1, "x2": g2}

            def ev(tag, i, ps, pk):
                P.op("act", lambda e: e.activation(out=dsts[tag][:, i, :], in_=ps, func=AF.Copy), reads=[pk], writes=["y_" + tag])
            self.proj_tm(st, l, [(1, 0, 256, "v", cbrow[0:1, 0:256]), (1, 256, 256, "x1", cbrow[0:1, 256:512]),
                                 (1, 512, 256, "x2", cbrow[0:1, 512:768])], ev)
            P.flush()
        with ExitStack() as st:
            Y = self.sb(st, "y_Y", [128, 32, 256], BF16)
            fsl = Slots("y_F", [self.sb(st, f"y_F{i}", [128, NT, 128], BF16) for i in range(4)])
            ksl = Slots("y_k", [self.sb(st, f"y_k{i}", [128, 3, 256]) for i in range(2)])
            Zp = [self.ps(st, f"y_Zp{i}", [128, 256]) for i in range(2)]
            Cp = [self.ps(st, f"y_Cp{i}", [128, 256]) for i in range(2)]
            zs = [self.sb(st, f"y_zs{i}", [128, 256]) for i in range(2)]
            tt_ = [self.sb(st, f"y_t{i}", [128, 256]) for i in range(4)]
            zf = [self.sb(st, f"y_zf{i}", [128, 256]) for i in range(3)]
            epi = self.epilogue(st, "y_")
            if b == 0 and l == 0:
                self.dump("z0", z[:], "y_v")
                self.dump("g1", g1[:], "y_x1")
            for o in range(2):
                if o == 1 and b == 0 and l == 0:
                    self.dump("z1", z[:], "y_v")
                gate = g1 if o == 0 else g2
                gk = "y_x1" if o == 0 else "y_x2"
                for j in range(NT):
                    kt, kk_ = ksl.next()
                    P.op("sp", lambda e, kt=kt, o=o, j=j: e.dma_start(out=kt[:], in_=self.kfs[l, o, j]), reads=["kfs"], writes=[kk_], dma=kk_)
                    for part in range(2):
                        f, fk = fsl.next()
                        P.op("sp", lambda e, f=f, j=j, part=part: e.dma_start(out=f[:], in_=I["FTt"][j + 16 * part]), writes=[fk], dma=fk)
                        for kk in range(NT):
                            P.op("pe", lambda e, f=f, kk=kk, part=part: e.matmul(Zp[part][:], lhsT=f[:, kk, :], rhs=z[:, kk, :], start=(kk == 0), stop=(kk == NT - 1)),
                                 reads=[fk, "y_v"], writes=[("y_Zp", part)], sig=(kk == NT - 1))
                        P.op("act", lambda e, part=part: e.activation(out=zs[part][:], in_=Zp[part][:], func=AF.Copy), reads=[("y_Zp", part)], writes=[("y_zs", part)])
                    for n_, (src, tb_) in enumerate(((0, 0), (1, 1), (0, 1), (1, 2))):
                        P.op("dve", lambda e, n_=n_, src=src, tb_=tb_, kt=kt: e.tensor_tensor(out=tt_[n_][:], in0=zs[src][:], in1=kt[:, tb_, :], op=ALU.mult),
                             reads=[("y_zs", src), kk_], writes=[("y_t", n_)])
                    P.op("pool", lambda e, j=j: e.tensor_tensor(out=Y[:, j, :], in0=tt_[0][:], in1=tt_[1][:], op=ALU.subtract),
                         reads=[("y_t", 0), ("y_t", 1)], writes=["y_Y"])
                    P.op("pool", lambda e, j=j: e.tensor_tensor(out=Y[:, 16 + j, :], in0=tt_[2][:], in1=tt_[3][:], op=ALU.add),
                         reads=[("y_t", 2), ("y_t", 3)], writes=["y_Y"])
                if o == 0 and b == 0 and l == 0:
                    self.dump("Y0", Y[:], "y_Y")
                for i in range(NT):
                    C, Ck = Cp[i % 2], ("y_Cp", i % 2)
                    for hf in range(2):
                        f, fk = fsl.next()
                        P.op("sp", lambda e, f=f, i=i, hf=hf: e.dma_start(out=f[:], in_=I["Ft"][i, :, hf * 16:(hf + 1) * 16, :]), writes=[fk], dma=fk)
                        for rr in range(16):
                            r = hf * 16 + rr
                            P.op("pe", lambda e, f=f, rr=rr, r=r, C=C: e.matmul(C[:], lhsT=f[:, rr, :], rhs=Y[:, r, :], start=(r == 0), stop=(r == 31)),
                                 reads=[fk, "y_Y"], writes=[Ck], sig=(rr == 15))
                    t_, tk = tt_[i % 2], ("y_t", i % 2)
                    P.op("dve", lambda e, t_=t_, i=i, o=o: e.tensor_tensor(out=t_[:], in0=z[:, i, :], in1=skb[:, o, :], op=ALU.mult),
                         reads=["y_v", "y_skb"], writes=[tk])
                    P.op("dve", lambda e, t_=t_, C=C: e.tensor_tensor(out=t_[:], in0=t_[:], in1=C[:], op=ALU.add), reads=[tk, Ck], writes=[tk])
                    if o == 0:
                        P.op("pool", lambda e, t_=t_, i=i, gate=gate: e.tensor_tensor(out=z[:, i, :], in0=t_[:], in1=gate[:, i, :], op=ALU.mult),
                             reads=[tk, gk], writes=["y_v"])
                    else:
                        zz, zk = zf[i % 3], ("y_zf", i % 3)
                        P.op("pool", lambda e, t_=t_, i=i, zz=zz, gate=gate: e.tensor_tensor(out=zz[:], in0=t_[:], in1=gate[:, i, :], op=ALU.mult),
                             reads=[tk, gk], writes=[zk])
                        epi(i, zz[:], zk, grow[:], "y_grow")
            epi.finish()
            P.flush()


KB.phase0_hyena = _phase0_hyena
KB.mix_hy = _mix_hy


def kernel(**inputs):
    kb = KB()
    nc = kb.build()
    maps = make_in_maps(inputs, ncores=8)
    res = run_bass_kernel_spmd(nc, maps, core_ids=list(range(8)))
    return np.concatenate([np.asarray(r["out"], dtype=np.float32) for r in res.results], axis=0)
```

```python
import os
import numpy as np
from contextlib import ExitStack
import concourse.bass as bass
import concourse.mybir as mybir
from concourse.bass_utils import run_bass_kernel_spmd

F32 = mybir.dt.float32
BF16 = mybir.dt.bfloat16
AF = mybir.ActivationFunctionType
ALU = mybir.AluOpType
AX = mybir.AxisListType

COMPUTE = ("pe", "act", "dve", "pool")
QUEUES = ("pe", "act", "dve", "pool", "sp")


class Prog:
    def __init__(self, nc, stack):
        self.nc = nc
        self.stack = stack
        self.ops = []
        self.W = {}
        self.R = {}
        self.chan_sem = {}
        self.chan_cnt = {}
        self.waited = {q: {} for q in QUEUES}
        self.nphase = 0
        self.epoch = {q: 0 for q in COMPUTE}
        self.unsig = {}
        self.last_unsig = {}
        for q in COMPUTE:
            self._sem(("E", q, 0))

    def _sem(self, chan):
        if chan not in self.chan_sem:
            name = "s_" + "_".join(str(c) for c in (chan if isinstance(chan, tuple) else (chan,)))
            self.chan_sem[chan] = self.stack.enter_context(self.nc.semaphore(name))
            self.chan_cnt[chan] = 0
        return self.chan_sem[chan]

    def op(self, q, fn, reads=(), writes=(), dma=None, sig=True):
        if dma is not None:
            chan = ("dma", dma, q)
            self._sem(chan)
            step = 16
        else:
            if self.chan_cnt[("E", q, self.epoch[q])] >= 30000 and not self.unsig.get(q):
                self.epoch[q] += 1
                self._sem(("E", q, self.epoch[q]))
            chan = ("E", q, self.epoch[q])
            step = 1
        deps = {}

        def need(c, v):
            if c[0] == "E" and c[1] == "pe" and q == "pe" and dma is None:
                return
            if c[0] == "dma":
                v = self.chan_cnt[c]
            elif v > self.chan_cnt[c]:
                ent = self.last_unsig[c]
                ent[4] = 1
                self.chan_cnt[c] += 1
                self.unsig[c[1]] = False
            if deps.get(c, 0) < v:
                deps[c] = v

        for k in reads:
            for c, v in self.W.get(k, {}).items():
                need(c, v)
        for k in writes:
            for c, v in self.W.get(k, {}).items():
                if c[0] == "E" and c[1] == q and dma is None:
                    continue
                need(c, v)
            for c, v in self.R.get(k, {}).items():
                if c[0] == "E" and c[1] == q and dma is None:
                    continue
                need(c, v)
        if sig or dma is not None:
            self.chan_cnt[chan] += step
            tok = self.chan_cnt[chan]
            if dma is None:
                self.unsig[q] = False
        else:
            tok = self.chan_cnt[chan] + step
            self.unsig[q] = True
            step = 0
        for k in reads:
            self.R.setdefault(k, {})[chan] = tok
        for k in writes:
            self.W.setdefault(k, {})[chan] = tok
        ent = [q, fn, deps, chan, step]
        if step == 0:
            self.last_unsig[chan] = ent
        self.ops.append(ent)
        return tok

    def barrier(self):
        snap = dict(self.chan_cnt)
        for q in QUEUES:
            self.ops.append([q, None, snap, None, 0])

    def flush(self, name=None):
        nc = self.nc
        ops, self.ops = self.ops, []
        self.nphase += 1
        per_q = {q: [o for o in ops if o[0] == q] for q in QUEUES}
        prog = self

        def emit(q, eng):
            waited = prog.waited[q]
            for (_, fn, deps, chan, step) in per_q[q]:
                for c, v in deps.items():
                    if v <= 0 or waited.get(c, 0) >= v:
                        continue
                    eng.wait_ge(prog.chan_sem[c], v)
                    waited[c] = v
                if fn is None:
                    continue
                ins = fn(eng)
                if step:
                    ins.then_inc(prog.chan_sem[chan], step)

        with nc.Block() as blk:
            @blk.tensor
            def _(e):
                emit("pe", e)

            @blk.scalar
            def _(e):
                emit("act", e)

            @blk.vector
            def _(e):
                emit("dve", e)

            @blk.gpsimd
            def _(e):
                emit("pool", e)

            @blk.sync
            def _(e):
                emit("sp", e)
        self.barrier()

    def finish(self, q="sp"):
        snap = dict(self.chan_cnt)
        self.ops.append([q, None, snap, None, 0])


L = 2048
D = 1024
NT = 16
FH = 2816
NFC = 22
PI = float(np.pi)
OFF_NA, OFF_ML, OFF_HY = 1032, 1800, 2840
_CONST_CACHE = {}


def _consts():
    if _CONST_CACHE:
        return _CONST_CACHE
    import ml_dtypes
    bf = ml_dtypes.bfloat16
    C = _CONST_CACHE
    C["ident_bf"] = np.eye(128, dtype=np.float32).astype(bf)
    C["ident_f"] = np.eye(128, dtype=np.float32)
    C["ones_f"] = np.ones((128, 128), np.float32)
    k = np.arange(128)[:, None]
    s = np.arange(128)[None, :]
    lm = np.stack([(k > s), (k < s), (k <= s), (k >= s)]).astype(np.float32)
    C["lmask"] = np.ascontiguousarray(lm.transpose(1, 0, 2))
    negf = np.where(k > s, -30000.0, 0.0)
    negb = np.where(k < s, -30000.0, 0.0)
    ng = np.stack([np.tile(negf, (1, 4)), np.tile(negb, (1, 4))]).astype(np.float32)
    C["neg"] = np.ascontiguousarray(ng.transpose(1, 0, 2)).astype(bf)
    j = (np.arange(128) % 64)[:, None]
    kc = np.arange(64)[None, :]
    cs = np.clip(j - 8, 0, 48)
    valid = (kc >= cs) & (kc < cs + 16)
    m = np.where(valid, 0.0, -30000.0).astype(np.float32)
    C["na_mask"] = np.tile(m, (1, 8)).astype(bf)
    t = np.linspace(0.0, 1.0, L, dtype=np.float32)[:, None]
    f = np.linspace(1e-4, 15.0, 16, dtype=np.float32)
    ang = (np.float32(2.0 * np.pi) * (np.arange(L, dtype=np.float32) / np.float32(L))[:, None] * f[None, :]).astype(np.float32)
    feats = np.concatenate([t, np.cos(ang), -np.sin(ang)], axis=-1).astype(np.float32)
    C["featsT"] = np.ascontiguousarray(feats.T)
    deltas = np.abs(np.linspace(np.log(1e-2) / 1.5, np.log(1e-2) / 0.3, 256, dtype=np.float32))
    dec = np.exp(-t * deltas[None, :]).astype(np.float32)
    C["decay"] = np.ascontiguousarray(dec.reshape(16, 128, 256).transpose(1, 0, 2))
    tt = np.arange(L, dtype=np.float64)[None, :]
    ff = np.arange(2048, dtype=np.float64)[:, None]
    Fm = np.empty((4096, L), np.float64)
    Fm[:2048] = np.cos(2 * np.pi * ff * tt / 4096.0)
    Fm[2048:] = -np.sin(2 * np.pi * ff * tt / 4096.0)
    Fm[2048] = np.where(np.arange(L) % 2 == 0, 1.0, -1.0)
    Fb = Fm.astype(np.float32).astype(bf)
    C["FTt"] = np.ascontiguousarray(Fb.reshape(32, 128, 16, 128).transpose(0, 3, 2, 1))
    C["Ft"] = np.ascontiguousarray(Fb.reshape(32, 128, 16, 128).transpose(2, 1, 0, 3))
    return C


def _na_bias_gather(rpb):
    j = (np.arange(128) % 64)[:, None]
    kc = np.arange(64)[None, :]
    dc = np.clip(kc - j + 15, 0, 30)
    out = np.empty((2, 8, 128, 4, 8, 64), np.float32)
    for dl in range(8):
        for kr in range(8):
            g = rpb[:, :, dl + kr, :][:, :, dc]
            out[:, dl, :, :, kr, :] = g.transpose(0, 2, 1, 3)
    return out.reshape(2, 8, 128, 4, 512)


class Slots:
    def __init__(self, name, tiles):
        self.name, self.tiles, self.i = name, tiles, 0

    def next(self):
        n = self.i % len(self.tiles)
        self.i += 1
        return self.tiles[n], (self.name, n)


class KB:
    def __init__(self, dbg=None, nseq=2, nlayer=2, mixers=("ssd", "na", "ml", "hy"), ycat_in=False):
        self.dbg = dbg or {}
        self.nseq, self.nlayer, self.mixers, self.ycat_in = nseq, nlayer, mixers, ycat_in
        self.nc = nc = bass.Bass("TRN2", target_bir_lowering=False)
        self.I = {}
        self.dbg_out = {}

        def din(name, shape, dt=F32):
            self.I[name] = nc.dram_tensor(name, list(shape), dt, kind="ExternalInput").ap()

        din("x", [2, L, D]); din("cT", [128, 8, 2]); din("mod_w", [2, D, 6 * D]); din("mod_bT", [2, 128, 48])
        din("w_in", [2, D, 3608]); din("ssd_conv_w", [2, 3, 768]); din("ssd_conv_b", [2, 768])
        din("ssd_cbT", [2, 128, 6]); din("ssd_dt_bias", [2, 8]); din("ssd_a_log", [2, 8]); din("ssd_d", [2, 4]); din("ssd_norm_g", [2, 256])
        din("na_bias", [2, 8, 128, 4, 512]); din("na_norm_g", [2, 256])
        din("ml_i_bias", [2, 8]); din("ml_f_bias", [2, 8]); din("ml_norm_g", [2, 256])
        din("hy_conv_w", [2, 3, 768]); din("hy_conv_b", [2, 768]); din("hy_w1", [2, 33, 64]); din("hy_b1", [2, 64, 1])
        din("hy_w2", [2, 64, 64]); din("hy_b2", [2, 64, 1]); din("hy_w3", [2, 64, 1024]); din("hy_freq", [2, 2, 64, 1])
        din("hy_skip", [2, 2, 256]); din("hy_norm_g", [2, 256]); din("w_out", [2, D, D])
        din("ffn_w_gate", [2, D, FH]); din("ffn_w_up", [2, D, FH]); din("ffn_w_down", [2, FH, D]); din("final_norm_g", [D])
        din("ident_bf", [128, 128], BF16); din("ident_f", [128, 128]); din("ones_f", [128, 128])
        din("lmask", [128, 4, 128]); din("neg", [128, 2, 512], BF16); din("na_mask", [128, 512], BF16)
        din("featsT", [33, L]); din("decay", [128, 16, 256]); din("FTt", [32, 128, 16, 128], BF16); din("Ft", [16, 128, 32, 128], BF16)
        if ycat_in:
            din("ycatT", [2, 2, 128, 8, L], BF16)
        self.out = nc.dram_tensor("out", [2, L, D], F32, kind="ExternalOutput").ap()
        for k, shp in self.dbg.items():
            self.dbg_out[k] = nc.dram_tensor("dbg_" + k, list(shp), F32, kind="ExternalOutput").ap()
        self.wcv = nc.dram_tensor("wcv", [2, 2, 3, D, 768], BF16, kind="Internal").ap()
        self.kfs = nc.dram_tensor("kfs", [2, 2, 16, 128, 3, 256], F32, kind="Internal").ap()

    def sb(self, st, name, shape, dt=F32):
        self._uid = getattr(self, "_uid", 0) + 1
        return st.enter_context(self.nc.sbuf_tensor(f"{name}_{self._uid}", list(shape), dt))

    def ps(self, st, name, shape, dt=F32):
        self._uid = getattr(self, "_uid", 0) + 1
        return st.enter_context(self.nc.psum_tensor(f"{name}_{self._uid}", list(shape), dt))

    def dump(self, name, src_ap, key, dst=None):
        if name not in self.dbg_out:
            return
        d = self.dbg_out[name] if dst is None else dst
        self.P.op("pool", lambda e: e.dma_start(out=d, in_=src_ap), reads=[key], dma="dbg")

    def build(self):
        nc, I = self.nc, self.I
        with ExitStack() as st:
            self.P = P = Prog(nc, st)
            self.ident_bf = self.sb(st, "ident_bf", [128, 128], BF16)
            self.ident_f = self.sb(st, "ident_f", [128, 128])
            self.ones_f = self.sb(st, "ones_f", [128, 128])
            self.modT = self.sb(st, "modT", [128, 2, 48, 2])
            self.condT = self.sb(st, "condT", [128, 8, 2], BF16)
            for nm in ("ident_bf", "ident_f", "ones_f"):
                t = getattr(self, nm)
                P.op("sp", lambda e, t=t, nm=nm: e.dma_start(out=t[:], in_=I[nm]), writes=[nm], dma="const")
            self.phase0()
            with ExitStack() as sx:
                self.x = self.sb(sx, "xres", [128, NT, D])
                for b in range(self.nseq):
                    for i in range(NT):
                        P.op("sp", lambda e, b=b, i=i: e.dma_start(out=self.x[:, i, :], in_=I["x"][b, i * 128:(i + 1) * 128, :]),
                             writes=[("x", i)], dma="xload")
                    for l in range(self.nlayer):
                        self.layer(b, l)
                    self.final(b)
            P.finish()
            P.flush()
        return nc

    def phase0(self):
        nc, I, P = self.nc, self.I, self.P
        with ExitStack() as st:
            cTf = self.sb(st, "cTf", [128, 8, 2])
            mbT = self.sb(st, "mbT", [128, 2, 48])
            wsl = Slots("mw", [self.sb(st, f"mw{i}", [128, 8, 512], BF16) for i in range(2)])
            mps = self.ps(st, "mps", [128, 96])
            P.op("sp", lambda e: e.dma_start(out=cTf[:], in_=I["cT"]), writes=["cTf"], dma="const")
            for l in range(2):
                P.op("sp", lambda e, l=l: e.dma_start(out=mbT[:, l, :], in_=I["mod_bT"][l]), writes=["mbT"], dma="const")
            P.op("act", lambda e: e.activation(out=self.condT[:], in_=cTf[:], func=AF.Silu), reads=["cTf"], writes=["condT"])
            for l in range(2):
                wv = I["mod_w"][l].rearrange("(j p) f -> p j f", p=128)
                for blk in range(12):
                    w, wk = wsl.next()
                    P.op("pool", lambda e, w=w, blk=blk, wv=wv: e.dma_start(out=w[:], in_=wv[:, :, blk * 512:(blk + 1) * 512]),
                         writes=[wk], dma=wk)
                    for fc in range(4):
                        col = (blk * 4 + fc) * 2
                        for j in range(8):
                            P.op("pe", lambda e, w=w, fc=fc, j=j, col=col: e.matmul(
                                mps[:, col:col + 2], lhsT=w[:, j, fc * 128:(fc + 1) * 128], rhs=self.condT[:, j, :],
                                start=(j == 0), stop=(j == 7)), reads=[wk, "condT"], writes=["mps"], sig=(j == 7))
                mv = mps[:].rearrange("p (c b) -> p c b", b=2)
                P.op("dve", lambda e, l=l, mv=mv: e.tensor_tensor(
                    out=self.modT[:, l, :, :], in0=mv, in1=mbT[:, l, :].unsqueeze(2).broadcast_to([128, 48, 2]), op=ALU.add),
                    reads=["mps", "mbT"], writes=["modT"])
                for c0 in (8, 32):
                    P.op("dve", lambda e, l=l, c0=c0: e.tensor_scalar_add(
                        out=self.modT[:, l, c0:c0 + 8, :], in0=self.modT[:, l, c0:c0 + 8, :], scalar1=1.0),
                        reads=["modT"], writes=["modT"])
            if "modT" in self.dbg_out:
                self.dump("modT", self.modT[:], "modT")
            P.flush()
        if "hy" in self.mixers or "ssd" in self.mixers:
            self.phase0_wcv()
        if "hy" in self.mixers:
            self.phase0_hyena()

    def phase0_wcv(self):
        pass

    def phase0_hyena(self):
        pass

    def norm_to_hT(self, st, b, l, which):
        nc, P = self.nc, self.P
        sh_c, sc_c = (0, 8) if which == 0 else (24, 32)
        ss = self.sb(st, "n_ss", [128, NT])
        rstd = self.sb(st, "n_rstd", [128, NT])
        junk = self.sb(st, "n_junk", [128, D], BF16)
        xn = [self.sb(st, f"n_xn{i}", [128, 4, D], BF16) for i in range(2)]
        tp = [self.ps(st, f"n_tp{i}", [128, 512], BF16) for i in range(2)]
        hT = self.hT
        P.op("dve", lambda e: e.memset(hT[:, :, 0:1], 0.0), writes=["hT"])
        P.op("dve", lambda e: e.memset(hT[:, :, L + 1:L + 2], 0.0), writes=["hT"])
        cnt = [0]

        def chain(tb):
            xb, xk = xn[tb % 2], ("n_xn", tb % 2)
            for ii in range(4):
                i = tb * 4 + ii
                P.op("act", lambda e, i=i: e.activation(out=junk[:], in_=self.x[:, i, :], func=AF.Square,
                                                         accum_out=ss[:, i:i + 1]), reads=[("x", i)], writes=["n_junk", ("n_ss", i)])
                P.op("act", lambda e, i=i: e.activation(out=rstd[:, i:i + 1], in_=ss[:, i:i + 1], func=AF.Sqrt, bias=1e-6, scale=1.0 / D),
                     reads=[("n_ss", i)], writes=[("n_rs", i)])
                P.op("dve", lambda e, i=i: e.reciprocal(out=rstd[:, i:i + 1], in_=rstd[:, i:i + 1]), reads=[("n_rs", i)], writes=[("n_rs", i)])
                P.op("dve", lambda e, i=i, ii=ii, xb=xb: e.tensor_scalar(out=xb[:, ii, :], in0=self.x[:, i, :],
                                                                        scalar1=rstd[:, i:i + 1], scalar2=None, op0=ALU.mult),
                     reads=[("x", i), ("n_rs", i)], writes=[xk])

        def trans(tb):
            xb, xk = xn[tb % 2], ("n_xn", tb % 2)
            for j in range(8):
                n = cnt[0]
                cnt[0] += 1
                t, tk = tp[n % 2], ("n_tp", n % 2)
                for ii in range(4):
                    P.op("pe", lambda e, t=t, ii=ii, j=j, xb=xb: e.transpose(
                        t[:, ii * 128:(ii + 1) * 128], xb[:, ii, j * 128:(j + 1) * 128], self.ident_bf[:]),
                        reads=[xk, "ident_bf"], writes=[tk], sig=(ii == 3))
                dst = hT[:, j, 1 + tb * 512:1 + (tb + 1) * 512]
                if j % 2 == 0:
                    P.op("act", lambda e, t=t, dst=dst, j=j: e.activation(
                        out=dst, in_=t[:], func=AF.Identity, bias=self.modT[:, l, sh_c + j, b:b + 1],
                        scale=self.modT[:, l, sc_c + j, b:b + 1]), reads=[tk, "modT"], writes=[("hT", tb)])
                else:
                    P.op("dve", lambda e, t=t, dst=dst, j=j: e.tensor_scalar(
                        out=dst, in0=t[:], scalar1=self.modT[:, l, sc_c + j, b:b + 1],
                        scalar2=self.modT[:, l, sh_c + j, b:b + 1], op0=ALU.mult, op1=ALU.add),
                        reads=[tk, "modT"], writes=[("hT", tb)])

        chain(0)
        for tb in range(4):
            if tb + 1 < 4:
                chain(tb + 1)
            trans(tb)

    HT_ALL = ["hT"] + [("hT", tb) for tb in range(4)]

    def layer(self, b, l):
        nc, I, P = self.nc, self.I, self.P
        with ExitStack() as sl:
            self.G = [self.sb(sl, f"Gk{g}", [128, D]) for g in range(2)]
            with ExitStack() as st:
                self._grow_into(st, b, l)
                P.flush()
            with ExitStack() as sm:
                self.hT = self.sb(sm, "hT", [128, 8, L + 2], BF16)
                with ExitStack() as st:
                    self.norm_to_hT(st, b, l, 0)
                    if l == 0 and b == 0 and "hT0" in self.dbg_out:
                        pass
                    P.flush()
                for mi, m in enumerate(("ssd", "na", "ml", "hy")):
                    with ExitStack() as sy:
                        self.yT = self.sb(sy, "yT", [128, 2, L], BF16)
                        if m in self.mixers:
                            getattr(self, "mix_" + m)(b, l)
                        elif self.ycat_in:
                            P.op("sp", lambda e, mi=mi: e.dma_start(out=self.yT[:], in_=I["ycatT"][b, l, :, 2 * mi:2 * mi + 2, :]),
                                 writes=["yT"], dma="yT")
                        else:
                            P.op("dve", lambda e: e.memset(self.yT[:], 0.0), writes=["yT"])
                        with ExitStack() as st:
                            self.wout_part(st, b, l, mi)
                            P.flush()
            with ExitStack() as sm:
                self.hT = self.sb(sm, "hT", [128, 8, L + 2], BF16)
                with ExitStack() as st:
                    self.norm_to_hT(st, b, l, 1)
                    P.flush()
                self.ffn(b, l)

    def _grow_into(self, st, b, l):
        P = self.P
        dg = [self.sb(st, f"g_dg{i}", [128, 128]) for i in range(2)]
        gps = self.ps(st, "g_ps", [128, D])
        n = 0
        for g in range(2):
            c0 = 16 if g == 0 else 40
            for j in range(8):
                d, dk = dg[n % 2], ("g_dg", n % 2)
                n += 1
                P.op("dve", lambda e, d=d, j=j, c0=c0: e.tensor_scalar(
                    out=d[:], in0=self.ident_f[:], scalar1=self.modT[:, l, c0 + j, b:b + 1], scalar2=None, op0=ALU.mult),
                    reads=["ident_f", "modT"], writes=[dk])
                P.op("pe", lambda e, d=d, j=j: e.matmul(gps[:, j * 128:(j + 1) * 128], lhsT=self.ones_f[:], rhs=d[:],
                                                         start=True, stop=True), reads=["ones_f", dk], writes=["g_ps"], sig=(j == 7))
            P.op("act", lambda e, g=g: e.activation(out=self.G[g][:], in_=gps[:], func=AF.Copy), reads=["g_ps"], writes=[("G", g)])

    def wout_part(self, st, b, l, mi):
        I, P = self.I, self.P
        wo = self.sb(st, "wo", [128, 2, D], BF16)
        tmp = [self.sb(st, f"wo_t{i}", [128, 512]) for i in range(2)]
        ops_ = [self.ps(st, f"wo_ps{i}", [128, 512]) for i in range(2)]
        wv = I["w_out"][l, 256 * mi:256 * (mi + 1), :].rearrange("(j p) f -> p j f", p=128)
        P.op("pool", lambda e: e.dma_start(out=wo[:], in_=wv), writes=["wo"], dma="wo")
        n = 0
        for i in range(NT):
            for dh in range(2):
                pt, pk = ops_[n % 2], ("wo_ps", n % 2)
                tt, tk = tmp[n % 2], ("wo_t", n % 2)
                n += 1
                for j in range(2):
                    P.op("pe", lambda e, pt=pt, i=i, j=j, dh=dh: e.matmul(
                        pt[:], lhsT=self.yT[:, j, i * 128:(i + 1) * 128], rhs=wo[:, j, dh * 512:(dh + 1) * 512],
                        start=(j == 0), stop=(j == 1)), reads=["yT", "wo"], writes=[pk], sig=(j == 1))
                P.op("dve", lambda e, pt=pt, tt=tt, dh=dh: e.tensor_tensor(
                    out=tt[:], in0=pt[:], in1=self.G[0][:, dh * 512:(dh + 1) * 512], op=ALU.mult),
                    reads=[pk, ("G", 0)], writes=[tk])
                xs = self.x[:, i, dh * 512:(dh + 1) * 512]
                P.op("pool", lambda e, tt=tt, xs=xs: e.tensor_tensor(out=xs, in0=xs, in1=tt[:], op=ALU.add),
                     reads=[tk, ("x", i)], writes=[("x", i)])

    def ffn(self, b, l):
        I, P = self.I, self.P
        wg_v = I["ffn_w_gate"][l].rearrange("(j p) f -> p j f", p=128)
        wu_v = I["ffn_w_up"][l].rearrange("(j p) f -> p j f", p=128)
        wd_v = I["ffn_w_down"][l].rearrange("(c p) f -> p c f", p=128)
        for half in range(2):
            with ExitStack() as sa:
                act = self.sb(sa, "f_act", [128, NFC, 1024], BF16)
                with ExitStack() as st:
                    wsl = Slots("f_w", [self.sb(st, f"f_w{i}", [128, 2, 8, 128], BF16) for i in range(5)])
                    sg = [self.sb(st, f"f_sg{i}", [128, 512]) for i in range(2)]
                    pg = [self.ps(st, f"f_pg{i}", [128, 512]) for i in range(2)]
                    pu = [self.ps(st, f"f_pu{i}", [128, 512]) for i in range(2)]
                    n = 0
                    for fc in range(NFC):
                        w, wk = wsl.next()
                        P.op("pool", lambda e, w=w, fc=fc: e.dma_start(out=w[:, 0, :, :], in_=wg_v[:, :, fc * 128:(fc + 1) * 128]),
                             writes=[wk], dma=wk)
                        P.op("pool", lambda e, w=w, fc=fc: e.dma_start(out=w[:, 1, :, :], in_=wu_v[:, :, fc * 128:(fc + 1) * 128]),
                             writes=[wk], dma=wk)
                        for q in range(2):
                            t0 = 1 + half * 1024 + q * 512
                            g_, gk = pg[n % 2], ("f_pg", n % 2)
                            u_, uk = pu[n % 2], ("f_pu", n % 2)
                            s_, sk = sg[n % 2], ("f_sg", n % 2)
                            n += 1
                            for gi, (pt, pk) in enumerate(((g_, gk), (u_, uk))):
                                for j in range(8):
                                    P.op("pe", lambda e, pt=pt, w=w, gi=gi, j=j, t0=t0: e.matmul(
                                        pt[:], lhsT=w[:, gi, j, :], rhs=self.hT[:, j, t0:t0 + 512], start=(j == 0), stop=(j == 7)),
                                        reads=[wk] + self.HT_ALL, writes=[pk], sig=(j == 7))
                            P.op("act", lambda e, g_=g_, s_=s_: e.activation(out=s_[:], in_=g_[:], func=AF.Silu),
                                 reads=[gk], writes=[sk])
                            P.op("dve", lambda e, u_=u_, s_=s_, fc=fc, q=q: e.tensor_tensor(
                                out=act[:, fc, q * 512:(q + 1) * 512], in0=u_[:], in1=s_[:], op=ALU.mult),
                                reads=[uk, sk], writes=["f_act"])
                    P.flush()
                with ExitStack() as st:
                    wsl = Slots("f_wd", [self.sb(st, f"f_wd{i}", [128, 512], BF16) for i in range(6)])
                    acc = [self.ps(st, f"f_acc{i}", [128, 512]) for i in range(8)]
                    tmp = [self.sb(st, f"f_t{i}", [128, 512]) for i in range(2)]
                    n = 0
                    for dh in range(2):
                        for fc in range(NFC):
                            w, wk = wsl.next()
                            P.op("pool", lambda e, w=w, fc=fc, dh=dh: e.dma_start(out=w[:], in_=wd_v[:, fc, dh * 512:(dh + 1) * 512]),
                                 writes=[wk], dma=wk)
                            for tt in range(8):
                                P.op("pe", lambda e, w=w, fc=fc, tt=tt: e.matmul(
                                    acc[tt][:], lhsT=act[:, fc, tt * 128:(tt + 1) * 128], rhs=w[:], start=(fc == 0), stop=(fc == NFC - 1)),
                                    reads=[wk, "f_act"], writes=[("f_acc", tt)], sig=(tt == 7 or fc == NFC - 1))
                        for tt in range(8):
                            i = half * 8 + tt
                            t_, tk = tmp[n % 2], ("f_t", n % 2)
                            n += 1
                            P.op("dve", lambda e, t_=t_, tt=tt, dh=dh: e.tensor_tensor(
                                out=t_[:], in0=acc[tt][:], in1=self.G[1][:, dh * 512:(dh + 1) * 512], op=ALU.mult),
                                reads=[("f_acc", tt), ("G", 1)], writes=[tk])
                            xs = self.x[:, i, dh * 512:(dh + 1) * 512]
                            P.op("pool", lambda e, t_=t_, xs=xs: e.tensor_tensor(out=xs, in0=xs, in1=t_[:], op=ALU.add),
                                 reads=[tk, ("x", i)], writes=[("x", i)])
                    P.flush()

    def final(self, b):
        I, P = self.I, self.P
        with ExitStack() as st:
            gb = self.sb(st, "fn_g", [128, D])
            ss = self.sb(st, "fn_ss", [128, NT])
            rs = self.sb(st, "fn_rs", [128, NT])
            junk = self.sb(st, "fn_junk", [128, D], BF16)
            ob = [self.sb(st, f"fn_o{i}", [128, D]) for i in range(2)]
            P.op("sp", lambda e: e.dma_start(out=gb[:], in_=I["final_norm_g"].partition_broadcast(128)), writes=["fn_g"], dma="fn_g")
            for i in range(NT):
                o, ok = ob[i % 2], ("fn_o", i % 2)
                P.op("act", lambda e, i=i: e.activation(out=junk[:], in_=self.x[:, i, :], func=AF.Square, accum_out=ss[:, i:i + 1]),
                     reads=[("x", i)], writes=["fn_junk", ("fn_ss", i)])
                P.op("act", lambda e, i=i: e.activation(out=rs[:, i:i + 1], in_=ss[:, i:i + 1], func=AF.Sqrt, bias=1e-6, scale=1.0 / D),
                     reads=[("fn_ss", i)], writes=[("fn_rs", i)])
                P.op("dve", lambda e, i=i: e.reciprocal(out=rs[:, i:i + 1], in_=rs[:, i:i + 1]), reads=[("fn_rs", i)], writes=[("fn_rs", i)])
                P.op("dve", lambda e, i=i, o=o: e.scalar_tensor_tensor(out=o[:], in0=self.x[:, i, :], scalar=rs[:, i:i + 1],
                                                                       in1=gb[:], op0=ALU.mult, op1=ALU.mult),
                     reads=[("x", i), ("fn_rs", i), "fn_g"], writes=[ok])
                P.op("sp", lambda e, i=i, o=o: e.dma_start(out=self.out[b, i * 128:(i + 1) * 128, :], in_=o[:]),
                     reads=[ok], dma=("out", i % 2))
            P.flush()


def make_in_maps(inp, ncores=8):
    C = _consts()
    g = lambda k: np.ascontiguousarray(np.asarray(inp[k], dtype=np.float32))
    shared = {
        "mod_w": g("mod_w"), "mod_bT": np.ascontiguousarray(g("mod_b").reshape(2, 48, 128).transpose(0, 2, 1)),
        "w_in": g("w_in"), "ssd_conv_w": g("ssd_conv_w"), "ssd_conv_b": g("ssd_conv_b"),
        "ssd_cbT": np.ascontiguousarray(g("ssd_conv_b").reshape(2, 6, 128).transpose(0, 2, 1)), "ssd_dt_bias": g("ssd_dt_bias").reshape(2, 8), "ssd_a_log": g("ssd_a_log").reshape(2, 8), "ssd_d": g("ssd_d"),
        "ssd_norm_g": g("ssd_norm_g"), "na_bias": _na_bias_gather(g("na_rpb")), "na_norm_g": g("na_norm_g"),
        "ml_i_bias": g("ml_i_bias").reshape(2, 8), "ml_f_bias": g("ml_f_bias").reshape(2, 8), "ml_norm_g": g("ml_norm_g"),
        "hy_conv_w": g("hy_conv_w"), "hy_conv_b": g("hy_conv_b"), "hy_w1": g("hy_w1"), "hy_b1": g("hy_b1").reshape(2, 64, 1),
        "hy_w2": g("hy_w2"), "hy_b2": g("hy_b2").reshape(2, 64, 1), "hy_w3": g("hy_w3"), "hy_freq": g("hy_freq").reshape(2, 2, 64, 1),
        "hy_skip": g("hy_skip"), "hy_norm_g": g("hy_norm_g"), "w_out": g("w_out"),
        "ffn_w_gate": g("ffn_w_gate"), "ffn_w_up": g("ffn_w_up"), "ffn_w_down": g("ffn_w_down"), "final_norm_g": g("final_norm_g"),
    }
    for k in ("ident_bf", "ident_f", "ones_f", "lmask", "neg", "na_mask", "featsT", "decay", "FTt", "Ft"):
        shared[k] = C[k]
    x, c = g("x"), g("c")
    maps = []
    for i in range(ncores):
        m = dict(shared)
        m["x"] = np.ascontiguousarray(x[2 * i:2 * i + 2])
        m["cT"] = np.ascontiguousarray(c[2 * i:2 * i + 2].reshape(2, 8, 128).transpose(2, 1, 0))
        maps.append(m)
    return maps


def _phase0_wcv(self):
    I, P = self.I, self.P
    with ExitStack() as st:
        cw = self.sb(st, "cv_cw", [128, 3, 768])
        wf = Slots("cv_wf", [self.sb(st, f"cv_wf{i}", [128, 768]) for i in range(2)])
        ob = Slots("cv_ob", [self.sb(st, f"cv_ob{i}", [128, 768], BF16) for i in range(3)])
        n = 0
        for l in range(2):
            for grp, (cwn, c0) in enumerate((("ssd_conv_w", 256), ("hy_conv_w", OFF_HY))):
                P.op("sp", lambda e, l=l, cwn=cwn: e.dma_start(
                    out=cw[:].rearrange("p a b -> p (a b)"), in_=I[cwn][l].rearrange("a b -> (a b)").partition_broadcast(128)),
                    writes=["cv_cw"], dma="cv_cw")
                for j in range(8):
                    w, wk = wf.next()
                    P.op("sp", lambda e, w=w, l=l, j=j, c0=c0: e.dma_start(out=w[:], in_=I["w_in"][l, j * 128:(j + 1) * 128, c0:c0 + 768]),
                         writes=[wk], dma=wk)
                    for tap in range(3):
                        o, ok = ob.next()
                        q = "dve" if n % 2 == 0 else "pool"
                        n += 1
                        P.op(q, lambda e, o=o, w=w, tap=tap: e.tensor_tensor(out=o[:], in0=w[:], in1=cw[:, tap, :], op=ALU.mult),
                             reads=[wk, "cv_cw"], writes=[ok])
                        P.op("sp", lambda e, o=o, l=l, grp=grp, tap=tap, j=j: e.dma_start(
                            out=self.wcv[l, grp, tap, j * 128:(j + 1) * 128, :], in_=o[:]), reads=[ok], writes=["wcv"], dma=("wcvst", ok[1]))
        P.flush()


def _proj_tm(self, st, l, specs, evac, tiles=range(NT), tok_off=0, tagp="ptm"):
    I, P = self.I, self.P
    mt = 3 if any(sp[0] is not None for sp in specs) else 1
    wsl = Slots(tagp + "w", [self.sb(st, f"{tagp}w{i}", [128, mt, 8, 256], BF16) for i in range(2)])
    pps = [self.ps(st, f"{tagp}p{i}", [128, 256]) for i in range(2)]
    cnt = 0
    for (src, c0, n, tag, bias_ap) in specs:
        w, wk = wsl.next()
        taps = 1 if src is None else 3
        for tap in range(taps):
            if src is None:
                v = I["w_in"][l].rearrange("(j p) c -> p j c", p=128)[:, :, c0:c0 + n]
                P.op("pool", lambda e, w=w, v=v, n=n: e.dma_start(out=w[:, 0, :, :n], in_=v), writes=[wk], dma=wk)
            else:
                v = self.wcv[l, src, tap].rearrange("(j p) c -> p j c", p=128)[:, :, c0:c0 + n]
                P.op("sp", lambda e, w=w, v=v, n=n, tap=tap: e.dma_start(out=w[:, tap, :, :n], in_=v), reads=["wcv"], writes=[wk], dma=wk)
        for i in tiles:
            pt, pk = pps[cnt % 2], (tagp + "p", cnt % 2)
            cnt += 1
            k, tot = 0, taps * 8
            for tap in range(taps):
                sh = 0 if src is None else tap - 1
                t0 = 1 + tok_off + i * 128 + sh
                for j in range(8):
                    last = (k == tot - 1) and bias_ap is None
                    P.op("pe", lambda e, pt=pt, w=w, tap=tap, j=j, t0=t0, n=n, k=k, last=last: e.matmul(
                        pt[:, :n], lhsT=self.hT[:, j, t0:t0 + 128], rhs=w[:, tap, j, :n], start=(k == 0), stop=last),
                        reads=[wk] + self.HT_ALL, writes=[pk], sig=(k == tot - 1))
                    k += 1
            if bias_ap is not None:
                P.op("pe", lambda e, pt=pt, n=n, bias_ap=bias_ap: e.matmul(pt[:, :n], lhsT=self.ones_f[0:1, :], rhs=bias_ap,
                                                                           start=False, stop=True), reads=["ones_f", "cbrow"], writes=[pk])
            evac(tag, i, pt[:, :n], pk)


def _proj_fm(self, st, l, specs, evac, tagp="pfm"):
    I, P = self.I, self.P
    mt = 3 if any(sp[0] is not None for sp in specs) else 1
    wsl = Slots(tagp + "w", [self.sb(st, f"{tagp}w{i}", [128, mt, 8, 128], BF16) for i in range(2)])
    pps = [self.ps(st, f"{tagp}p{i}", [128, 512]) for i in range(2)]
    cnt = 0
    for (src, c0, n, tag) in specs:
        w, wk = wsl.next()
        taps = 1 if src is None else 3
        for tap in range(taps):
            if src is None:
                v = I["w_in"][l].rearrange("(j p) c -> p j c", p=128)[:, :, c0:c0 + n]
                P.op("pool", lambda e, w=w, v=v, n=n: e.dma_start(out=w[:, 0, :, :n], in_=v), writes=[wk], dma=wk)
            else:
                v = self.wcv[l, src, tap].rearrange("(j p) c -> p j c", p=128)[:, :, c0:c0 + n]
                P.op("sp", lambda e, w=w, v=v, n=n, tap=tap: e.dma_start(out=w[:, tap, :, :n], in_=v), reads=["wcv"], writes=[wk], dma=wk)
        for tb in range(4):
            pt, pk = pps[cnt % 2], (tagp + "p", cnt % 2)
            cnt += 1
            k, tot = 0, taps * 8
            for tap in range(taps):
                sh = 0 if src is None else tap - 1
                t0 = 1 + tb * 512 + sh
                for j in range(8):
                    P.op("pe", lambda e, pt=pt, w=w, tap=tap, j=j, t0=t0, n=n, k=k, tot=tot: e.matmul(
                        pt[:n, :], lhsT=w[:, tap, j, :n], rhs=self.hT[:, j, t0:t0 + 512], start=(k == 0), stop=(k == tot - 1)),
                        reads=[wk] + self.HT_ALL, writes=[pk], sig=(k == tot - 1))
                    k += 1
            evac(tag, tb, pt[:n, :], pk)


def _epilogue(self, st, tagp):
    P = self.P
    junk = self.sb(st, tagp + "ej", [128, 256])
    ss = self.sb(st, tagp + "ess", [128, NT])
    rs = self.sb(st, tagp + "ers", [128, NT])
    yb = [self.sb(st, f"{tagp}eyb{i}", [128, 256], BF16) for i in range(3)]
    tp = [self.ps(st, f"{tagp}etp{i}", [128, 256], BF16) for i in range(2)]
    pending = []

    def fn(i, src, skey, grow, gkey):
        y, yk = yb[i % 3], (tagp + "eyb", i % 3)
        t, tk = tp[i % 2], (tagp + "etp", i % 2)
        P.op("act", lambda e: e.activation(out=junk[:], in_=src, func=AF.Square, accum_out=ss[:, i:i + 1]),
             reads=[skey], writes=[tagp + "ej", (tagp + "ess", i)])
        P.op("act", lambda e: e.activation(out=rs[:, i:i + 1], in_=ss[:, i:i + 1], func=AF.Sqrt, bias=1e-6, scale=1.0 / 256),
             reads=[(tagp + "ess", i)], writes=[(tagp + "ers", i)])
        P.op("dve", lambda e: e.reciprocal(out=rs[:, i:i + 1], in_=rs[:, i:i + 1]), reads=[(tagp + "ers", i)], writes=[(tagp + "ers", i)])
        P.op("dve", lambda e: e.scalar_tensor_tensor(out=y[:], in0=src, scalar=rs[:, i:i + 1], in1=grow, op0=ALU.mult, op1=ALU.mult),
             reads=[skey, (tagp + "ers", i), gkey], writes=[yk])
        fn.finish()
        pending.append((i, y, yk, t, tk))

    def finish():
        while pending:
            self.to_yT(*pending.pop(0))
    fn.finish = finish
    return fn


def _to_yT(self, i, y, yk, t, tk):
    P = self.P
    for jj in range(2):
        P.op("pe", lambda e, jj=jj: e.transpose(t[:, jj * 128:(jj + 1) * 128], y[:, jj * 128:(jj + 1) * 128], self.ident_bf[:]),
             reads=[yk, "ident_bf"], writes=[tk], sig=(jj == 1))
    P.op("act", lambda e: e.activation(out=self.yT[:, :, i * 128:(i + 1) * 128], in_=t[:].rearrange("p (a b) -> p a b", a=2), func=AF.Copy),
         reads=[tk], writes=["yT"])


def _sweep(self, st, tagp, nq, pv, sgroups, KT, QT, Ktm, strow, a_ap, build_vd, accum, ufull=False):
    P = self.P
    H = 4
    lm = self.sb(st, tagp + "lm", [128, 4, 128])
    neg = self.sb(st, tagp + "neg", [128, 2, 512], BF16)
    P.op("sp", lambda e: e.dma_start(out=lm[:], in_=self.I["lmask"]), writes=[tagp + "lm"], dma=tagp + "c")
    P.op("sp", lambda e: e.dma_start(out=neg[:], in_=self.I["neg"]), writes=[tagp + "neg"], dma=tagp + "c")
    pvp = ((pv + 7) // 8) * 8
    St = self.sb(st, tagp + "St", [128, 2, H, pvp])[:, :, :, 0:pv]
    Stb = self.sb(st, tagp + "Stb", [128, 2, H, pvp], BF16)[:, :, :, 0:pv]
    P.op("dve", lambda e: e.memset(St[:], 0.0), writes=[tagp + "St0", tagp + "St1"])
    P.op("dve", lambda e: e.memset(Stb[:], 0.0), writes=[tagp + "Stb0", tagp + "Stb1"])
    hv = lambda t: t[:, 0:H * pvp].rearrange("p (h c) -> p h c", h=H)[:, :, 0:pv]
    gps = self.ps(st, tagp + "gps", [128, 512])
    Yps = hv(self.ps(st, tagp + "Yps", [128, 512]))
    Ups = hv(self.ps(st, tagp + "Ups", [128, 512]))
    U2ps = hv(self.ps(st, tagp + "U2ps", [128, 512]))
    Dps_ = [self.ps(st, f"{tagp}Dps{d}", [128, 512]) for d in range(2)]
    Sps_ = [self.ps(st, f"{tagp}Sps{d}", [128, 512]) for d in range(2)]
    egs_ = [self.sb(st, f"{tagp}egs{d}", [128, 16]) for d in range(2)]
    Lm_ = [self.sb(st, f"{tagp}Lm{d}", [128, 4, 128]) for d in range(2)]
    ET_ = [self.sb(st, f"{tagp}ET{d}", [128, 512]) for d in range(2)]
    PT_ = [self.sb(st, f"{tagp}PT{d}", [128, 512], BF16) for d in range(2)]
    Vd_ = [self.sb(st, f"{tagp}Vd{d}", [128, H, pvp], BF16)[:, :, 0:pv] for d in range(2)]
    Vp_ = [self.sb(st, f"{tagp}Vp{d}", [128, H, pvp], BF16)[:, :, 0:pv] for d in range(2)]
    T1_ = [self.sb(st, f"{tagp}T1{d}", [128, H, pvp])[:, :, 0:pv] for d in range(2)]
    ng = len(sgroups)
    rep = H // ng
    def stageA(step, d):
        k = lambda nm, d=d: tagp + nm + str(d)
        kg = lambda nm: tagp + nm
        Dps, Sps, egs, Lm, ET, PT, Vd, Vp, T1 = Dps_[d], Sps_[d], egs_[d], Lm_[d], ET_[d], PT_[d], Vd_[d], Vp_[d], T1_[d]
        c = step if d == 0 else NT - 1 - step
        a = a_ap(c, d)
        mL, mR = lm[:, d, :], lm[:, 2 + d, :]
        P.op("pe", lambda e, mR=mR, a=a: e.matmul(gps[:, 0:4], lhsT=mR, rhs=a, start=True, stop=True), reads=[kg("lm"), "avals"], writes=[kg("gps")], sig=False)
        P.op("pe", lambda e, mL=mL, a=a: e.matmul(gps[:, 4:8], lhsT=mL, rhs=a, start=True, stop=True), reads=[kg("lm"), "avals"], writes=[kg("gps")], sig=False)
        P.op("pe", lambda e, a=a: e.matmul(gps[:, 8:12], lhsT=self.ones_f[:], rhs=a, start=True, stop=True), reads=["ones_f", "avals"], writes=[kg("gps")])
        P.op("act", lambda e, egs=egs: e.activation(out=egs[:, 0:12], in_=gps[:, 0:12], func=AF.Exp), reads=[kg("gps")], writes=[k("egs")])
        P.op("dve", lambda e, mL=mL, a=a, Lm=Lm: e.tensor_tensor(out=Lm[:], in0=mL.unsqueeze(1).broadcast_to([128, 4, 128]),
                                                                 in1=a.unsqueeze(2).broadcast_to([128, 4, 128]), op=ALU.mult),
             reads=[kg("lm"), "avals"], writes=[k("Lm")])
        P.op("pe", lambda e, d=d, Dps=Dps: e.matmul(Dps[:], lhsT=self.ident_bf[:], rhs=neg[:, d, :], start=True, stop=False),
             reads=["ident_bf", kg("neg")], writes=[k("Dps")], sig=False)
        for h in range(H):
            P.op("pe", lambda e, h=h, mR=mR, Dps=Dps, Lm=Lm: e.matmul(Dps[:, h * 128:(h + 1) * 128], lhsT=Lm[:, h, :], rhs=mR, start=False, stop=(h == H - 1)),
                 reads=[k("Lm"), kg("lm")], writes=[k("Dps")], sig=(h == H - 1))
        P.op("act", lambda e, ET=ET, Dps=Dps: e.activation(out=ET[:], in_=Dps[:], func=AF.Exp), reads=[k("Dps")], writes=[k("ET")])
        for gi, (g, heads) in enumerate(sgroups):
            P.op("pe", lambda e, gi=gi, g=g, c=c, Sps=Sps: e.matmul(Sps[:, gi * 128:(gi + 1) * 128], lhsT=KT(g, c), rhs=QT(g, c), start=True, stop=True),
                 reads=["qkT"], writes=[k("Sps")], sig=(gi == ng - 1))

    def stageB(step, d):
        k = lambda nm, d=d: tagp + nm + str(d)
        kg = lambda nm: tagp + nm
        Dps, Sps, egs, Lm, ET, PT, Vd, Vp, T1 = Dps_[d], Sps_[d], egs_[d], Lm_[d], ET_[d], PT_[d], Vd_[d], Vp_[d], T1_[d]
        c = step if d == 0 else NT - 1 - step
        a = a_ap(c, d)
        mL, mR = lm[:, d, :], lm[:, 2 + d, :]
        if rep == 1:
            P.op("dve", lambda e, PT=PT, ET=ET, Sps=Sps: e.tensor_tensor(out=PT[:], in0=ET[:], in1=Sps[:], op=ALU.mult), reads=[k("ET"), k("Sps")], writes=[k("PT")])
        else:
            P.op("dve", lambda e, PT=PT, ET=ET, Sps=Sps: e.tensor_tensor(
                out=PT[:].rearrange("p (g r t) -> p g r t", g=ng, r=rep), in0=ET[:].rearrange("p (g r t) -> p g r t", g=ng, r=rep),
                in1=Sps[:, 0:ng * 128].rearrange("p (g t) -> p g t", g=ng).unsqueeze(2).broadcast_to([128, ng, rep, 128]), op=ALU.mult),
                reads=[k("ET"), k("Sps")], writes=[k("PT")])
        build_vd(c, d, Vd, k("Vd"))
        P.op("pool", lambda e, Vp=Vp, Vd=Vd, egs=egs: e.tensor_tensor(out=Vp[:], in0=Vd[:], in1=egs[:, 4:8].unsqueeze(2).broadcast_to([128, H, pv]), op=ALU.mult),
             reads=[k("Vd"), k("egs")], writes=[k("Vp")])
        for h in range(H):
            P.op("pe", lambda e, h=h, PT=PT, Vd=Vd: e.matmul(Yps[:, h, :], lhsT=PT[:, h * 128:(h + 1) * 128], rhs=Vd[:, h, :], start=True, stop=True),
                 reads=[k("PT"), k("Vd")], writes=[kg("Yps")], sig=(h == H - 1))
        for h in range(H):
            r0, r1 = (0, 128) if ufull else strow(h)
            P.op("pe", lambda e, h=h, c=c, d=d, r0=r0, r1=r1: e.matmul(Ups[:, h, :], lhsT=QT(h if ng == H else h // rep, c), rhs=Stb[r0:r1, d, h, :],
                                                                       start=True, stop=True), reads=["qkT", k("Stb")], writes=[kg("Ups")], sig=(h == H - 1))
        P.op("dve", lambda e, T1=T1, egs=egs: e.tensor_tensor(out=T1[:], in0=Ups[:], in1=egs[:, 0:4].unsqueeze(2).broadcast_to([128, H, pv]), op=ALU.mult),
             reads=[kg("Ups"), k("egs")], writes=[k("T1")])
        P.op("dve", lambda e, T1=T1: e.tensor_tensor(out=T1[:], in0=T1[:], in1=Yps[:], op=ALU.add), reads=[k("T1"), kg("Yps")], writes=[k("T1")])
        accum(c, d, T1, k("T1"))
        for h in range(H):
            P.op("pe", lambda e, h=h, c=c, Vp=Vp: e.matmul(U2ps[:, h, :], lhsT=Ktm(h, c), rhs=Vp[:, h, :], start=True, stop=True),
                 reads=["ktm", k("Vp")], writes=[kg("U2ps")], sig=(h == H - 1))
        rows = sorted(set(strow(h) for h in range(H)))
        for (r0, r1) in rows:
            hs = [h for h in range(H) if strow(h) == (r0, r1)]
            h0, hstep = hs[0], (hs[1] - hs[0] if len(hs) > 1 else 1)
            sl = slice(h0, hs[-1] + 1, hstep)
            P.op("pool", lambda e, r0=r0, r1=r1, sl=sl, d=d, nh=len(hs), egs=egs: e.tensor_tensor(
                out=St[r0:r1, d, sl, :], in0=St[r0:r1, d, sl, :], in1=egs[r0:r1, 8:12][:, sl].unsqueeze(2).broadcast_to([r1 - r0, nh, pv]), op=ALU.mult),
                reads=[k("St"), k("egs")], writes=[k("St")])
            P.op("dve", lambda e, r0=r0, r1=r1, sl=sl, d=d: e.tensor_tensor(out=St[r0:r1, d, sl, :], in0=St[r0:r1, d, sl, :], in1=U2ps[r0:r1, sl, :], op=ALU.add),
                 reads=[k("St"), kg("U2ps")], writes=[k("St")])
            P.op("act", lambda e, r0=r0, r1=r1, sl=sl, d=d: e.activation(out=Stb[r0:r1, d, sl, :], in_=St[r0:r1, d, sl, :], func=AF.Copy),
                 reads=[k("St")], writes=[k("Stb")])


    items = [(step, d) for step in range(NT) for d in range(2)]
    stageA(*items[0])
    for n, it in enumerate(items):
        if n + 1 < len(items):
            stageA(*items[n + 1])
        stageB(*it)


def _mix_ssd(self, b, l):
    I, P = self.I, self.P
    with ExitStack() as sm:
        xs = self.sb(sm, "s_xs", [128, NT, 256], BF16)
        Btm = self.sb(sm, "s_Btm", [128, NT, 256], BF16)
        BT = self.sb(sm, "s_BT", [128, 2, L], BF16)
        CT = self.sb(sm, "s_CT", [128, 2, L], BF16)
        dt = self.sb(sm, "s_dt", [128, NT, 8])
        av = self.sb(sm, "s_av", [128, NT, 8])
        cbT = self.sb(sm, "s_cbT", [128, 6])
        cbrow = self.sb(sm, "s_cbrow", [1, 768])
        dtb = self.sb(sm, "s_dtb", [128, 8])
        Arow = self.sb(sm, "s_Arow", [128, 8])
        Drow = self.sb(sm, "s_Drow", [128, 4])
        grow = self.sb(sm, "s_grow", [128, 256])
        Yacc = self.sb(sm, "s_Yacc", [128, NT, 256])
        P.op("sp", lambda e: e.dma_start(out=cbT[:], in_=I["ssd_cbT"][l]), writes=["s_cbT"], dma="s_c")
        P.op("sp", lambda e: e.dma_start(out=cbrow[:], in_=I["ssd_conv_b"][l:l + 1, :]), writes=["cbrow"], dma="s_c")
        P.op("sp", lambda e: e.dma_start(out=dtb[:], in_=I["ssd_dt_bias"][l].partition_broadcast(128)), writes=["s_dtb"], dma="s_c")
        P.op("sp", lambda e: e.dma_start(out=Arow[:], in_=I["ssd_a_log"][l].partition_broadcast(128)), writes=["s_Arow"], dma="s_c")
        P.op("sp", lambda e: e.dma_start(out=Drow[:], in_=I["ssd_d"][l].partition_broadcast(128)), writes=["s_Drow"], dma="s_c")
        P.op("sp", lambda e: e.dma_start(out=grow[:], in_=I["ssd_norm_g"][l].partition_broadcast(128)), writes=["s_grow"], dma="s_c")
        P.op("act", lambda e: e.activation(out=Arow[:], in_=Arow[:], func=AF.Exp), reads=["s_Arow"], writes=["s_Arow"])
        P.op("dve", lambda e: e.tensor_scalar(out=Arow[:], in0=Arow[:], scalar1=-1.0, scalar2=None, op0=ALU.mult), reads=["s_Arow"], writes=["s_Arow"])
        with ExitStack() as st:
            def ev_tm(tag, i, ps, pk):
                if tag == "x":
                    P.op("act", lambda e: e.activation(out=xs[:, i, :], in_=ps, func=AF.Silu), reads=[pk], writes=["s_xs"])
                elif tag == "B":
                    P.op("act", lambda e: e.activation(out=Btm[:, i, :], in_=ps, func=AF.Silu), reads=[pk], writes=["ktm"])
                else:
                    P.op("dve", lambda e: e.tensor_tensor(out=dt[:, i, :], in0=ps, in1=dtb[:], op=ALU.add), reads=[pk, "s_dtb"], writes=["s_dt"])
            self.proj_tm(st, l, [(0, 0, 256, "x", cbrow[0:1, 0:256]), (0, 256, 256, "B", cbrow[0:1, 256:512]),
                                 (None, 1024, 8, "dt", None)], ev_tm)

            def ev_fm(tag, tb, ps, pk):
                dst = (BT if tag < 2 else CT)[:, tag % 2, tb * 512:(tb + 1) * 512]
                P.op("act", lambda e: e.activation(out=dst, in_=ps, func=AF.Silu, bias=cbT[:, 2 + tag:3 + tag]), reads=[pk, "s_cbT"], writes=["qkT"])
            self.proj_fm(st, l, [(0, 256 + 128 * t, 128, t) for t in range(4)], ev_fm)
            P.op("act", lambda e: e.activation(out=av[:], in_=dt[:], func=AF.Exp), reads=["s_dt"], writes=["avals"])
            P.op("act", lambda e: e.activation(out=dt[:], in_=av[:], func=AF.Ln, bias=1.0), reads=["avals"], writes=["s_dt"])
            P.op("dve", lambda e: e.tensor_tensor(out=av[:], in0=dt[:], in1=Arow[:].unsqueeze(1).broadcast_to([128, NT, 8]), op=ALU.mult),
                 reads=["s_dt", "s_Arow"], writes=["avals"])
            P.op("dve", lambda e: e.tensor_tensor(out=Yacc[:].rearrange("p i (h c) -> p i h c", h=4), in0=xs[:].rearrange("p i (h c) -> p i h c", h=4),
                                                  in1=Drow[:].unsqueeze(1).unsqueeze(3).broadcast_to([128, NT, 4, 64]), op=ALU.mult),
                 reads=["s_xs", "s_Drow"], writes=["s_Yacc"])
            P.flush()
        with ExitStack() as st:
            def build_vd(c, d, Vd, vk):
                P.op("dve", lambda e: e.tensor_tensor(out=Vd[:], in0=xs[:, c, :].rearrange("p (h c) -> p h c", h=4),
                                                      in1=dt[:, c, 4 * d:4 * d + 4].unsqueeze(2).broadcast_to([128, 4, 64]), op=ALU.mult),
                     reads=["s_xs", "s_dt"], writes=[vk])

            def accum(c, d, T1, tk):
                P.op("pool", lambda e: e.tensor_tensor(out=Yacc[:, c, :], in0=Yacc[:, c, :], in1=T1[:].rearrange("p h c -> p (h c)"), op=ALU.add),
                     reads=[tk, "s_Yacc"], writes=["s_Yacc"])
            self.sweep(st, "ss_", 128, 64, [(0, [0, 1]), (1, [2, 3])],
                       KT=lambda g, c: BT[:, g, c * 128:(c + 1) * 128], QT=lambda g, c: CT[:, g, c * 128:(c + 1) * 128],
                       Ktm=lambda h, c: Btm[:, c, (h // 2) * 128:(h // 2 + 1) * 128], strow=lambda h: (0, 128),
                       a_ap=lambda c, d: av[:, c, 4 * d:4 * d + 4], build_vd=build_vd, accum=accum)
            P.flush()
        with ExitStack() as st:
            epi = self.epilogue(st, "s_")
            zt = [self.sb(st, f"s_zt{i}", [128, 256]) for i in range(3)]

            def ev_z(tag, i, ps, pk):
                z, zk = zt[i % 3], ("s_zt", i % 3)
                P.op("act", lambda e: e.activation(out=z[:], in_=ps, func=AF.Silu), reads=[pk], writes=[zk])
                P.op("dve", lambda e: e.tensor_tensor(out=z[:], in0=z[:], in1=Yacc[:, i, :], op=ALU.mult), reads=[zk, "s_Yacc"], writes=[zk])
                epi(i, z[:], zk, grow[:], "s_grow")
            self.proj_tm(st, l, [(None, 0, 256, "z", None)], ev_z, tagp="pz")
            epi.finish()
            P.flush()


KB.phase0_wcv = _phase0_wcv
KB.proj_tm = _proj_tm
KB.proj_fm = _proj_fm
KB.epilogue = _epilogue
KB.to_yT = _to_yT
KB.sweep = _sweep
KB.mix_ssd = _mix_ssd


def _mix_ml(self, b, l):
    I, P = self.I, self.P
    base = OFF_ML
    with ExitStack() as sm:
        qT = self.sb(sm, "m_qT", [128, 2, L], BF16)
        kT = self.sb(sm, "m_kT", [128, 4, L], BF16)
        P.op("pool", lambda e: e.memset(kT[:], 0.0), writes=["qkT"])
        Ktm = self.sb(sm, "m_Ktm", [128, NT, 256], BF16)
        vtm = self.sb(sm, "m_vtm", [128, NT, 256], BF16)
        gt = self.sb(sm, "m_gt", [128, NT, 16])
        ei = self.sb(sm, "m_ei", [128, NT, 8])
        av = self.sb(sm, "m_av", [128, NT, 8])
        ib = self.sb(sm, "m_ib", [128, 8])
        fb = self.sb(sm, "m_fb", [128, 8])
        grow = self.sb(sm, "m_grow", [128, 256])
        Hacc = self.sb(sm, "m_Hacc", [128, NT, 256])
        P.op("sp", lambda e: e.dma_start(out=ib[:], in_=I["ml_i_bias"][l].partition_broadcast(128)), writes=["m_ib"], dma="m_c")
        P.op("sp", lambda e: e.dma_start(out=fb[:], in_=I["ml_f_bias"][l].partition_broadcast(128)), writes=["m_fb"], dma="m_c")
        P.op("sp", lambda e: e.dma_start(out=grow[:], in_=I["ml_norm_g"][l].partition_broadcast(128)), writes=["m_grow"], dma="m_c")
        P.op("pool", lambda e: e.memset(Hacc[:], 0.0), writes=["m_Hacc"])
        with ExitStack() as st:
            def ev_fm(tag, tb, ps, pk):
                sl = slice(tb * 512, (tb + 1) * 512)
                if tag < 2:
                    P.op("act", lambda e: e.activation(out=qT[:, tag, sl], in_=ps, func=AF.Copy), reads=[pk], writes=["qkT"])
                else:
                    pair = tag - 2
                    P.op("act", lambda e: e.activation(out=kT[0:64, 2 * pair, sl], in_=ps[0:64, :], func=AF.Copy), reads=[pk], writes=["qkT"])
                    P.op("dve", lambda e: e.tensor_copy(out=kT[64:128, 2 * pair + 1, sl], in_=ps[64:128, :]), reads=[pk], writes=["qkT"])
            self.proj_fm(st, l, [(None, base + 128 * t, 128, t) for t in range(4)], ev_fm)

            def ev_tm(tag, i, ps, pk):
                if tag == "k":
                    P.op("act", lambda e: e.activation(out=Ktm[:, i, :], in_=ps, func=AF.Copy), reads=[pk], writes=["ktm"])
                elif tag == "v":
                    P.op("dve", lambda e: e.tensor_copy(out=vtm[:, i, :], in_=ps), reads=[pk], writes=["m_vtm"])
                else:
                    P.op("dve", lambda e: e.tensor_copy(out=gt[:, i, :], in_=ps), reads=[pk], writes=["m_gt"])
            self.proj_tm(st, l, [(None, base + 256, 256, "k", None), (None, base + 512, 256, "v", None),
                                 (None, base + 1024, 16, "g", None)], ev_tm)
            P.op("dve", lambda e: e.tensor_tensor(out=ei[:], in0=gt[:, :, 0:8], in1=ib[:].unsqueeze(1).broadcast_to([128, NT, 8]), op=ALU.add),
                 reads=["m_gt", "m_ib"], writes=["m_ei"])
            P.op("act", lambda e: e.activation(out=ei[:], in_=ei[:], func=AF.Exp), reads=["m_ei"], writes=["m_ei"])
            P.op("dve", lambda e: e.tensor_scalar(out=ei[:], in0=ei[:], scalar1=0.125, scalar2=None, op0=ALU.mult), reads=["m_ei"], writes=["m_ei"])
            P.op("dve", lambda e: e.tensor_tensor(out=av[:], in0=gt[:, :, 8:16], in1=fb[:].unsqueeze(1).broadcast_to([128, NT, 8]), op=ALU.add),
                 reads=["m_gt", "m_fb"], writes=["avals"])
            P.op("act", lambda e: e.activation(out=av[:], in_=av[:], func=AF.Exp, scale=-1.0), reads=["avals"], writes=["avals"])
            P.op("act", lambda e: e.activation(out=av[:], in_=av[:], func=AF.Ln, bias=1.0), reads=["avals"], writes=["avals"])
            P.op("dve", lambda e: e.tensor_scalar(out=av[:], in0=av[:], scalar1=-1.0, scalar2=None, op0=ALU.mult), reads=["avals"], writes=["avals"])
            P.flush()
        with ExitStack() as st:
            dd_ = [self.sb(st, f"m_dd{i}", [128, 4, 1]) for i in range(2)]
            hh_ = [self.sb(st, f"m_hh{i}", [128, 4, 64]) for i in range(2)]

            def build_vd(c, d, Vd, vk):
                P.op("dve", lambda e: e.tensor_tensor(out=Vd[:, :, 0:64], in0=vtm[:, c, :].rearrange("p (h c) -> p h c", h=4),
                                                      in1=ei[:, c, 4 * d:4 * d + 4].unsqueeze(2).broadcast_to([128, 4, 64]), op=ALU.mult),
                     reads=["m_vtm", "m_ei"], writes=[vk])
                P.op("act", lambda e: e.activation(out=Vd[:, :, 64:65], in_=ei[:, c, 4 * d:4 * d + 4].unsqueeze(2), func=AF.Copy),
                     reads=["m_ei"], writes=[vk])

            def accum(c, d, T1, tk):
                dd, hh, dk, hk = dd_[d], hh_[d], f"m_dd{d}", f"m_hh{d}"
                den = T1[:, :, 64:65]
                P.op("dve", lambda e: e.scalar_tensor_tensor(out=dd[:], in0=den, scalar=-1.0, in1=den, op0=ALU.mult, op1=ALU.max),
                     reads=[tk], writes=[dk])
                P.op("dve", lambda e: e.tensor_scalar_max(out=dd[:], in0=dd[:], scalar1=1.0), reads=[dk], writes=[dk])
                P.op("dve", lambda e: e.reciprocal(out=dd[:], in_=dd[:]), reads=[dk], writes=[dk])
                P.op("dve", lambda e: e.tensor_tensor(out=hh[:], in0=T1[:, :, 0:64], in1=dd[:].broadcast_to([128, 4, 64]), op=ALU.mult),
                     reads=[tk, dk], writes=[hk])
                P.op("pool", lambda e: e.tensor_tensor(out=Hacc[:, c, :], in0=Hacc[:, c, :], in1=hh[:].rearrange("p h c -> p (h c)"), op=ALU.add),
                     reads=[hk, "m_Hacc"], writes=["m_Hacc"])
            pr = lambda h: ((h % 2) * 64, (h % 2) * 64 + 64)
            self.sweep(st, "ms_", 64, 65, [(h, [h]) for h in range(4)],
                       KT=lambda h, c: kT[:, h, c * 128:(c + 1) * 128],
                       QT=lambda h, c: qT[:, h // 2, c * 128:(c + 1) * 128],
                       Ktm=lambda h, c: Ktm[:, c, (h // 2) * 128:(h // 2 + 1) * 128], strow=pr,
                       a_ap=lambda c, d: av[:, c, 4 * d:4 * d + 4], build_vd=build_vd, accum=accum, ufull=True)
            P.flush()
        with ExitStack() as st:
            junk = self.sb(st, "m_junk", [128, 256])
            ss4 = self.sb(st, "m_ss4", [128, NT, 4])
            sg = [self.sb(st, f"m_sg{i}", [128, 256]) for i in range(2)]
            yb = [self.sb(st, f"m_yb{i}", [128, 256], BF16) for i in range(3)]
            mpend = []
            tp = [self.ps(st, f"m_tp{i}", [128, 256], BF16) for i in range(2)]

            def ev_o(tag, i, ps, pk):
                s, sk = sg[i % 2], ("m_sg", i % 2)
                y, yk = yb[i % 3], ("m_yb", i % 3)
                P.op("act", lambda e: e.activation(out=s[:], in_=ps, func=AF.Sigmoid), reads=[pk], writes=[sk])
                P.op("act", lambda e: e.activation(out=junk[:], in_=Hacc[:, i, :], func=AF.Square), reads=["m_Hacc"], writes=["m_junk"])
                P.op("dve", lambda e: e.reduce_sum(out=ss4[:, i, :], in_=junk[:].rearrange("p (h c) -> p h c", h=4), axis=AX.X),
                     reads=["m_junk"], writes=[("m_ss4", i)])
                P.op("act", lambda e: e.activation(out=ss4[:, i, :], in_=ss4[:, i, :], func=AF.Sqrt, bias=1e-6, scale=1.0 / 64),
                     reads=[("m_ss4", i)], writes=[("m_ss4", i)])
                P.op("dve", lambda e: e.reciprocal(out=ss4[:, i, :], in_=ss4[:, i, :]), reads=[("m_ss4", i)], writes=[("m_ss4", i)])
                P.op("dve", lambda e: e.tensor_tensor(out=s[:], in0=s[:], in1=grow[:], op=ALU.mult), reads=[sk, "m_grow"], writes=[sk])
                P.op("dve", lambda e: e.tensor_tensor(out=s[:].rearrange("p (h c) -> p h c", h=4), in0=s[:].rearrange("p (h c) -> p h c", h=4),
                                                      in1=ss4[:, i, :].unsqueeze(2).broadcast_to([128, 4, 64]), op=ALU.mult),
                     reads=[sk, ("m_ss4", i)], writes=[sk])
                P.op("dve", lambda e: e.tensor_tensor(out=y[:], in0=s[:], in1=Hacc[:, i, :], op=ALU.mult), reads=[sk, "m_Hacc"], writes=[yk])
                while mpend:
                    self.to_yT(*mpend.pop(0))
                mpend.append((i, y, yk, tp[i % 2], ("m_tp", i % 2)))
            self.proj_tm(st, l, [(None, base + 768, 256, "o", None)], ev_o, tagp="po")
            while mpend:
                self.to_yT(*mpend.pop(0))
            P.flush()


def _mix_na(self, b, l):
    I, P = self.I, self.P
    base = OFF_NA
    with ExitStack() as sm:
        qT = self.sb(sm, "a_qT", [128, 2, L], BF16)
        kT = self.sb(sm, "a_kT", [128, 2, L], BF16)
        Ve = self.sb(sm, "a_Ve", [128, NT, 256], BF16)
        Vo = self.sb(sm, "a_Vo", [128, NT - 1, 256], BF16)
        yna = self.sb(sm, "a_y", [128, NT, 256])
        mask = self.sb(sm, "a_mask", [128, 512], BF16)
        grow = self.sb(sm, "a_grow", [128, 256])
        P.op("sp", lambda e: e.dma_start(out=mask[:], in_=I["na_mask"]), writes=["a_mask"], dma="a_c")
        P.op("sp", lambda e: e.dma_start(out=grow[:], in_=I["na_norm_g"][l].partition_broadcast(128)), writes=["a_grow"], dma="a_c")
        with ExitStack() as st:
            def ev_fm(tag, tb, ps, pk):
                dst = (qT if tag < 2 else kT)[:, tag % 2, tb * 512:(tb + 1) * 512]
                P.op("act", lambda e: e.activation(out=dst, in_=ps, func=AF.Copy, scale=(0.125 if tag < 2 else 1.0)), reads=[pk], writes=["a_qk"])
            self.proj_fm(st, l, [(None, base + 128 * t, 128, t) for t in range(4)], ev_fm)
            self.proj_tm(st, l, [(None, base + 512, 256, "v", None)],
                         lambda tag, i, ps, pk: P.op("act", lambda e: e.activation(out=Ve[:, i, :], in_=ps, func=AF.Copy), reads=[pk], writes=["a_V"]),
                         tagp="pve")
            P.flush()
        with ExitStack() as st:
            self.proj_tm(st, l, [(None, base + 512, 256, "v", None)],
                         lambda tag, i, ps, pk: P.op("act", lambda e: e.activation(out=Vo[:, i, :], in_=ps, func=AF.Copy), reads=[pk], writes=["a_V"]),
                         tiles=range(NT - 1), tok_off=64, tagp="pvo")
            P.flush()
        with ExitStack() as st:
            bsl = Slots("a_b", [self.sb(st, f"a_b{i}", [128, 4, 512], BF16) for i in range(2)])
            NB = 4
            Sps = [self.ps(st, f"a_S{i}", [128, 512]) for i in range(NB)]
            PTall = self.ps(st, "a_PTall", [128, 2048], BF16)
            PTp = [PTall[:, i * 512:(i + 1) * 512] for i in range(NB)]
            Oall = self.ps(st, "a_Oall", [128, 512])
            Ops = [Oall[:, i * 64:(i + 1) * 64] for i in range(NB)]
            Pb = [self.sb(st, f"a_P{i}", [128, 512], BF16) for i in range(NB)]
            PTs = [self.sb(st, f"a_PT{i}", [128, 512], BF16) for i in range(NB)]
            nmx = self.sb(st, "a_nmx", [128, NB])
            rsum = self.sb(st, "a_rs", [128, NB])
            n = 0
            last_dl, bt, bk = None, None, None
            for r in range(32):
                r0 = min(max(r - 4, 0), 24)
                dl = r0 - r + 7
                if dl != last_dl:
                    bt, bk = bsl.next()
                    P.op("pool", lambda e, bt=bt, dl=dl: e.dma_start(out=bt[:], in_=I["na_bias"][l, dl]), writes=[bk], dma=bk)
                    last_dl = dl
                ti, prow = r // 2, (r % 2) * 64
                Vt, v0 = (Ve, r0 // 2) if r0 % 2 == 0 else (Vo, (r0 - 1) // 2)
                for h in range(4):
                    p0 = (h % 2) * 64
                    u = n % NB
                    n += 1
                    S, Sk = Sps[u], ("a_S", u)
                    P.op("pe", lambda e, S=S, p0=p0, h=h, ti=ti, r0=r0: e.matmul(
                        S[:], lhsT=qT[p0:p0 + 64, h // 2, ti * 128:(ti + 1) * 128], rhs=kT[p0:p0 + 64, h // 2, 64 * r0:64 * r0 + 512],
                        start=True, stop=False), reads=["a_qk"], writes=[Sk], sig=False)
                    P.op("pe", lambda e, S=S, bt=bt, h=h: e.matmul(S[:], lhsT=self.ident_bf[:], rhs=bt[:, h, :], start=False, stop=False),
                         reads=["ident_bf", bk], writes=[Sk], sig=False)
                    P.op("pe", lambda e, S=S: e.matmul(S[:], lhsT=self.ident_bf[:], rhs=mask[:], start=False, stop=True),
                         reads=["ident_bf", "a_mask"], writes=[Sk])
                    P.op("dve", lambda e, S=S, u=u: e.reduce_max(out=nmx[:, u:u + 1], in_=S[:], axis=AX.X, negate=True), reads=[Sk], writes=[("a_nmx", u)])
                    P.op("act", lambda e, S=S, u=u: e.activation(out=Pb[u][:], in_=S[:], func=AF.Exp, bias=nmx[:, u:u + 1], accum_out=rsum[:, u:u + 1]),
                         reads=[Sk, ("a_nmx", u)], writes=[("a_P", u), ("a_rs", u)])
                    for kb in range(4):
                        P.op("pe", lambda e, u=u, kb=kb: e.transpose(PTp[u][:, kb * 128:(kb + 1) * 128], Pb[u][:, kb * 128:(kb + 1) * 128], self.ident_bf[:]),
                             reads=[("a_P", u), "ident_bf"], writes=[("a_PTp", u)], sig=(kb == 3))
                    P.op("dve", lambda e, u=u: e.tensor_copy(out=PTs[u][:], in_=PTp[u]), reads=[("a_PTp", u)], writes=[("a_PT", u)])
                    for kb in range(4):
                        P.op("pe", lambda e, u=u, kb=kb, Vt=Vt, v0=v0, h=h: e.matmul(
                            Ops[u], lhsT=PTs[u][:, kb * 128:(kb + 1) * 128], rhs=Vt[:, v0 + kb, h * 64:(h + 1) * 64], start=(kb == 0), stop=(kb == 3)),
                            reads=[("a_PT", u), "a_V"], writes=[("a_O", u)], sig=(kb == 3))
                    P.op("dve", lambda e, u=u: e.reciprocal(out=rsum[:, u:u + 1], in_=rsum[:, u:u + 1]), reads=[("a_rs", u)], writes=[("a_rs", u)])
                    P.op("act", lambda e, u=u, prow=prow, ti=ti, h=h: e.activation(
                        out=yna[prow:prow + 64, ti, h * 64:(h + 1) * 64], in_=Ops[u][prow:prow + 64, :], func=AF.Copy, scale=rsum[prow:prow + 64, u:u + 1]),
                        reads=[("a_O", u), ("a_rs", u)], writes=["a_y"])
            P.flush()
        with ExitStack() as st:
            epi = self.epilogue(st, "a_")
            for i in range(NT):
                epi(i, yna[:, i, :], "a_y", grow[:], "a_grow")
            epi.finish()
            P.flush()


KB.mix_ml = _mix_ml
KB.mix_na = _mix_na


def _phase0_hyena(self):
    I, P = self.I, self.P
    M = 12582912.0
    for l in range(2):
        with ExitStack() as sm:
            hn = self.sb(sm, "h0_hn", [128, NT, 1024], BF16)
            with ExitStack() as st:
                featsT = self.sb(st, "h0_f", [33, L])
                w1 = self.sb(st, "h0_w1", [33, 64]); w2 = self.sb(st, "h0_w2", [64, 64]); w3 = self.sb(st, "h0_w3", [64, 1024])
                bb = self.sb(st, "h0_bb", [64, 2]); fq = self.sb(st, "h0_fq", [64, 2])
                dec = self.sb(st, "h0_dec", [128, NT, 256])
                hT_ = [self.sb(st, f"h0_hT{i}", [64, L]) for i in range(2)]
                arg = self.sb(st, "h0_arg", [64, 512]); t1 = self.sb(st, "h0_t1", [64, 512])
                hw = self.sb(st, "h0_hw", [128, NT, 1024])
                sq = self.sb(st, "h0_sq", [128, 1024])
                rn = self.sb(st, "h0_rn", [128, 2, 256])
                mp = self.ps(st, "h0_mp", [64, 512])
                hp = self.ps(st, "h0_hp", [128, 1024])
                ssq = self.ps(st, "h0_ssq", [128, 1024])
                ld = lambda t, src, key: P.op("sp", lambda e: e.dma_start(out=t, in_=src), writes=[key], dma="h0c")
                ld(featsT[:], I["featsT"], "h0_f"); ld(w1[:], I["hy_w1"][l], "h0_w"); ld(w2[:], I["hy_w2"][l], "h0_w"); ld(w3[:], I["hy_w3"][l], "h0_w")
                ld(bb[:, 0:1], I["hy_b1"][l], "h0_w"); ld(bb[:, 1:2], I["hy_b2"][l], "h0_w")
                ld(fq[:, 0:1], I["hy_freq"][l, 0], "h0_w"); ld(fq[:, 1:2], I["hy_freq"][l, 1], "h0_w"); ld(dec[:], I["decay"], "h0_dec")
                for layer in range(2):
                    for tb in range(4):
                        sl = slice(tb * 512, (tb + 1) * 512)
                        if layer == 0:
                            P.op("pe", lambda e, sl=sl: e.matmul(mp[:], lhsT=w1[:], rhs=featsT[:, sl], start=True, stop=True), reads=["h0_w", "h0_f"], writes=["h0_mp"])
                        else:
                            P.op("pe", lambda e, sl=sl: e.matmul(mp[:], lhsT=w2[:], rhs=hT_[0][:, sl], start=True, stop=True), reads=["h0_w", "h0_hT0"], writes=["h0_mp"])
                        P.op("dve", lambda e, layer=layer: e.tensor_scalar(out=arg[:], in0=mp[:], scalar1=bb[:, layer:layer + 1], scalar2=fq[:, layer:layer + 1],
                                                                            op0=ALU.add, op1=ALU.mult), reads=["h0_mp", "h0_w"], writes=["h0_arg"])
                        P.op("dve", lambda e: e.tensor_scalar(out=t1[:], in0=arg[:], scalar1=1.0 / (2 * PI), scalar2=M, op0=ALU.mult, op1=ALU.add),
                             reads=["h0_arg"], writes=["h0_t1"])
                        P.op("dve", lambda e: e.tensor_scalar(out=t1[:], in0=t1[:], scalar1=-M, scalar2=-2 * PI, op0=ALU.add, op1=ALU.mult),
                             reads=["h0_t1"], writes=["h0_t1"])
                        P.op("dve", lambda e: e.tensor_tensor(out=arg[:], in0=arg[:], in1=t1[:], op=ALU.add), reads=["h0_arg", "h0_t1"], writes=["h0_arg"])
                        P.op("act", lambda e, layer=layer, sl=sl: e.activation(out=hT_[layer][:, sl], in_=arg[:], func=AF.Sin), reads=["h0_arg"], writes=[f"h0_hT{layer}"])
                for tt in range(NT):
                    for half in range(2):
                        P.op("pe", lambda e, tt=tt, half=half: e.matmul(hp[:, half * 512:(half + 1) * 512], lhsT=hT_[1][:, tt * 128:(tt + 1) * 128],
                                                                        rhs=w3[:, half * 512:(half + 1) * 512], start=True, stop=True),
                             reads=["h0_hT1", "h0_w"], writes=["h0_hp"], sig=(half == 1))
                    P.op("dve", lambda e, tt=tt: e.tensor_tensor(out=hw[:, tt, :].rearrange("p (a c) -> p a c", a=4), in0=hp[:].rearrange("p (a c) -> p a c", a=4),
                                                                  in1=dec[:, tt, :].unsqueeze(1).broadcast_to([128, 4, 256]), op=ALU.mult),
                         reads=["h0_hp", "h0_dec"], writes=["h0_hw"])
                    P.op("act", lambda e, tt=tt: e.activation(out=sq[:], in_=hw[:, tt, :], func=AF.Square), reads=["h0_hw"], writes=["h0_sq"])
                    for half in range(2):
                        P.op("pe", lambda e, tt=tt, half=half: e.matmul(ssq[:, half * 512:(half + 1) * 512], lhsT=self.ones_f[:], rhs=sq[:, half * 512:(half + 1) * 512],
                                                                        start=(tt == 0), stop=(tt == NT - 1)), reads=["ones_f", "h0_sq"], writes=["h0_ssq"], sig=(half == 1))
                P.op("act", lambda e: e.activation(out=sq[:], in_=ssq[:], func=AF.Copy), reads=["h0_ssq"], writes=["h0_sq"])
                sv = sq[:].rearrange("p (o d c) -> p o d c", o=2, d=2)
                P.op("dve", lambda e: e.tensor_tensor(out=rn[:], in0=sv[:, :, 0, :], in1=sv[:, :, 1, :], op=ALU.add), reads=["h0_sq"], writes=["h0_rn"])
                P.op("act", lambda e: e.activation(out=rn[:], in_=rn[:], func=AF.Sqrt, bias=1e-6), reads=["h0_rn"], writes=["h0_rn"])
                P.op("dve", lambda e: e.reciprocal(out=rn[:], in_=rn[:]), reads=["h0_rn"], writes=["h0_rn"])
                for tt in range(NT):
                    P.op("dve" if tt % 2 else "pool", lambda e, tt=tt: e.tensor_tensor(
                        out=hn[:, tt, :].rearrange("p (o d c) -> p o d c", o=2, d=2), in0=hw[:, tt, :].rearrange("p (o d c) -> p o d c", o=2, d=2),
                        in1=rn[:].unsqueeze(2).broadcast_to([128, 2, 2, 256]), op=ALU.mult), reads=["h0_hw", "h0_rn"], writes=["h0_hn"])
                P.op("dve", lambda e: e.memset(hn[0:1, 0, :].rearrange("p (o d c) -> p o d c", o=2, d=2)[:, :, 1, :], 0.0), reads=["h0_hn"], writes=["h0_hn"])
                if l == 0:
                    self.dump("hn", hn[:], "h0_hn")
                    self.dump("hw", hw[:], "h0_hw")
                    self.dump("h2T", hT_[1][:], "h0_hT1")
                P.flush()
            with ExitStack() as st:
                fsl = Slots("h0_F", [self.sb(st, f"h0_F{i}", [128, NT, 128], BF16) for i in range(3)])
                KP = [self.ps(st, f"h0_KP{i}", [128, 1024]) for i in range(2)]
                ksb = [self.sb(st, f"h0_ks{i}", [128, 1024]) for i in range(2)]
                tab = self.sb(st, "h0_tab", [128, 2, 3, 256])
                sc = 2.0 / 4096.0
                for j in range(NT):
                    for part in range(2):
                        r = j + 16 * part
                        f, fk = fsl.next()
                        P.op("sp", lambda e, f=f, r=r: e.dma_start(out=f[:], in_=I["FTt"][r]), writes=[fk], dma=fk)
                        for kk in range(NT):
                            for half in range(2):
                                P.op("pe", lambda e, f=f, kk=kk, half=half, part=part: e.matmul(
                                    KP[part][:, half * 512:(half + 1) * 512], lhsT=f[:, kk, :], rhs=hn[:, kk, half * 512:(half + 1) * 512],
                                    start=(kk == 0), stop=(kk == NT - 1)), reads=[fk, "h0_hn"], writes=[("h0_KP", part)], sig=(kk == NT - 1 and half == 1))
                        P.op("act", lambda e, part=part: e.activation(out=ksb[part][:], in_=KP[part][:], func=AF.Copy, scale=sc),
                             reads=[("h0_KP", part)], writes=[("h0_ks", part)])
                    kre = ksb[0][:].rearrange("p (o d c) -> p o d c", o=2, d=2)
                    kim = ksb[1][:].rearrange("p (o d c) -> p o d c", o=2, d=2)
                    P.op("dve", lambda e, kre=kre: e.tensor_tensor(out=tab[:, :, 0, :], in0=kre[:, :, 0, :], in1=kre[:, :, 1, :], op=ALU.add),
                         reads=[("h0_ks", 0)], writes=["h0_tab"])
                    P.op("dve", lambda e, kim=kim: e.tensor_tensor(out=tab[:, :, 1, :], in0=kim[:, :, 0, :], in1=kim[:, :, 1, :], op=ALU.subtract),
                         reads=[("h0_ks", 1)], writes=["h0_tab"])
                    P.op("dve", lambda e: e.tensor_copy(out=tab[:, :, 2, :], in_=tab[:, :, 0, :]), reads=["h0_tab"], writes=["h0_tab"])
                    if j == 0:
                        P.op("dve", lambda e, kim=kim: e.tensor_tensor(out=tab[0:1, :, 2, :], in0=kim[0:1, :, 0, :], in1=kim[0:1, :, 1, :], op=ALU.add),
                             reads=[("h0_ks", 1), "h0_tab"], writes=["h0_tab"])
                        P.op("dve", lambda e: e.tensor_scalar(out=tab[0:1, :, 2, :], in0=tab[0:1, :, 2, :], scalar1=0.5, scalar2=None, op0=ALU.mult),
                             reads=["h0_tab"], writes=["h0_tab"])
                        P.op("dve", lambda e: e.tensor_scalar(out=tab[0:1, :, 0, :], in0=tab[0:1, :, 0, :], scalar1=0.5, scalar2=None, op0=ALU.mult),
                             reads=["h0_tab"], writes=["h0_tab"])
                        P.op("dve", lambda e: e.memset(tab[0:1, :, 1, :], 0.0), reads=["h0_tab"], writes=["h0_tab"])
                    for o in range(2):
                        P.op("sp", lambda e, o=o, j=j: e.dma_start(out=self.kfs[l, o, j], in_=tab[:, o, :, :]), reads=["h0_tab"], writes=["kfs"], dma="kfst")
                        if l == 0 and "kfs" in self.dbg_out:
                            P.op("sp", lambda e, o=o, j=j: e.dma_start(out=self.dbg_out["kfs"][o, j], in_=tab[:, o, :, :]), reads=["h0_tab"], dma="dbg2")
                P.flush()


def _mix_hy(self, b, l):
    I, P = self.I, self.P
    with ExitStack() as sm:
        z = self.sb(sm, "y_z", [128, NT, 256], BF16)
        g1 = self.sb(sm, "y_g1", [128, NT, 256], BF16)
        g2 = self.sb(sm, "y_g2", [128, NT, 256], BF16)
        cbrow = self.sb(sm, "y_cbrow", [1, 768])
        skb = self.sb(sm, "y_skb", [128, 2, 256])
        grow = self.sb(sm, "y_grow", [128, 256])
        P.op("sp", lambda e: e.dma_start(out=cbrow[:], in_=I["hy_conv_b"][l:l + 1, :]), writes=["cbrow"], dma="y_c")
        P.op("sp", lambda e: e.dma_start(out=skb[:].rearrange("p a b -> p (a b)"), in_=I["hy_skip"][l].rearrange("a b -> (a b)").partition_broadcast(128)),
             writes=["y_skb"], dma="y_c")
        P.op("sp", lambda e: e.dma_start(out=grow[:], in_=I["hy_norm_g"][l].partition_broadcast(128)), writes=["y_grow"], dma="y_c")
        with ExitStack() as st:
            dsts = {"v": z, "x1": g1, "x2": g2}

            def ev(tag, i, ps, pk):
                P.op("act", lambda e: e.activation(out=dsts[tag][:, i, :], in_=ps, func=AF.Copy), reads=[pk], writes=["y_" + tag])
            self.proj_tm(st, l, [(1, 0, 256, "v", cbrow[0:1, 0:256]), (1, 256, 256, "x1", cbrow[0:1, 256:512]),
                                 (1, 512, 256, "x2", cbrow[0:1, 512:768])], ev)
            P.flush()
        with ExitStack() as st:
            Y = self.sb(st, "y_Y", [128, 32, 256], BF16)
            fsl = Slots("y_F", [self.sb(st, f"y_F{i}", [128, NT, 128], BF16) for i in range(4)])
            ksl = Slots("y_k", [self.sb(st, f"y_k{i}", [128, 3, 256]) for i in range(2)])
            Zp = [self.ps(st, f"y_Zp{i}", [128, 256]) for i in range(2)]
            Cp = [self.ps(st, f"y_Cp{i}", [128, 256]) for i in range(2)]
            zs = [self.sb(st, f"y_zs{i}", [128, 256]) for i in range(2)]
            tt_ = [self.sb(st, f"y_t{i}", [128, 256]) for i in range(4)]
            zf = [self.sb(st, f"y_zf{i}", [128, 256]) for i in range(3)]
            epi = self.epilogue(st, "y_")
            if b == 0 and l == 0:
                self.dump("z0", z[:], "y_v")
                self.dump("g1", g1[:], "y_x1")
            for o in range(2):
                if o == 1 and b == 0 and l == 0:
                    self.dump("z1", z[:], "y_v")
                gate = g1 if o == 0 else g2
                gk = "y_x1" if o == 0 else "y_x2"
                for j in range(NT):
                    kt, kk_ = ksl.next()
                    P.op("sp", lambda e, kt=kt, o=o, j=j: e.dma_start(out=kt[:], in_=self.kfs[l, o, j]), reads=["kfs"], writes=[kk_], dma=kk_)
                    for part in range(2):
                        f, fk = fsl.next()
                        P.op("sp", lambda e, f=f, j=j, part=part: e.dma_start(out=f[:], in_=I["FTt"][j + 16 * part]), writes=[fk], dma=fk)
                        for kk in range(NT):
                            P.op("pe", lambda e, f=f, kk=kk, part=part: e.matmul(Zp[part][:], lhsT=f[:, kk, :], rhs=z[:, kk, :], start=(kk == 0), stop=(kk == NT - 1)),
                                 reads=[fk, "y_v"], writes=[("y_Zp", part)], sig=(kk == NT - 1))
                        P.op("act", lambda e, part=part: e.activation(out=zs[part][:], in_=Zp[part][:], func=AF.Copy), reads=[("y_Zp", part)], writes=[("y_zs", part)])
                    for n_, (src, tb_) in enumerate(((0, 0), (1, 1), (0, 1), (1, 2))):
                        P.op("dve", lambda e, n_=n_, src=src, tb_=tb_, kt=kt: e.tensor_tensor(out=tt_[n_][:], in0=zs[src][:], in1=kt[:, tb_, :], op=ALU.mult),
                             reads=[("y_zs", src), kk_], writes=[("y_t", n_)])
                    P.op("pool", lambda e, j=j: e.tensor_tensor(out=Y[:, j, :], in0=tt_[0][:], in1=tt_[1][:], op=ALU.subtract),
                         reads=[("y_t", 0), ("y_t", 1)], writes=["y_Y"])
                    P.op("pool", lambda e, j=j: e.tensor_tensor(out=Y[:, 16 + j, :], in0=tt_[2][:], in1=tt_[3][:], op=ALU.add),
                         reads=[("y_t", 2), ("y_t", 3)], writes=["y_Y"])
                if o == 0 and b == 0 and l == 0:
                    self.dump("Y0", Y[:], "y_Y")
                for i in range(NT):
                    C, Ck = Cp[i % 2], ("y_Cp", i % 2)
                    for hf in range(2):
                        f, fk = fsl.next()
                        P.op("sp", lambda e, f=f, i=i, hf=hf: e.dma_start(out=f[:], in_=I["Ft"][i, :, hf * 16:(hf + 1) * 16, :]), writes=[fk], dma=fk)
                        for rr in range(16):
                            r = hf * 16 + rr
                            P.op("pe", lambda e, f=f, rr=rr, r=r, C=C: e.matmul(C[:], lhsT=f[:, rr, :], rhs=Y[:, r, :], start=(r == 0), stop=(r == 31)),
                                 reads=[fk, "y_Y"], writes=[Ck], sig=(rr == 15))
                    t_, tk = tt_[i % 2], ("y_t", i % 2)
                    P.op("dve", lambda e, t_=t_, i=i, o=o: e.tensor_tensor(out=t_[:], in0=z[:, i, :], in1=skb[:, o, :], op=ALU.mult),
                         reads=["y_v", "y_skb"], writes=[tk])
                    P.op("dve", lambda e, t_=t_, C=C: e.tensor_tensor(out=t_[:], in0=t_[:], in1=C[:], op=ALU.add), reads=[tk, Ck], writes=[tk])
                    if o == 0:
                        P.op("pool", lambda e, t_=t_, i=i, gate=gate: e.tensor_tensor(out=z[:, i, :], in0=t_[:], in1=gate[:, i, :], op=ALU.mult),
                             reads=[tk, gk], writes=["y_v"])
                    else:
                        zz, zk = zf[i % 3], ("y_zf", i % 3)
                        P.op("pool", lambda e, t_=t_, i=i, zz=zz, gate=gate: e.tensor_tensor(out=zz[:], in0=t_[:], in1=gate[:, i, :], op=ALU.mult),
                             reads=[tk, gk], writes=[zk])
                        epi(i, zz[:], zk, grow[:], "y_grow")
            epi.finish()
            P.flush()


KB.phase0_hyena = _phase0_hyena
KB.mix_hy = _mix_hy


def kernel(**inputs):
    kb = KB()
    nc = kb.build()
    maps = make_in_maps(inputs, ncores=8)
    res = run_bass_kernel_spmd(nc, maps, core_ids=list(range(8)))
    return np.concatenate([np.asarray(r["out"], dtype=np.float32) for r in res.results], axis=0)
```
